# Optimizing a Trainium2 kernel written in Bass

```python
import math
import jax
import jax.numpy as jnp
from jax import lax
import numpy as np

D_MODEL = 1024
BATCH = 8
SEQ = 2048
DEPTH = 2

ATTN_HEADS = 8
ATTN_HEAD_DIM = 64
ATTN_WIDTH = ATTN_HEADS * ATTN_HEAD_DIM
MOBA_BLOCK = 256
MOBA_TOPK = 3
Q_CHUNK = 16
SGU_GROUPS = 8
SGU_WIDTH = 512
SGU_GROUP_DIM = SGU_WIDTH // SGU_GROUPS
SGU_CHUNK = 128
SSM_WIDTH = 512
SSM_GROUP_DIM = 16
SSM_GROUPS = SSM_WIDTH // SSM_GROUP_DIM
SSM_STATE = 64
N_BRANCHES = 3
OFF_K = ATTN_WIDTH
OFF_V = 2 * ATTN_WIDTH
OFF_SGU = 3 * ATTN_WIDTH
OFF_SSM = OFF_SGU + 2 * SGU_WIDTH
OFF_GATE = OFF_SSM + SSM_WIDTH
IN_COLS = OFF_GATE + N_BRANCHES * D_MODEL
D_FF_DENSE = 2816
N_EXPERTS = 8
TOP_K = 2
D_FF_EXPERT = 3584
N_DENSE = (DEPTH + 1) // 2
N_MOE = DEPTH // 2
DN_ALPHA = (2.0 * DEPTH) ** 0.25
DN_BETA = (8.0 * DEPTH) ** -0.25
LN_EPS = 1e-5

kernel_name = 'hybrid_moba_sgu_s5_moe_deepnorm'


def layer_norm(x, g, b):
    xf = x.astype(jnp.float32)
    mu = jnp.mean(xf, axis=-1, keepdims=True)
    var = jnp.mean(jnp.square(xf - mu), axis=-1, keepdims=True)
    y = (xf - mu) * lax.rsqrt(var + LN_EPS)
    return (y * g.astype(jnp.float32) + b.astype(jnp.float32)).astype(x.dtype)


def moba_attention(q, k, v):
    bsz, t, h, dh = q.shape
    nb = -(-t // MOBA_BLOCK)
    pad = nb * MOBA_BLOCK - t
    q = q.transpose(0, 2, 1, 3)
    k = jnp.pad(k.transpose(0, 2, 1, 3), ((0, 0), (0, 0), (0, pad), (0, 0)))
    v = jnp.pad(v.transpose(0, 2, 1, 3), ((0, 0), (0, 0), (0, pad), (0, 0)))
    kb = k.reshape(bsz, h, nb, MOBA_BLOCK, dh)
    vb = v.reshape(bsz, h, nb, MOBA_BLOCK, dh)
    k_mean = jnp.mean(kb.astype(jnp.float32), axis=3).astype(q.dtype)
    n_sel = max(1, min(MOBA_TOPK, nb - 1))
    scale = dh ** -0.5
    bi = jnp.arange(bsz)[:, None, None, None]
    hi = jnp.arange(h)[None, :, None, None]

    def chunk(c):
        start = c * Q_CHUNK
        blk = start // MOBA_BLOCK
        qc = lax.dynamic_slice_in_dim(q, start, Q_CHUNK, axis=2)
        qpos = start + jnp.arange(Q_CHUNK)
        gate = jnp.einsum('bhqd,bhnd->bhqn', qc, k_mean).astype(jnp.float32)
        gate = jnp.where(jnp.arange(nb) < blk, gate, -jnp.inf)
        _, idx = lax.top_k(gate, n_sel)
        slot_ok = jnp.arange(n_sel) < blk
        k_sel = kb[bi, hi, idx]
        v_sel = vb[bi, hi, idx]
        s_sel = jnp.einsum('bhqd,bhqskd->bhqsk', qc, k_sel).astype(jnp.float32) * scale
        s_sel = jnp.where(slot_ok[:, None], s_sel, -jnp.inf)
        s_sel = s_sel.reshape(bsz, h, Q_CHUNK, n_sel * MOBA_BLOCK)
        k_own = lax.dynamic_slice_in_dim(kb, blk, 1, axis=2)[:, :, 0]
        v_own = lax.dynamic_slice_in_dim(vb, blk, 1, axis=2)[:, :, 0]
        kpos = blk * MOBA_BLOCK + jnp.arange(MOBA_BLOCK)
        s_own = jnp.einsum('bhqd,bhkd->bhqk', qc, k_own).astype(jnp.float32) * scale
        s_own = jnp.where(kpos[None, :] <= qpos[:, None], s_own, -jnp.inf)
        p = jax.nn.softmax(jnp.concatenate([s_own, s_sel], axis=-1), axis=-1).astype(v.dtype)
        p_own = p[..., :MOBA_BLOCK]
        p_sel = p[..., MOBA_BLOCK:].reshape(bsz, h, Q_CHUNK, n_sel, MOBA_BLOCK)
        return (jnp.einsum('bhqk,bhkd->bhqd', p_own, v_own)
                + jnp.einsum('bhqsk,bhqskd->bhqd', p_sel, v_sel))

    out = lax.map(chunk, jnp.arange(t // Q_CHUNK))
    return out.transpose(1, 0, 3, 2, 4).reshape(bsz, t, h * dh)


def spatial_gating(z, ws, bs, ln_g, ln_b):
    bsz, t, _ = z.shape
    nc = t // SGU_CHUNK
    u, v = jnp.split(z, 2, axis=-1)
    v = layer_norm(v.reshape(bsz, t, SGU_GROUPS, SGU_GROUP_DIM), ln_g, ln_b)
    v = v.reshape(bsz, nc, SGU_CHUNK, SGU_GROUPS, SGU_GROUP_DIM)
    causal = jnp.tril(jnp.ones((SGU_CHUNK, SGU_CHUNK), dtype=bool))
    w = jnp.where(causal, ws, 0.0)
    mixed = jnp.einsum('gts,bcsgd->bctgd', w, v) + bs.T[None, None, :, :, None]
    return u * mixed.reshape(bsz, t, SGU_WIDTH)


def s5_layer(u, lam_re, lam_im, log_dt, b_re, b_im, c_re, c_im, d_skip):
    bsz, t, _ = u.shape
    uf = u.astype(jnp.float32).reshape(bsz, t, SSM_GROUPS, SSM_GROUP_DIM)
    lam = lax.complex(lam_re.astype(jnp.float32), lam_im.astype(jnp.float32))
    dt = jnp.exp(log_dt.astype(jnp.float32))[:, None]
    lam_bar = jnp.exp(lam * dt)
    b = lax.complex(b_re.astype(jnp.float32), b_im.astype(jnp.float32))
    b_bar = ((lam_bar - 1.0) / lam)[..., None] * b
    bu = jnp.einsum('gpi,btgi->btgp', b_bar, uf.astype(jnp.complex64))
    a = jnp.broadcast_to(lam_bar, bu.shape)

    def combine(left, right):
        a_l, x_l = left
        a_r, x_r = right
        return a_r * a_l, a_r * x_l + x_r

    _, states = lax.associative_scan(combine, (a, bu), axis=1)
    c = lax.complex(c_re.astype(jnp.float32), c_im.astype(jnp.float32))
    y = jnp.real(jnp.einsum('gip,btgp->btgi', c, states))
    y = y + d_skip.astype(jnp.float32).reshape(SSM_GROUPS, SSM_GROUP_DIM) * uf
    return y.reshape(bsz, t, SSM_WIDTH).astype(u.dtype)


def token_mixer(x, w_in, gate_bias, sgu_ws, sgu_bias, sgu_ln_g, sgu_ln_b,
                lam_re, lam_im, log_dt, b_re, b_im, c_re, c_im, d_skip,
                w_glu, b_glu, p_attn, p_sgu, p_ssm, w_out):
    bsz, t, _ = x.shape
    hcat = x @ w_in
    q, k, v, z_sgu, u_ssm, g = jnp.split(hcat, [OFF_K, OFF_V, OFF_SGU, OFF_SSM, OFF_GATE], axis=-1)
    shp = (bsz, t, ATTN_HEADS, ATTN_HEAD_DIM)
    y_a = moba_attention(q.reshape(shp), k.reshape(shp), v.reshape(shp))
    y_b = spatial_gating(jax.nn.gelu(z_sgu), sgu_ws, sgu_bias, sgu_ln_g, sgu_ln_b)
    y_c = jax.nn.gelu(s5_layer(u_ssm, lam_re, lam_im, log_dt, b_re, b_im, c_re, c_im, d_skip))
    y_c = y_c * jax.nn.sigmoid(y_c @ w_glu + b_glu)
    gates = jax.nn.sigmoid(g.reshape(bsz, t, N_BRANCHES, D_MODEL) + gate_bias)
    merged = (gates[:, :, 0] * (y_a @ p_attn)
              + gates[:, :, 1] * (y_b @ p_sgu)
              + gates[:, :, 2] * (y_c @ p_ssm))
    return merged @ w_out


def swiglu(x, wg, wu, wd):
    return (jax.nn.silu(x @ wg) * (x @ wu)) @ wd


def moe_swiglu(x, router, router_bias, wg, wu, wd):
    logits = (x @ router).astype(jnp.float32) + router_bias.astype(jnp.float32)
    top_vals, top_idx = lax.top_k(logits, TOP_K)
    top_w = jax.nn.softmax(top_vals, axis=-1)
    out = jnp.zeros_like(x)
    for e in range(N_EXPERTS):
        w_e = jnp.sum(jnp.where(top_idx == e, top_w, 0.0), axis=-1, keepdims=True)
        out = out + w_e.astype(x.dtype) * swiglu(x, wg[e], wu[e], wd[e])
    return out


def setup_inputs(seed: int = 0) -> dict:
    key = jax.random.key(seed)
    ks = list(jax.random.split(key, 40))
    f32 = jnp.float32

    def nrm(shape, scale):
        return jax.random.normal(ks.pop(), shape, f32) * scale

    L = DEPTH
    G, C = SGU_GROUPS, SGU_CHUNK
    S, P, Hg = SSM_GROUPS, SSM_STATE, SSM_GROUP_DIM
    lam_im_init = math.pi * jnp.arange(P, dtype=f32)
    return {
        'x': nrm((BATCH, SEQ, D_MODEL), 1.0),
        'w_in': nrm((L, D_MODEL, IN_COLS), D_MODEL ** -0.5),
        'gate_bias': nrm((L, N_BRANCHES, D_MODEL), 0.1),
        'sgu_ws': nrm((L, G, C, C), C ** -0.5),
        'sgu_bias': 1.0 + nrm((L, G, C), 0.1),
        'sgu_ln_g': 1.0 + nrm((L, G, SGU_GROUP_DIM), 0.1),
        'sgu_ln_b': nrm((L, G, SGU_GROUP_DIM), 0.1),
        'ssm_lam_re': -0.5 + nrm((L, S, P), 0.01),
        'ssm_lam_im': lam_im_init + nrm((L, S, P), 0.01),
        'ssm_log_dt': jax.random.uniform(ks.pop(), (L, S), f32, math.log(1e-3), math.log(1e-1)),
        'ssm_b_re': nrm((L, S, P, Hg), (2.0 * Hg) ** -0.5),
        'ssm_b_im': nrm((L, S, P, Hg), (2.0 * Hg) ** -0.5),
        'ssm_c_re': nrm((L, S, Hg, P), (2.0 * P) ** -0.5),
        'ssm_c_im': nrm((L, S, Hg, P), (2.0 * P) ** -0.5),
        'ssm_d': nrm((L, SSM_WIDTH), 0.5),
        'ssm_w_glu': nrm((L, SSM_WIDTH, SSM_WIDTH), SSM_WIDTH ** -0.5),
        'ssm_b_glu': nrm((L, SSM_WIDTH), 0.1),
        'p_attn': nrm((L, ATTN_WIDTH, D_MODEL), DN_BETA * ATTN_WIDTH ** -0.5),
        'p_sgu': nrm((L, SGU_WIDTH, D_MODEL), DN_BETA * SGU_WIDTH ** -0.5),
        'p_ssm': nrm((L, SSM_WIDTH, D_MODEL), DN_BETA * SSM_WIDTH ** -0.5),
        'w_out': nrm((L, D_MODEL, D_MODEL), DN_BETA * D_MODEL ** -0.5),
        'ln1_g': 1.0 + nrm((L, D_MODEL), 0.1),
        'ln1_b': nrm((L, D_MODEL), 0.1),
        'ffn_w_gate': nrm((N_DENSE, D_MODEL, D_FF_DENSE), D_MODEL ** -0.5),
        'ffn_w_up': nrm((N_DENSE, D_MODEL, D_FF_DENSE), D_MODEL ** -0.5),
        'ffn_w_down': nrm((N_DENSE, D_FF_DENSE, D_MODEL), DN_BETA * D_FF_DENSE ** -0.5),
        'moe_router': nrm((N_MOE, D_MODEL, N_EXPERTS), D_MODEL ** -0.5),
        'moe_router_bias': nrm((N_MOE, N_EXPERTS), 0.01),
        'moe_w_gate': nrm((N_MOE, N_EXPERTS, D_MODEL, D_FF_EXPERT), D_MODEL ** -0.5),
        'moe_w_up': nrm((N_MOE, N_EXPERTS, D_MODEL, D_FF_EXPERT), D_MODEL ** -0.5),
        'moe_w_down': nrm((N_MOE, N_EXPERTS, D_FF_EXPERT, D_MODEL), DN_BETA * D_FF_EXPERT ** -0.5),
        'ln2_g': 1.0 + nrm((L, D_MODEL), 0.1),
        'ln2_b': nrm((L, D_MODEL), 0.1),
    }


def reference(x, w_in, gate_bias, sgu_ws, sgu_bias, sgu_ln_g, sgu_ln_b,
              ssm_lam_re, ssm_lam_im, ssm_log_dt, ssm_b_re, ssm_b_im, ssm_c_re, ssm_c_im,
              ssm_d, ssm_w_glu, ssm_b_glu, p_attn, p_sgu, p_ssm, w_out, ln1_g, ln1_b,
              ffn_w_gate, ffn_w_up, ffn_w_down, moe_router, moe_router_bias,
              moe_w_gate, moe_w_up, moe_w_down, ln2_g, ln2_b):
    for layer in range(DEPTH):
        mix = token_mixer(x, w_in[layer], gate_bias[layer], sgu_ws[layer], sgu_bias[layer],
                          sgu_ln_g[layer], sgu_ln_b[layer], ssm_lam_re[layer], ssm_lam_im[layer],
                          ssm_log_dt[layer], ssm_b_re[layer], ssm_b_im[layer], ssm_c_re[layer],
                          ssm_c_im[layer], ssm_d[layer], ssm_w_glu[layer], ssm_b_glu[layer],
                          p_attn[layer], p_sgu[layer], p_ssm[layer], w_out[layer])
        x = layer_norm(DN_ALPHA * x + mix, ln1_g[layer], ln1_b[layer])
        i = layer // 2
        if layer % 2 == 0:
            f = swiglu(x, ffn_w_gate[i], ffn_w_up[i], ffn_w_down[i])
        else:
            f = moe_swiglu(x, moe_router[i], moe_router_bias[i], moe_w_gate[i], moe_w_up[i], moe_w_down[i])
        x = layer_norm(DN_ALPHA * x + f, ln2_g[layer], ln2_b[layer])
    return x
```

```python
import contextlib
import numpy as np
import concourse.bass as bass
import concourse.mybir as mybir
from concourse.bass_utils import run_bass_kernel_spmd

F32 = mybir.dt.float32
BF16 = mybir.dt.bfloat16
I32 = mybir.dt.int32
ALU = mybir.AluOpType
AF = mybir.ActivationFunctionType
AX = mybir.AxisListType

ENGS = ("pe", "act", "dve", "pool", "sp")

T = 2048
D = 1024
NL = 2
DN_ALPHA = (2.0 * NL) ** 0.25
LN_EPS = 1e-5
NEG = -30000.0
OFF_SGU = 1536
OFF_SSM = 2560
OFF_GATE = 3072
F_DENSE = 2816
F_EXP = 3584
NEXP = 8
MOE_FAKE = False
RING_NS = 4


class Op:
    __slots__ = ("eng", "fn", "deps", "dma", "semkey", "sig", "cnt")

    def __init__(self, eng, fn, deps, dma, semkey):
        self.eng = eng
        self.fn = fn
        self.deps = deps
        self.dma = dma
        self.semkey = semkey
        self.sig = False
        self.cnt = 0


class Prog:
    def __init__(self, nc):
        self.nc = nc
        self.ops = []
        self.state = {}
        self.dma_counts = {}
        self.stack = contextlib.ExitStack()
        self._nm = 0
        self.epoch = 0
        self.barrier_ops = []
        self.last_eng = {}
        self.last_dma = {}

    def sb(self, shape, dtype, name=None):
        self._nm += 1
        return self.stack.enter_context(self.nc.sbuf_tensor(name or f"sb{self._nm}", list(shape), dtype))

    def ps(self, shape, dtype=F32, name=None):
        self._nm += 1
        return self.stack.enter_context(self.nc.psum_tensor(name or f"ps{self._nm}", list(shape), dtype))

    def barrier(self):
        self.epoch += 1
        self.barrier_ops = list(self.last_eng.values()) + list(self.last_dma.values())

    def _deps(self, reads, writes):
        deps = []
        seen = set()

        def add(o):
            if o is not None and id(o) not in seen:
                seen.add(id(o))
                deps.append(o)
        for r in reads:
            st = self.state.get(r)
            if st is not None:
                add(st[0])
        for w in writes:
            st = self.state.get(w)
            if st is not None:
                add(st[0])
                lastr = {}
                for o in st[1]:
                    lastr[(o.eng, o.semkey if o.dma else None)] = o
                for o in lastr.values():
                    add(o)
            exempt = isinstance(w, tuple) and w[0] == "ring"
            if not exempt and (st is None or st[2] < self.epoch):
                for o in self.barrier_ops:
                    add(o)
        return deps

    def _update(self, o, reads, writes):
        for r in reads:
            st = self.state.setdefault(r, [None, [], self.epoch])
            st[1].append(o)
        for w in writes:
            self.state[w] = [o, [], self.epoch]

    def op(self, eng, fn, reads=(), writes=(), dma=False, semkey=None, slot=None):
        o = Op(eng, fn, self._deps(reads, writes), dma, semkey)
        if dma:
            self.dma_counts[semkey] = self.dma_counts.get(semkey, 0) + 1
            if slot is None:
                self.last_dma[semkey] = o
        else:
            self.last_eng[eng] = o
        self._update(o, reads, writes)
        if slot is None:
            self.ops.append(o)
        else:
            slot.append(o)
        return o

    def placeholder(self):
        ph = []
        self.ops.append(ph)
        return ph

    def alias(self, new_keys, old_keys):
        olds = []
        for k in old_keys:
            st = self.state.get(k)
            if st is not None:
                if st[0] is not None:
                    olds.append(st[0])
                olds.extend(st[1])
        for k in new_keys:
            st = self.state.setdefault(k, [None, [], self.epoch])
            st[1].extend(olds)

    def dma(self, eng, out, in_, reads=(), writes=(), semkey=None, slot=None, **kw):
        sk = semkey if semkey is not None else writes[0]
        return self.op(eng, lambda e: e.dma_start(out=out, in_=in_, **kw), reads=reads, writes=writes,
                       dma=True, semkey=sk, slot=slot)

    def mm(self, out, lhsT, rhs, start, stop, reads=(), writes=()):
        return self.op("pe", lambda e: e.matmul(out, lhsT, rhs, start=start, stop=stop), reads=reads, writes=writes)

    def emit(self):
        nc = self.nc
        ops = []
        for o in self.ops:
            if isinstance(o, list):
                ops.extend(o)
            else:
                ops.append(o)
        for o in ops:
            for d in o.deps:
                if not d.dma and not (d.eng == "pe" and o.eng == "pe"):
                    d.sig = True
        cnt = {e: 0 for e in ENGS}
        for o in ops:
            if not o.dma and o.sig:
                cnt[o.eng] += 1
                o.cnt = cnt[o.eng]
        run = {}
        for o in ops:
            if o.dma:
                run[o.semkey] = run.get(o.semkey, 0) + 1
                o.cnt = run[o.semkey]
        run = {}
        waits = []
        for o in ops:
            w = {}
            for d in o.deps:
                if d.dma:
                    key = ("dma", d.semkey)
                    if isinstance(d.semkey, tuple) and d.semkey[0] == "ring":
                        val = 16 * d.cnt
                    else:
                        val = 16 * max(run.get(d.semkey, 0), d.cnt)
                else:
                    if d.eng == "pe" and o.eng == "pe":
                        continue
                    key = ("eng", d.eng)
                    val = d.cnt
                if w.get(key, 0) < val:
                    w[key] = val
            waits.append(w)
            if o.dma:
                run[o.semkey] = run.get(o.semkey, 0) + 1
        sems = {}
        for e in ENGS:
            sems[("eng", e)] = self.stack.enter_context(nc.semaphore(f"s_{e}"))
        for i, k in enumerate(self.dma_counts.keys()):
            sems[("dma", k)] = self.stack.enter_context(nc.semaphore(f"d_{i}"))
        self.n_sems = len(sems)
        per_eng = {e: [] for e in ENGS}
        for o, w in zip(ops, waits):
            per_eng[o.eng].append((o, w))
        self.stats = {e: len(per_eng[e]) for e in ENGS}
        self.sigcnt = cnt

        semv = {k: 0 for k in sems}
        pos = {e: 0 for e in ENGS}
        progress = True
        while progress:
            progress = False
            for e in ENGS:
                q = per_eng[e]
                while pos[e] < len(q):
                    o, w = q[pos[e]]
                    if all(semv[k] >= v for k, v in w.items()):
                        if o.dma:
                            semv[("dma", o.semkey)] += 16
                        elif o.sig:
                            semv[("eng", e)] += 1
                        pos[e] += 1
                        progress = True
                    else:
                        break
        stuck = {e: (pos[e], len(per_eng[e])) for e in ENGS if pos[e] < len(per_eng[e])}
        if stuck:
            msg = []
            for e in stuck:
                o, w = per_eng[e][pos[e]]
                msg.append((e, pos[e], {k: (v, semv[k]) for k, v in w.items() if semv[k] < v}))
            raise RuntimeError(f"sync deadlock: {msg}")

        def run_engine(ename, eobj):
            waited = {}
            for o, w in per_eng[ename]:
                for key, val in w.items():
                    if waited.get(key, 0) >= val:
                        continue
                    waited[key] = val
                    eobj.wait_ge(sems[key], val)
                ins = o.fn(eobj)
                if o.dma:
                    ins.then_inc(sems[("dma", o.semkey)], 16)
                elif o.sig:
                    ins.then_inc(sems[("eng", ename)], 1)
            last = {}
            for o, w in per_eng[ename]:
                if o.dma:
                    last[o.semkey] = True
            for k in last:
                eobj.wait_ge(sems[("dma", k)], 16 * self.dma_counts[k])

        with nc.Block() as block:
            @block.sync
            def _(e):
                run_engine("sp", e)

            @block.scalar
            def _(e):
                run_engine("act", e)

            @block.vector
            def _(e):
                run_engine("dve", e)

            @block.gpsimd
            def _(e):
                run_engine("pool", e)

            @block.tensor
            def _(e):
                run_engine("pe", e)
        self.stack.close()


W_SHAPES = {
    "w_in": [2, 1024, 6144], "gate_bias": [2, 3, 1024], "sgu_ws": [2, 8, 128, 128], "sgu_bias": [2, 8, 128],
    "sgu_ln_g": [2, 8, 64], "sgu_ln_b": [2, 8, 64], "ssm_lam_re": [2, 32, 64], "ssm_lam_im": [2, 32, 64],
    "ssm_log_dt": [2, 32], "ssm_b_re": [2, 32, 64, 16], "ssm_b_im": [2, 32, 64, 16], "ssm_c_re": [2, 32, 16, 64],
    "ssm_c_im": [2, 32, 16, 64], "ssm_d": [2, 512], "ssm_w_glu": [2, 512, 512], "ssm_b_glu": [2, 512],
    "p_attn": [2, 512, 1024], "p_sgu": [2, 512, 1024], "p_ssm": [2, 512, 1024], "w_out": [2, 1024, 1024],
    "ln1_g": [2, 1024], "ln1_b": [2, 1024], "ffn_w_gate": [1, 1024, 2816], "ffn_w_up": [1, 1024, 2816],
    "ffn_w_down": [1, 2816, 1024], "moe_router": [1, 1024, 8], "moe_router_bias": [1, 8],
    "moe_w_gate": [1, 8, 1024, 3584], "moe_w_up": [1, 8, 1024, 3584], "moe_w_down": [1, 8, 3584, 1024],
    "ln2_g": [2, 1024], "ln2_b": [2, 1024],
}


def host_consts():
    c = {}
    c["c_ident"] = np.eye(128, dtype=np.float32)
    k = np.arange(128)[:, None, None]
    j = np.arange(4)[None, :, None]
    q = np.arange(512)[None, None, :]
    c["c_caus"] = np.where(j * 128 + k > q, NEG, 0.0).astype(np.float32)
    sel = np.zeros((96, 8, 128), np.float32)
    for hh in range(3):
        for n in range(8):
            sel[32 * hh + n, n, :] = 1.0
    c["c_sel96"] = sel
    s = np.arange(128)
    c["c_m01sgu"] = (s[:, None] <= s[None, :]).astype(np.float32)
    c["c_m01ssm"] = ((s[None, :] // 16) >= (s[:, None] // 16)).astype(np.float32)
    kv = np.concatenate([-np.arange(8), np.arange(8), np.arange(1, 9)]).astype(np.float32)
    c["c_kvec"] = np.tile(kv[None, :], (128, 1))
    return c


CONST_SHAPES = {"c_ident": [128, 128], "c_caus": [128, 4, 512], "c_sel96": [96, 8, 128], "c_m01sgu": [128, 128],
                "c_m01ssm": [128, 128], "c_kvec": [128, 24]}

PHASES = ["qkv", "attn", "sgu", "ssmw", "ssm", "merge", "ln1", "ffn", "ln2"]


def build_program(debug=None, nlayers=NL, dbg_layer=0):
    nc = bass.Bass("TRN2", target_bir_lowering=False)
    din = {}
    din["x"] = nc.dram_tensor("x", [T, D], F32, kind="ExternalInput").ap()
    for n, shp in W_SHAPES.items():
        din[n] = nc.dram_tensor(n, shp, F32, kind="ExternalInput").ap()
    for n, shp in CONST_SHAPES.items():
        din[n] = nc.dram_tensor(n, shp, F32, kind="ExternalInput").ap()
    out_ap = nc.dram_tensor("out", [T, D], F32, kind="ExternalOutput").ap()
    dbg_ap = None
    if debug is not None:
        dbg_ap = nc.dram_tensor("dbg", [128, 16384], F32, kind="ExternalOutput").ap()
    xs_ap = nc.dram_tensor("xs_scr", [T, D], F32).ap()
    scrU = nc.dram_tensor("scrU", [128, 4, 8, 256], BF16).ap()
    scrY = nc.dram_tensor("scrY", [128, 32, 256], BF16).ap()

    P = Prog(nc)
    xT = P.sb([128, 8, T], BF16, "xT")
    AB = P.sb([128, 32768], BF16, "AB")
    C = P.sb([128, 24576], BF16, "C")
    NS = RING_NS
    ring = [P.sb([128, 4096], BF16, f"ring{i}") for i in range(NS)]
    ident = P.sb([128, 128], F32, "ident")
    identb = P.sb([128, 128], BF16, "identb")
    caus = P.sb([128, 4, 512], BF16, "caus")
    sel96 = P.sb([96, 8, 128], BF16, "sel96")
    m01sgu = P.sb([128, 128], F32, "m01sgu")
    m01ssm = P.sb([128, 128], F32, "m01ssm")
    kvec = P.sb([128, 24], F32, "kvec")
    lnG = P.sb([128, 1024], F32, "lnG")
    lnB = P.sb([128, 1024], F32, "lnB")
    vecS = P.sb([32, 128], F32, "vecS")
    vecT = P.sb([128, 32], F32, "vecT")
    small = P.sb([128, 1024], F32, "small")
    xin = [P.sb([128, 1024], F32, f"xin{i}") for i in range(2)]
    psb = [P.ps([128, 512], F32, f"pb{i}") for i in range(8)]

    yaT = AB[:, 0:8192].rearrange("p (c t) -> p c t", c=4)
    ybT = AB[:, 8192:16384].rearrange("p (c t) -> p c t", c=4)
    ycT = AB[:, 16384:24576].rearrange("p (c t) -> p c t", c=4)
    xres = AB[:].bitcast(F32).rearrange("p (t d) -> p t d", d=1024)

    def Cf32(off, n):
        return C[:, off:off + 2 * n].bitcast(F32)

    def ABf32(off, n):
        return AB[:, off:off + 2 * n].bitcast(F32)

    def K(name, rng):
        return [(name, i) for i in rng]

    def act(out, in_, func, R, W, bias=0.0, scale=1.0):
        P.op("act", lambda e: e.activation(out=out, in_=in_, func=func, bias=bias, scale=scale), reads=R, writes=W)

    def tt(out, in0, in1, op, R, W, eng="dve"):
        P.op(eng, lambda e: e.tensor_tensor(out=out, in0=in0, in1=in1, op=op), reads=R, writes=W)

    def ts(out, in0, s1, s2, op0, op1, R, W, eng="dve"):
        if s2 is None:
            P.op(eng, lambda e: e.tensor_scalar(out=out, in0=in0, scalar1=s1, scalar2=None, op0=op0), reads=R, writes=W)
        else:
            P.op(eng, lambda e: e.tensor_scalar(out=out, in0=in0, scalar1=s1, scalar2=s2, op0=op0, op1=op1),
                 reads=R, writes=W)

    def stt(out, in0, scalar, in1, op0, op1, R, W, eng="dve"):
        P.op(eng, lambda e: e.scalar_tensor_tensor(out=out, in0=in0, scalar=scalar, in1=in1, op0=op0, op1=op1),
             reads=R, writes=W)

    def cp(out, in_, R, W, eng="dve"):
        if eng == "act":
            P.op("act", lambda e: e.copy(out=out, in_=in_), reads=R, writes=W)
        else:
            P.op(eng, lambda e: e.tensor_copy(out=out, in_=in_), reads=R, writes=W)

    def red(out, in_, R, W, op=ALU.add):
        P.op("dve", lambda e: e.tensor_reduce(out=out, in_=in_, axis=AX.X, op=op), reads=R, writes=W)

    def memset(ap, val, W, eng="dve", R=()):
        P.op(eng, lambda e: e.memset(ap, val), reads=R, writes=W)

    def recip(out, in_, R, W):
        P.op("dve", lambda e: e.reciprocal(out=out, in_=in_), reads=R, writes=W)

    def transpose(out, in_, idn, R, W):
        P.op("pe", lambda e: e.transpose(out, in_, idn), reads=list(R) + ["ident"], writes=W)

    def mm(out, lhsT, rhs, start, stop, R, W, sgc=False):
        if sgc:
            P.op("pe", lambda e: e.matmul(out, lhsT, rhs, start=start, stop=stop, skip_group_check=True),
                 reads=R, writes=W)
        else:
            P.op("pe", lambda e: e.matmul(out, lhsT, rhs, start=start, stop=stop), reads=R, writes=W)

    class WS:
        def __init__(self):
            self.n = 0
            self.ph = {}

        def acquire(self, name, idx, r0, nr, c0, ncw):
            n = self.n
            self.n += 1
            kc = nr // 128
            assert kc * ncw <= 4096 and nr % 128 == 0
            src = din[name]
            for i in idx:
                src = src[i]
            src = src[r0:r0 + nr, c0:c0 + ncw].rearrange("(kc p) n -> p kc n", p=128)
            s = n % NS
            view = ring[s][:, 0:kc * ncw].rearrange("p (kc n) -> p kc n", n=ncw)
            key = ("ring", s)
            slot = None if n < NS else self.ph[n - NS + 1]
            P.dma("pool", view, src, writes=[key], slot=slot)
            self.ph[n] = P.placeholder()
            return view, key

    ws = WS()

    P.dma("sp", ident[:], din["c_ident"], writes=["ident"], semkey="setup")
    P.dma("sp", m01sgu[:], din["c_m01sgu"], writes=["m01sgu"], semkey="setup")
    P.dma("sp", m01ssm[:], din["c_m01ssm"], writes=["m01ssm"], semkey="setup")
    P.dma("sp", kvec[:], din["c_kvec"], writes=["kvec"], semkey="setup")
    P.dma("pool", identb[:], din["c_ident"], writes=["identb"], semkey="setupc")
    P.dma("pool", caus[:], din["c_caus"], writes=["caus"], semkey="setupc")
    P.dma("pool", sel96[:], din["c_sel96"], writes=["sel96"], semkey="setupc")

    pcnt = [0]

    def next_ps():
        i = pcnt[0] % 2
        pcnt[0] += 1
        return psb[i], ("ps", i)

    mcnt = [0]

    def misc_ps():
        i = 6 + mcnt[0] % 2
        mcnt[0] += 1
        return psb[i], ("ps", i)

    dbg_off = [0]

    def dump(ap2d, R, ncols):
        done = 0
        i = 0
        while done < ncols:
            n = min(1024, ncols - done)
            st = xin[i % 2]
            kx = ("xin", i % 2)
            cp(st[:, 0:n], ap2d[:, done:done + n], list(R), [kx])
            P.dma("sp", dbg_ap[:, dbg_off[0]:dbg_off[0] + n], st[:, 0:n], reads=[kx], writes=[("dbgout", dbg_off[0])],
                  semkey="dbg")
            dbg_off[0] += n
            done += n
            i += 1

    class Stop(Exception):
        pass

    def stage(name, L):
        return debug == name and L == dbg_layer

    def tiles_to_xT(tt_, src_tile, ksrc, router=None):
        for h2 in range(2):
            pm, kpm = misc_ps()
            for c4 in range(4):
                kc = h2 * 4 + c4
                transpose(pm[:, c4 * 128:(c4 + 1) * 128], src_tile[:, kc * 128:(kc + 1) * 128], ident[:], ksrc, [kpm])
            cp(xT[:, h2 * 4:(h2 + 1) * 4, tt_ * 128:(tt_ + 1) * 128], pm[:].rearrange("p (c t) -> p c t", c=4),
               [kpm], [("xT", tt_)], eng="act" if (h2 and router is None) else "dve")
            if router is not None:
                router(tt_, h2, pm, kpm)

    for tt_ in range(16):
        xb = xin[tt_ % 2]
        kx = ("xin", tt_ % 2)
        P.dma("sp", xb[:], din["x"][tt_ * 128:(tt_ + 1) * 128, :], writes=[kx])
        tiles_to_xT(tt_, xb, [kx])

    try:
        for L in range(nlayers):
            P.dma("sp", vecS[0:24, :], din["gate_bias"][L].rearrange("b (n p) -> (b n) p", p=128), writes=["vecS"],
                  semkey="vec")
            P.dma("sp", vecS[24:28, :], din["ssm_b_glu"][L].rearrange("(n p) -> n p", p=128), writes=["vecS2"],
                  semkey="vec")
            pm, kpm = misc_ps()
            transpose(pm[:, 0:28], vecS[0:28, :], ident[0:28, 0:28], ["vecS", "vecS2"], [kpm])
            cp(vecT[:, 0:28], pm[:, 0:28], [kpm], ["vecT"])

            P.barrier()
            qT = C[:, 0:8192].rearrange("p (c t) -> p c t", c=4)
            kT = C[:, 8192:16384].rearrange("p (c t) -> p c t", c=4)
            vA = AB[:, 8192:16512].rearrange("p (t h e) -> p t h e", t=16, h=8)
            mbT3 = AB[:, 16512:22656].rearrange("p (s t) -> p s t", s=3)
            yatok = ABf32(22656, 2048).rearrange("p (q f) -> p q f", q=4)
            PT = [AB[:, 26752 + i * 512:26752 + (i + 1) * 512] for i in range(2)]
            kmBD = AB[:, 27776:28288].rearrange("p (a c n) -> p a c n", a=2, c=4)

            for blk, (dstT, nm) in enumerate([(qT, "qT"), (kT, "kT")]):
                wv, wk = ws.acquire("w_in", (L,), 0, 1024, blk * 512, 512)
                for m in range(4):
                    for tq in range(4):
                        pacc, kp = next_ps()
                        for kc in range(8):
                            mm(pacc[:], wv[:, kc, m * 128:(m + 1) * 128], xT[:, kc, tq * 512:(tq + 1) * 512],
                               kc == 0, kc == 7, [wk] + K("xT", range(tq * 4, tq * 4 + 4)), [kp])
                        cp(dstT[:, m, tq * 512:(tq + 1) * 512], pacc[:], [kp], [(nm, m, tq)],
                           eng="act" if (m + tq) % 2 == 0 else "dve")
            wv, wk = ws.acquire("w_in", (L,), 0, 1024, 1024, 512)
            memset(vA[:, :, :, 64:65], 1.0, [("vA1",)])
            for tt_ in range(16):
                pacc, kp = next_ps()
                for kc in range(8):
                    mm(pacc[:], xT[:, kc, tt_ * 128:(tt_ + 1) * 128], wv[:, kc, :], kc == 0, kc == 7,
                       [wk, ("xT", tt_)], [kp])
                cp(vA[:, tt_, :, 0:64], pacc[:].rearrange("p (h e) -> p h e", h=8), [kp, ("vA1",)], [("vA", tt_)],
                   eng="act" if tt_ % 2 else "dve")
            qkeys = [("qT", m, tq) for m in range(4) for tq in range(4)]
            kkeys = [("kT", m, tq) for m in range(4) for tq in range(4)]
            if stage("qkv", L):
                dump(qT.rearrange("p c t -> p (c t)"), qkeys, 8192)
                dump(kT.rearrange("p c t -> p (c t)"), kkeys, 8192)
                raise Stop()

            km = small[:, 0:32].rearrange("p (c n) -> p c n", c=4)
            kmr = small[:, 32:64].rearrange("p (c n) -> p c n", c=4)
            red(km, kT.rearrange("p c (n l) -> p c n l", n=8), kkeys, ["km"])
            ts(km, km, 1.0 / 256.0, None, ALU.mult, None, ["km"], ["km"])
            memset(kmBD, 0.0, ["kmBD"])
            for c4 in range(4):
                for hh in range(2):
                    h = 2 * c4 + hh
                    pr = slice(hh * 64, hh * 64 + 64)
                    cp(kmBD[pr, 0, c4, h * 8:(h + 1) * 8], km[pr, c4, :], ["km", "kmBD"], ["kmBD"])
                    cp(kmr[pr, c4, :], kmBD[pr, 0, c4, h * 8:(h + 1) * 8], ["kmBD"], ["kmr"])
                    tt(kmr[pr, c4, :], km[pr, c4, :], kmr[pr, c4, :], ALU.subtract, ["km", "kmr"], ["kmr"])
                    cp(kmBD[pr, 1, c4, h * 8:(h + 1) * 8], kmr[pr, c4, :], ["kmr", "kmBD"], ["kmBD"])
            mbpad = small[:, 64:64 + 288].rearrange("p (h n) -> p h n", h=9)
            mb = mbpad[:, 0:8, 0:8]
            gate_sb = small[:, 352:416].rearrange("p (h n) -> p h n", h=8)
            cmpb = small[:, 416:928].rearrange("p (h n m) -> p h n m", h=8, n=8)
            rank = small[:, 928:992].rearrange("p (h n) -> p h n", h=8)
            memset(mbpad, 0.0, ["mb"])
            for tt_ in range(16):
                b = tt_ // 2
                memset(mb, NEG, ["mb"], R=["mb"])
                if b >= 4:
                    pm, kpm = misc_ps()
                    for c4 in range(4):
                        for a in range(2):
                            mm(pm[:, 0:64], qT[:, c4, tt_ * 128:(tt_ + 1) * 128], kmBD[:, a, c4, :],
                               c4 == 0 and a == 0, c4 == 3 and a == 1, [("qT", c4, tt_ // 4), "kmBD"], [kpm])
                    cp(gate_sb, pm[:, 0:64].rearrange("p (h n) -> p h n", h=8), [kpm], ["gate_sb"])
                    g = gate_sb[:, :, 0:b]
                    in0 = g.unsqueeze(2).broadcast_to([128, 8, b, b])
                    in1 = g.unsqueeze(3).broadcast_to([128, 8, b, b])
                    tt(cmpb[:, :, 0:b, 0:b], in0, in1, ALU.is_gt, ["gate_sb"], ["cmpb"])
                    red(rank[:, :, 0:b], cmpb[:, :, 0:b, 0:b], ["cmpb"], ["rank"])
                    ts(mb[:, :, 0:b], rank[:, :, 0:b], 2.5, NEG, ALU.is_gt, ALU.mult, ["rank", "mb"], ["mb"])
                elif b > 0:
                    memset(mb[:, :, 0:b], 0.0, ["mb"], R=["mb"])
                memset(mb[:, :, b:b + 1], 0.0, ["mb"], R=["mb"])
                pm, kpm = misc_ps()
                for s3 in range(3):
                    transpose(pm[0:96, s3 * 128:(s3 + 1) * 128],
                              mbpad[:, 3 * s3:3 * s3 + 3, :].rearrange("p h n -> p (h n)"), ident[:], ["mb"], [kpm])
                cp(mbT3[0:96, :, tt_ * 128:(tt_ + 1) * 128], pm[0:96, 0:384].rearrange("p (s t) -> p s t", s=3),
                   [kpm], [("mbT", tt_)])
            if stage("mask", L):
                dump(mbT3.rearrange("p s t -> p (s t)"), K("mbT", range(16)), 6144)
                raise Stop()

            steps = [(tq, h, kt) for tq in range(4) for h in range(8) for kt in range(4 * tq + 4)]
            sbuf_of = {}
            scnt = [0]

            def emit_S(tq, h, kt):
                c4, po = h // 2, (h % 2) * 64
                s3, hb = h // 3, 32 * (h % 3)
                i_ = scnt[0] % 2
                scnt[0] += 1
                pS, kS = psb[2 + i_], ("ps", 2 + i_)
                ptb, kpt = PT[i_], ("PT", i_)
                sbuf_of[(tq, h, kt)] = (ptb, kpt)
                diag = kt >= 4 * tq
                need_sel = tq >= 2
                mm(pS[:], kT[po:po + 64, c4, kt * 128:(kt + 1) * 128], qT[po:po + 64, c4, tq * 512:(tq + 1) * 512],
                   True, not (need_sel or diag), [("kT", c4, kt // 4), ("qT", c4, tq)], [kS])
                if need_sel:
                    mm(pS[:], sel96[hb:hb + 8, kt // 2, :], mbT3[hb:hb + 8, s3, tq * 512:(tq + 1) * 512],
                       False, not diag, ["sel96"] + K("mbT", range(tq * 4, tq * 4 + 4)), [kS])
                if diag:
                    mm(pS[:], identb[:], caus[:, kt - 4 * tq, :], False, True, ["identb", "caus"], [kS])
                act(ptb, pS[:], AF.Exp, [kS], [kpt], scale=0.125)

            def emit_PV(tq, h, kt):
                ptb, kpt = sbuf_of.pop((tq, h, kt))
                pv, kpv = psb[4 + h % 2], ("ps", 4 + h % 2)
                pvv = pv[:, 0:260].rearrange("p (q e) -> p q e", q=4)
                for qs in range(4):
                    if kt > 4 * tq + qs:
                        continue
                    first = (kt == 0 and qs == 0)
                    mm(pvv[:, qs, :], ptb[:, qs * 128:(qs + 1) * 128], vA[:, kt, h, :], first, kt == 4 * tq + qs,
                       [kpt, ("vA", kt), ("vA1",)], [kpv], sgc=True)
                if kt == 4 * tq + 3:
                    rc = small[:, 992:996]
                    recip(rc, pvv[:, :, 64], [kpv], ["rc"])
                    tt(yatok[:, :, h * 64:(h + 1) * 64], pvv[:, :, 0:64], rc.unsqueeze(2).broadcast_to([128, 4, 64]),
                       ALU.mult, [kpv, "rc"], [("yatok", h)])
                    if h == 7:
                        for qs in range(4):
                            pm, kpm = misc_ps()
                            for c4 in range(4):
                                transpose(pm[:, c4 * 128:(c4 + 1) * 128], yatok[:, qs, c4 * 128:(c4 + 1) * 128], ident[:],
                                          K("yatok", range(8)), [kpm])
                            tti = tq * 4 + qs
                            cp(yaT[:, :, tti * 128:(tti + 1) * 128], pm[:].rearrange("p (c t) -> p c t", c=4), [kpm],
                               [("yaT", tti)], eng="act" if qs % 2 else "dve")

            emit_S(*steps[0])
            for i_s, st_ in enumerate(steps):
                if i_s + 1 < len(steps):
                    emit_S(*steps[i_s + 1])
                emit_PV(*st_)
            if stage("attn", L):
                dump(yaT.rearrange("p c t -> p (c t)"), K("yaT", range(16)), 8192)
                raise Stop()
            P.barrier()
            vln = C[:, 0:8192].rearrange("p (t f) -> p t f", t=16)
            vg = Cf32(8192, 512)
            sq = Cf32(9216, 512)
            sgG = Cf32(10240, 512)
            sgB = Cf32(11264, 512)
            WsT = C[:, 12288:13312].rearrange("p (g t) -> p g t", g=8)
            wsn = Cf32(13312, 1024).rearrange("p (g s) -> p g s", g=8)
            biasT = Cf32(15360, 512).rearrange("p (i t) -> p i t", i=4)
            stmp = Cf32(16384, 512)
            P.dma("sp", sgG, din["sgu_ln_g"][L].rearrange("g d -> (g d)").partition_broadcast(128), writes=["sgG"],
                  semkey="sgu")
            P.dma("sp", sgB, din["sgu_ln_b"][L].rearrange("g d -> (g d)").partition_broadcast(128), writes=["sgB"],
                  semkey="sgu")
            P.dma("sp", wsn, din["sgu_ws"][L].rearrange("g t s -> t g s"), writes=["wsn"], semkey="sgu")
            for g in range(8):
                P.dma("sp", biasT[(g % 2) * 64:(g % 2) * 64 + 64, g // 2, :],
                      din["sgu_bias"][L][g].partition_broadcast(64), writes=[("biasT", g)], semkey="sgu")
            for g in range(8):
                pm, kpm = misc_ps()
                transpose(pm[:, 0:128], wsn[:, g, :], ident[:], ["wsn"], [kpm])
                tt(WsT[:, g, :], pm[:, 0:128], m01sgu[:], ALU.mult, [kpm, "m01sgu"], [("WsT", g)])
            wv, wk = ws.acquire("w_in", (L,), 0, 1024, OFF_SGU, 512)
            for m in range(4):
                for tq in range(4):
                    pacc, kp = next_ps()
                    for kc in range(8):
                        mm(pacc[:], wv[:, kc, m * 128:(m + 1) * 128], xT[:, kc, tq * 512:(tq + 1) * 512],
                           kc == 0, kc == 7, [wk] + K("xT", range(tq * 4, tq * 4 + 4)), [kp])
                    act(ybT[:, m, tq * 512:(tq + 1) * 512], pacc[:], AF.Gelu_apprx_tanh, [kp], [("ybT", m, tq)])
            wv, wk = ws.acquire("w_in", (L,), 0, 1024, OFF_SGU + 512, 512)
            vgs = [vg, Cf32(17408, 512)]
            sqs = [sq, Cf32(18432, 512)]

            def sgu_bufs(tt_):
                par = tt_ % 2
                o = 24 * par
                return vgs[par], sqs[par], small[:, o:o + 8], small[:, o + 8:o + 16], small[:, o + 16:o + 24], par

            def sgu_A(tt_):
                vg_, sq_, st1, st2, st3, par = sgu_bufs(tt_)
                kv, ks = ("vg", par), ("sq", par)
                k1, k2, k3 = ("st1", par), ("st2", par), ("st3", par)
                vg3 = vg_.rearrange("p (g d) -> p g d", g=8)
                sq3 = sq_.rearrange("p (g d) -> p g d", g=8)
                pacc, kp = next_ps()
                for kc in range(8):
                    mm(pacc[:], xT[:, kc, tt_ * 128:(tt_ + 1) * 128], wv[:, kc, :], kc == 0, kc == 7,
                       [wk, ("xT", tt_)], [kp])
                act(vg_, pacc[:], AF.Gelu_apprx_tanh, [kp], [kv])
                red(st1, vg3, [kv], [k1])
                act(sq_, vg_, AF.Square, [kv], [ks])
                red(st2, sq3, [ks], [k2])
                ts(st1, st1, 1.0 / 64.0, None, ALU.mult, None, [k1], [k1])
                tt(st3, st1, st1, ALU.mult, [k1], [k3])
                stt(st2, st2, 1.0 / 64.0, st3, ALU.mult, ALU.subtract, [k2, k3], [k2])
                ts(st2, st2, LN_EPS, None, ALU.add, None, [k2], [k2])
                act(st2, st2, AF.Sqrt, [k2], [k2])

            def sgu_B(tt_):
                vg_, sq_, st1, st2, st3, par = sgu_bufs(tt_)
                kv = ("vg", par)
                k1, k2 = ("st1", par), ("st2", par)
                vg3 = vg_.rearrange("p (g d) -> p g d", g=8)
                recip(st2, st2, [k2], [k2])
                tt(vg3, vg3, st1.unsqueeze(2).broadcast_to([128, 8, 64]), ALU.subtract, [kv, k1], [kv])
                tt(vg3, vg3, st2.unsqueeze(2).broadcast_to([128, 8, 64]), ALU.mult, [kv, k2], [kv])
                tt(vg_, vg_, sgG, ALU.mult, [kv, "sgG"], [kv])
                tt(vln[:, tt_, :], vg_, sgB, ALU.add, [kv, "sgB"], [("vln", tt_)])

            sgu_A(0)
            for tt_ in range(16):
                if tt_ + 1 < 16:
                    sgu_A(tt_ + 1)
                sgu_B(tt_)
            for tq in range(4):
                for i in range(4):
                    pacc, kp = next_ps()
                    for q4 in range(4):
                        tti = tq * 4 + q4
                        for pi in range(2):
                            g = 2 * i + pi
                            mm(pacc[pi * 64:(pi + 1) * 64, q4 * 128:(q4 + 1) * 128], vln[:, tti, g * 64:(g + 1) * 64],
                               WsT[:, g, :], True, True, [("vln", tti), ("WsT", g)], [kp])
                    tt(stmp.rearrange("p (q t) -> p q t", q=4), pacc[:].rearrange("p (q t) -> p q t", q=4),
                       biasT[:, i, :].unsqueeze(1).broadcast_to([128, 4, 128]), ALU.add,
                       [kp, ("biasT", 2 * i), ("biasT", 2 * i + 1)], ["stmp"])
                    tt(ybT[:, i, tq * 512:(tq + 1) * 512], stmp, ybT[:, i, tq * 512:(tq + 1) * 512], ALU.mult,
                       ["stmp", ("ybT", i, tq)], [("ybT", i, tq)])
            ybkeys = [("ybT", m, tq) for m in range(4) for tq in range(4)]
            if stage("sgu", L):
                dump(ybT.rearrange("p c t -> p (c t)"), ybkeys, 8192)
                raise Stop()
            P.barrier()
            ussmP = AB[:, 16384:24576].rearrange("p (m s c) -> p m s c", m=4, s=8)
            Gm = C[:, 0:2048].rearrange("p (g n) -> p g n", g=16)
            Ere = C[:, 2048:3072].rearrange("p (g n) -> p g n", g=16)
            Eim = C[:, 3072:4096].rearrange("p (g n) -> p g n", g=16)
            Ire = C[:, 4096:5120].rearrange("p (g n) -> p g n", g=8)
            Iimn = C[:, 5120:6144].rearrange("p (g n) -> p g n", g=8)
            uW = C[:, 6144:10240].rearrange("p (g c) -> p g c", g=16)
            ycP = C[:, 10240:18432].rearrange("p (m s c) -> p m s c", m=4, s=8)
            Sb = [[Cf32(18432 + 1024 * (2 * b + ri), 512).rearrange("p (i c) -> p i c", i=2) for ri in range(2)]
                  for b in range(2)]
            Sprev = [C[:, 22528 + 512 * ri:22528 + 512 * (ri + 1)].rearrange("p (i c) -> p i c", i=2) for ri in range(2)]
            tA = Cf32(10240, 384).rearrange("p (i k) -> p i k", i=16)
            tB = Cf32(11008, 384).rearrange("p (i k) -> p i k", i=16)
            tC = Cf32(11776, 384).rearrange("p (i k) -> p i k", i=16)
            tD = Cf32(12544, 384).rearrange("p (i k) -> p i k", i=16)
            tI = C[:, 13312:14080].bitcast(I32).rearrange("p (i k) -> p i k", i=16)
            A0dup = Cf32(14080, 128)
            T5 = Cf32(14336, 1024)
            Xre = ABf32(24576, 1024)
            Ximn = ABf32(24576 + 2048, 1024)
            Yre = ABf32(24576 + 4096, 1024)
            Yim = ABf32(24576 + 6144, 1024)
            pwr = lnG[:, 0:384].rearrange("p (i k) -> p i k", i=16)
            pwi = lnG[:, 384:768].rearrange("p (i k) -> p i k", i=16)
            cfre, cfim = lnG[:, 768:784], lnG[:, 784:800]
            dcol = lnG[:, 800:832]
            lamre, lamim, dtv = lnG[:, 832:848], lnG[:, 848:864], lnG[:, 864:880]
            zr, zi, den = lnG[:, 880:896], lnG[:, 896:912], lnG[:, 912:928]
            t16a, t16b, nre = lnG[:, 928:944], lnG[:, 944:960], lnG[:, 960:976]
            bbre = lnB[:, 0:256].rearrange("p (i j) -> p i j", i=16)
            bbim = lnB[:, 256:512].rearrange("p (i j) -> p i j", i=16)
            cTre = lnB[:, 512:768].rearrange("p (i j) -> p i j", i=16)
            cTim = lnB[:, 768:1024].rearrange("p (i j) -> p i j", i=16)
            Alv = [small[:, 128 * q:128 * (q + 1)].rearrange("p (l i) -> p l i", l=8) for q in range(3)]
            braw = [small[:, 384 + 256 * q:384 + 256 * (q + 1)].rearrange("p (i j) -> p i j", i=16) for q in range(2)]

            wv, wk = ws.acquire("w_in", (L,), 0, 1024, OFF_SSM, 512)
            for m in range(4):
                for tq in range(4):
                    pacc, kp = next_ps()
                    for kc in range(8):
                        mm(pacc[:], wv[:, kc, m * 128:(m + 1) * 128], xT[:, kc, tq * 512:(tq + 1) * 512],
                           kc == 0, kc == 7, [wk] + K("xT", range(tq * 4, tq * 4 + 4)), [kp])
                    cp(ussmP[:, m, :, tq * 64:(tq + 1) * 64], pacc[:].rearrange("p (c s) -> p s c", s=8), [kp],
                       [("ussmP", m, tq)], eng="act" if (m + tq) % 2 else "dve")
            ukeys = [("ussmP", m, tq) for m in range(4) for tq in range(4)]
            P.dma("sp", scrU.rearrange("p m s c -> p (m s c)"), ussmP.rearrange("p m s c -> p (m s c)"), reads=ukeys,
                  writes=["scrU"], semkey="scrU")

            NCK = dict(allow_slow_non_contiguous=True)
            for pi in range(2):
                pr = slice(pi * 64, pi * 64 + 64)
                P.dma("sp", lamre[pr, :], din["ssm_lam_re"][L].rearrange("(i two) p -> two p i", two=2)[pi],
                      writes=[("lamre", pi)], semkey="ssmw", **NCK)
                P.dma("sp", lamim[pr, :], din["ssm_lam_im"][L].rearrange("(i two) p -> two p i", two=2)[pi],
                      writes=[("lamim", pi)], semkey="ssmw", **NCK)
                P.dma("sp", dtv[pr, :], din["ssm_log_dt"][L].rearrange("(i two) -> two i", two=2)[pi].partition_broadcast(64),
                      writes=[("dtv", pi)], semkey="ssmw", **NCK)
                P.dma("sp", braw[0][pr, :, :], din["ssm_b_re"][L].rearrange("(i two) p j -> two p i j", two=2)[pi],
                      writes=[("braw0", pi)], semkey="ssmw")
                P.dma("sp", braw[1][pr, :, :], din["ssm_b_im"][L].rearrange("(i two) p j -> two p i j", two=2)[pi],
                      writes=[("braw1", pi)], semkey="ssmw")
            for s in range(8):
                P.dma("sp", dcol[s * 16:(s + 1) * 16, :], din["ssm_d"][L].rearrange("(g j) -> j g", j=16),
                      writes=[("dcol", s)], semkey="ssmw", **NCK)
            both = lambda nm: [(nm, 0), (nm, 1)]
            for ai, (cname, cT) in enumerate([("ssm_c_re", cTre), ("ssm_c_im", cTim)]):
                for m in range(4):
                    srcc = din[cname][L][8 * m:8 * m + 8].rearrange("g i p -> (g i) p")
                    P.dma("sp", A0dup[:, 0:64], srcc, writes=["A0a"], semkey="ssmc")
                    P.dma("sp", A0dup[:, 64:128], srcc, writes=["A0b"], semkey="ssmc")
                    pm, kpm = misc_ps()
                    transpose(pm[:, 0:128], A0dup, ident[:], ["A0a", "A0b"], [kpm])
                    for pi in range(2):
                        pr = slice(pi * 64, pi * 64 + 64)
                        cp(cT[pr, 4 * m:4 * m + 4, :],
                           pm[pr, 0:128].rearrange("p (pl two i) -> p pl two i", two=2, i=16)[:, :, pi, :], [kpm],
                           [("cT", ai, m, pi)])
            cTkeys = [("cT", ai, m, pi) for ai in range(2) for m in range(4) for pi in range(2)]
            if stage("ssmw0", L):
                dump(lnB[:, 512:1024], cTkeys, 512)
                dump(lnG[:, 832:880], both("lamre") + both("lamim") + both("dtv"), 48)
                dump(small[:, 384:896], both("braw0") + both("braw1"), 512)
                dump(lnG[:, 800:832], K("dcol", range(8)), 32)
                raise Stop()
            act(dtv, dtv, AF.Exp, both("dtv"), ["dtvx"])
            tt(zr, lamre, dtv, ALU.mult, both("lamre") + ["dtvx"], ["zr"])
            tt(zi, lamim, dtv, ALU.mult, both("lamim") + ["dtvx"], ["zi"])
            kv_bc = kvec[:].unsqueeze(1).broadcast_to([128, 16, 24])
            tt(tA, zr.unsqueeze(2).broadcast_to([128, 16, 24]), kv_bc, ALU.mult, ["zr", "kvec"], ["tA"])
            act(tA, tA, AF.Exp, ["tA"], ["tA"])
            ts(t16a, zi, float(1.0 / (2 * np.pi)), None, ALU.mult, None, ["zi"], ["t16a"])
            tt(tB, t16a.unsqueeze(2).broadcast_to([128, 16, 24]), kv_bc, ALU.mult, ["t16a", "kvec"], ["tB"])
            cp(tI, tB, ["tB"], ["tI"])
            cp(tC, tI, ["tI"], ["tC"])
            tt(tB, tB, tC, ALU.subtract, ["tB", "tC"], ["tB"])
            for thr, sgn, op in ((0.5, -1.0, ALU.is_gt), (-0.5, 1.0, ALU.is_lt)):
                ts(tC, tB, thr, sgn, op, ALU.mult, ["tB"], ["tC"])
                tt(tB, tB, tC, ALU.add, ["tB", "tC"], ["tB"])
            ts(tD, tB, 0.25, None, ALU.add, None, ["tB"], ["tD"])
            ts(tC, tD, 0.5, -1.0, ALU.is_gt, ALU.mult, ["tD"], ["tC"])
            tt(tD, tD, tC, ALU.add, ["tD", "tC"], ["tD"])
            act(tC, tB, AF.Sin, ["tB"], ["tC"], scale=float(2 * np.pi))
            act(tD, tD, AF.Sin, ["tD"], ["tD"], scale=float(2 * np.pi))
            tt(pwr, tA, tD, ALU.mult, ["tA", "tD"], ["pwr"])
            tt(pwi, tA, tC, ALU.mult, ["tA", "tC"], ["pwi"])
            pw = ["pwr", "pwi"]
            ar, aim = pwr[:, :, 16], pwi[:, :, 16]
            ts(nre, ar, -1.0, None, ALU.add, None, pw, ["nre"])
            tt(den, lamre, lamre, ALU.mult, both("lamre"), ["den"])
            tt(t16a, lamim, lamim, ALU.mult, both("lamim"), ["t16a"])
            tt(den, den, t16a, ALU.add, ["den", "t16a"], ["den"])
            recip(den, den, ["den"], ["den"])
            tt(cfre, nre, lamre, ALU.mult, ["nre"] + both("lamre"), ["cfre"])
            tt(t16a, aim, lamim, ALU.mult, pw + both("lamim"), ["t16a"])
            tt(cfre, cfre, t16a, ALU.add, ["cfre", "t16a"], ["cfre"])
            tt(cfre, cfre, den, ALU.mult, ["cfre", "den"], ["cfre"])
            tt(cfim, aim, lamre, ALU.mult, pw + both("lamre"), ["cfim"])
            tt(t16a, nre, lamim, ALU.mult, ["nre"] + both("lamim"), ["t16a"])
            tt(cfim, cfim, t16a, ALU.subtract, ["cfim", "t16a"], ["cfim"])
            tt(cfim, cfim, den, ALU.mult, ["cfim", "den"], ["cfim"])
            bc16 = lambda v: v.unsqueeze(2).broadcast_to([128, 16, 16])
            t256 = tA[:, :, 0:16]
            tt(bbre, braw[0], bc16(cfre), ALU.mult, both("braw0") + ["cfre"], ["bbre"])
            tt(t256, braw[1], bc16(cfim), ALU.mult, both("braw1") + ["cfim", "tA"], ["tA"])
            tt(bbre, bbre, t256, ALU.subtract, ["bbre", "tA"], ["bbre"])
            tt(bbim, braw[1], bc16(cfre), ALU.mult, both("braw1") + ["cfre"], ["bbim"])
            tt(t256, braw[0], bc16(cfim), ALU.mult, both("braw0") + ["cfim", "tA"], ["tA"])
            tt(bbim, bbim, t256, ALU.add, ["bbim", "tA"], ["bbim"])
            if stage("ssmw1", L):
                dump(lnG[:, 0:768], pw, 768)
                dump(lnB[:, 0:512], ["bbre", "bbim"], 512)
                raise Stop()
            cp(Alv[0][:, 0, :], pwr[:, :, 23], pw, ["Alv"])
            cp(Alv[1][:, 0, :], pwi[:, :, 23], pw + ["Alv"], ["Alv"])
            for l in range(7):
                tt(t16a, Alv[0][:, l, :], Alv[0][:, l, :], ALU.mult, ["Alv"], ["t16a"])
                tt(t16b, Alv[1][:, l, :], Alv[1][:, l, :], ALU.mult, ["Alv"], ["t16b"])
                tt(Alv[0][:, l + 1, :], t16a, t16b, ALU.subtract, ["t16a", "t16b", "Alv"], ["Alv"])
                stt(Alv[1][:, l + 1, :], Alv[0][:, l, :], 2.0, Alv[1][:, l, :], ALU.mult, ALU.mult, ["Alv"], ["Alv"])
            ts(Alv[2], Alv[1], -1.0, None, ALU.mult, None, ["Alv"], ["Alv"])

            def v4(buf):
                return buf.rearrange("p (i a b) -> p i a b", i=8, a=8)

            def v3(buf):
                return buf.rearrange("p (i n) -> p i n", i=8)

            for hf in range(2):
                prs = slice(8 * hf, 8 * hf + 8)

                def bcj(v):
                    return v[:, prs, :].unsqueeze(2).broadcast_to([128, 8, 8, 16])

                def bck(v, k0):
                    return v[:, prs, k0:k0 + 8].unsqueeze(3).broadcast_to([128, 8, 8, 16])

                XY = ["Xre", "Ximn", "Yre", "Yim"]
                tt(v4(Xre), bcj(bbre), bck(pwr, 0), ALU.mult, ["bbre"] + pw, ["Xre"])
                tt(v4(T5), bcj(bbim), bck(pwi, 0), ALU.mult, ["bbim"] + pw, ["T5"])
                tt(Xre, Xre, T5, ALU.subtract, ["Xre", "T5"], ["Xre"])
                tt(v4(Ximn), bcj(bbre), bck(pwi, 0), ALU.mult, ["bbre"] + pw, ["Ximn"])
                tt(v4(T5), bcj(bbim), bck(pwr, 0), ALU.mult, ["bbim"] + pw, ["T5"])
                stt(Ximn, Ximn, -1.0, T5, ALU.mult, ALU.subtract, ["Ximn", "T5"], ["Ximn"])
                tt(v4(Yre), bcj(cTre), bck(pwr, 8), ALU.mult, cTkeys + pw, ["Yre"])
                tt(v4(T5), bcj(cTim), bck(pwi, 8), ALU.mult, cTkeys + pw, ["T5"])
                tt(Yre, Yre, T5, ALU.subtract, ["Yre", "T5"], ["Yre"])
                tt(v4(Yim), bcj(cTre), bck(pwi, 8), ALU.mult, cTkeys + pw, ["Yim"])
                tt(v4(T5), bcj(cTim), bck(pwr, 8), ALU.mult, cTkeys + pw, ["T5"])
                tt(Yim, Yim, T5, ALU.add, ["Yim", "T5"], ["Yim"])
                if stage("ssmw2", L):
                    dump(Xre, ["Xre"], 1024)
                    dump(Ximn, ["Ximn"], 1024)
                    dump(Yre, ["Yre"], 1024)
                    dump(Yim, ["Yim"], 1024)
                    raise Stop()
                for i in range(8):
                    for pi in range(2):
                        pr = slice(pi * 64, pi * 64 + 64)
                        gl = 2 * i + pi
                        g = 16 * hf + gl
                        pm, kpm = misc_ps()
                        mm(pm[:, 0:128], v3(Xre)[pr, i, :], v3(Yre)[pr, i, :], True, False, ["Xre", "Yre"], [kpm])
                        mm(pm[:, 0:128], v3(Ximn)[pr, i, :], v3(Yim)[pr, i, :], False, True, ["Ximn", "Yim"], [kpm])
                        tt(T5[:, 0:128], pm[:, 0:128], m01ssm[:], ALU.mult, [kpm, "m01ssm"], ["T5"])
                        stt(Gm[:, gl, :], ident[:], dcol[:, g:g + 1], T5[:, 0:128], ALU.mult, ALU.add,
                            ["ident", "T5"] + K("dcol", range(8)), [("Gm", gl)])
                if stage("ssmw3", L):
                    dump(Gm.rearrange("p g n -> p (g n)"), K("Gm", range(16)), 2048)
                    raise Stop()
                a7r = pwr[:, prs, 15:16].broadcast_to([128, 8, 128])
                a7i = pwi[:, prs, 15:16].broadcast_to([128, 8, 128])
                tt(v3(Yre), v3(Xre), a7r, ALU.mult, ["Xre"] + pw, ["Yre"])
                tt(v3(T5), v3(Ximn), a7i, ALU.mult, ["Ximn"] + pw, ["T5"])
                tt(Yre, Yre, T5, ALU.add, ["Yre", "T5"], ["Yre"])
                tt(v3(Yim), v3(Xre), a7i, ALU.mult, ["Xre"] + pw, ["Yim"])
                tt(v3(T5), v3(Ximn), a7r, ALU.mult, ["Ximn"] + pw, ["T5"])
                tt(Yim, Yim, T5, ALU.subtract, ["Yim", "T5"], ["Yim"])
                for i in range(8):
                    for pi in range(2):
                        pr = slice(pi * 64, pi * 64 + 64)
                        gl = 2 * i + pi
                        pm, kpm = misc_ps()
                        transpose(pm[:, 0:64], v3(Yre)[pr, i, :], ident[pr, pr], ["Yre"], [kpm])
                        transpose(pm[:, 64:128], v3(Yim)[pr, i, :], ident[pr, pr], ["Yim"], [kpm])
                        cp(Ere[:, gl, :], pm[:, 0:64], [kpm], [("Ere", gl)])
                        cp(Eim[:, gl, :], pm[:, 64:128], [kpm], [("Eim", gl)])
                if stage("ssmw4", L):
                    dump(Gm.rearrange("p g n -> p (g n)"), K("Gm", range(16)), 2048)
                    dump(Ere.rearrange("p g n -> p (g n)"), K("Ere", range(16)), 1024)
                    dump(Eim.rearrange("p g n -> p (g n)"), K("Eim", range(16)), 1024)
                    raise Stop()
                tt(v4(Xre), bcj(cTre), bck(pwr, 16), ALU.mult, cTkeys + pw, ["Xre"])
                tt(v4(Ximn), bcj(cTim), bck(pwi, 16), ALU.mult, cTkeys + pw, ["Ximn"])
                tt(Ire.rearrange("p g n -> p (g n)"), Xre, Ximn, ALU.subtract, ["Xre", "Ximn"], ["Ire"])
                tt(v4(Xre), bcj(cTre), bck(pwi, 16), ALU.mult, cTkeys + pw, ["Xre"])
                tt(v4(Ximn), bcj(cTim), bck(pwr, 16), ALU.mult, cTkeys + pw, ["Ximn"])
                stt(Iimn.rearrange("p g n -> p (g n)"), Xre, -1.0, Ximn, ALU.mult, ALU.subtract, ["Xre", "Ximn"],
                    ["Iimn"])
                if stage("ssmw", L) and hf == 0:
                    dump(Gm.rearrange("p g n -> p (g n)"), K("Gm", range(16)), 2048)
                    dump(Ere.rearrange("p g n -> p (g n)"), K("Ere", range(16)), 1024)
                    dump(Eim.rearrange("p g n -> p (g n)"), K("Eim", range(16)), 1024)
                    dump(Ire.rearrange("p g n -> p (g n)"), ["Ire"], 1024)
                    dump(Iimn.rearrange("p g n -> p (g n)"), ["Iimn"], 1024)
                    raise Stop()
                for s in range(8):
                    for m2 in range(2):
                        srcu = scrU.rearrange("(gg j) m s c -> j s m gg c", j=16)[:, s, 2 * hf + m2, :, :]
                        P.dma("sp", uW[s * 16:(s + 1) * 16, m2 * 8:(m2 + 1) * 8, :], srcu,
                              reads=["scrU"], writes=[("uW", s, m2)], semkey=("uW", hf))
                uWk = [("uW", s, m2) for s in range(8) for m2 in range(2)]

                def do_E(bt):
                    pe_ = [psb[2 + 2 * (bt % 2)], psb[3 + 2 * (bt % 2)]]
                    ke_ = [("ps", 2 + 2 * (bt % 2)), ("ps", 3 + 2 * (bt % 2))]
                    for pl in range(2):
                        for pi in range(2):
                            gl = 4 * bt + 2 * pl + pi
                            for ri, Em in enumerate((Ere, Eim)):
                                mm(pe_[ri][pi * 64:(pi + 1) * 64, pl * 256:(pl + 1) * 256], Em[:, gl, :], uW[:, gl, :],
                                   True, True, [("Ere" if ri == 0 else "Eim", gl), ("yW", gl)] + uWk, [ke_[ri]])
                    return pe_, ke_

                def do_scan(bt, pe_, ke_):
                    cp(Sb[0][0].rearrange("p i c -> p (i c)"), pe_[0][:], [ke_[0]], ["S00"], eng="act")
                    cp(Sb[0][1].rearrange("p i c -> p (i c)"), pe_[1][:], [ke_[1]], ["S01"])
                    for l in range(8):
                        d = 1 << l
                        cur, nxt = Sb[l % 2], Sb[(l + 1) % 2]
                        kc_ = [f"S{l % 2}0", f"S{l % 2}1"]
                        kn_ = [f"S{(l + 1) % 2}0", f"S{(l + 1) % 2}1"]
                        cp(nxt[0][:, :, 0:d], cur[0][:, :, 0:d], [kc_[0]], [kn_[0]])
                        cp(nxt[1][:, :, 0:d], cur[1][:, :, 0:d], [kc_[1]], [kn_[1]])
                        for pl in range(2):
                            pg = 8 * hf + 2 * bt + pl
                            sar = Alv[0][:, l, pg:pg + 1]
                            sai = Alv[1][:, l, pg:pg + 1]
                            sni = Alv[2][:, l, pg:pg + 1]
                            n = 256 - d
                            stt(nxt[0][:, pl, d:], cur[0][:, pl, 0:n], sar, cur[0][:, pl, d:], ALU.mult, ALU.add,
                                [kc_[0], "Alv"], [kn_[0]])
                            stt(nxt[0][:, pl, d:], cur[1][:, pl, 0:n], sni, nxt[0][:, pl, d:], ALU.mult, ALU.add,
                                [kc_[1], kn_[0], "Alv"], [kn_[0]])
                            stt(nxt[1][:, pl, d:], cur[1][:, pl, 0:n], sar, cur[1][:, pl, d:], ALU.mult, ALU.add,
                                [kc_[1], "Alv"], [kn_[1]])
                            stt(nxt[1][:, pl, d:], cur[0][:, pl, 0:n], sai, nxt[1][:, pl, d:], ALU.mult, ALU.add,
                                [kc_[0], kn_[1], "Alv"], [kn_[1]])
                    for ri in range(2):
                        memset(Sprev[ri][:, :, 0:1], 0.0, [("Sprev", ri)])
                        cp(Sprev[ri][:, :, 1:256], Sb[0][ri][:, :, 0:255], [f"S0{ri}", ("Sprev", ri)], [("Sprev", ri)],
                           eng="act" if ri else "dve")

                def do_GI(bt):
                    for pl in range(2):
                        for pi in range(2):
                            pr = slice(pi * 64, pi * 64 + 64)
                            gl = 4 * bt + 2 * pl + pi
                            il = 2 * bt + pl
                            py, kpy = next_ps()
                            mm(py[:, 0:256], Gm[:, gl, :], uW[:, gl, :], True, False, [("Gm", gl), ("yW", gl)] + uWk, [kpy])
                            mm(py[:, 0:256], Ire[pr, il, :], Sprev[0][pr, pl, :], False, False, ["Ire", ("Sprev", 0)], [kpy])
                            mm(py[:, 0:256], Iimn[pr, il, :], Sprev[1][pr, pl, :], False, True, ["Iimn", ("Sprev", 1)], [kpy])
                            cp(uW[:, gl, :], py[:, 0:256], [kpy] + uWk, [("yW", gl)], eng="act" if gl % 2 else "dve")

                e_cur = do_E(0)
                for bt in range(4):
                    do_scan(bt, *e_cur)
                    if bt < 3:
                        e_cur = do_E(bt + 1)
                    do_GI(bt)
                P.dma("sp", scrY[:, 16 * hf:16 * hf + 16, :], uW[:, :, :], reads=K("yW", range(16)) + uWk,
                      writes=[("scrY", hf)], semkey="scrY")
            P.alias([("ycP", gg, m) for gg in range(8) for m in range(4)], ["tA", "tB", "tC", "tD", "tI", "A0a", "A0b", "T5"])
            for gg in range(8):
                for m in range(4):
                    srcy = scrY.rearrange("(t i) (m gg) c -> i gg m t c", i=16, gg=8)[:, gg, m]
                    P.dma("sp", ycP[gg * 16:(gg + 1) * 16, m, :, :], srcy, reads=[("scrY", 0), ("scrY", 1)],
                          writes=[("ycP", gg, m)], semkey="ycP")
            ycPk = [("ycP", gg, m) for gg in range(8) for m in range(4)]
            if stage("ssm", L):
                dump(ycP.rearrange("p m s c -> p (m s c)"), ycPk, 8192)
                raise Stop()
            ycU = C[:, 0:8192].rearrange("p (m t) -> p m t", m=4)
            P.alias([("ycU", m) for m in range(4)],
                    K("Gm", range(16)) + K("Ere", range(16)) + K("Eim", range(16)) + ["Ire", "Iimn"]
                    + K("yW", range(16)) + [("uW", s_, m2) for s_ in range(8) for m2 in range(2)])
            for m in range(4):
                act(ycU[:, m, :].rearrange("p (c s) -> p c s", s=8), ycP[:, m].rearrange("p s c -> p c s"),
                    AF.Gelu_apprx_tanh, ycPk, [("ycU", m)])
            ycUk = K("ycU", range(4))
            if stage("glu0", L):
                dump(ycU.rearrange("p m t -> p (m t)"), ycUk, 8192)
                raise Stop()
            sigb = ABf32(24576, 512)
            wv, wk = ws.acquire("ssm_w_glu", (L,), 0, 512, 0, 512)
            P.alias([("ycT", n4, tq) for n4 in range(4) for tq in range(4)], ukeys)
            for n4 in range(4):
                for tq in range(4):
                    pacc, kp = next_ps()
                    for kc in range(4):
                        mm(pacc[:], wv[:, kc, n4 * 128:(n4 + 1) * 128], ycU[:, kc, tq * 512:(tq + 1) * 512],
                           kc == 0, kc == 3, [wk] + ycUk, [kp])
                    act(sigb, pacc[:], AF.Sigmoid, [kp, "vecT"], ["sigb"], bias=vecT[:, 24 + n4:25 + n4])
                    tt(ycT[:, n4, tq * 512:(tq + 1) * 512], ycU[:, n4, tq * 512:(tq + 1) * 512], sigb, ALU.mult,
                       ycUk + ["sigb"], [("ycT", n4, tq)])
            yckeys = [("ycT", n4, tq) for n4 in range(4) for tq in range(4)]
            if stage("glu", L):
                dump(ycT.rearrange("p c t -> p (c t)"), yckeys, 8192)
                raise Stop()
            P.barrier()
            mergedT = C[:, 0:16384].rearrange("p (n t) -> p n t", n=8)
            pwt = [AB[:, 24576:28672].rearrange("p (k n) -> p k n", k=4),
                   AB[:, 28672:32768].rearrange("p (k n) -> p k n", k=4),
                   C[:, 16384:20480].rearrange("p (k n) -> p k n", k=4)]
            acc = Cf32(20480, 512)
            tmpm = Cf32(21504, 512)
            sig = [C[:, 22528 + 512 * b:22528 + 512 * (b + 1)] for b in range(3)]
            for b, nm in enumerate(["p_attn", "p_sgu", "p_ssm"]):
                P.dma("pool", pwt[b], din[nm][L].rearrange("(k p) n -> p k n", p=128), writes=[("pwt", b)],
                      semkey=("pwt", b))
            ysrc = [(yaT, lambda tq: K("yaT", range(tq * 4, tq * 4 + 4))),
                    (ybT, lambda tq: [("ybT", m, tq) for m in range(4)]),
                    (ycT, lambda tq: [("ycT", m, tq) for m in range(4)])]
            ppc = 0
            for ng in range(2):
                gws = [ws.acquire("w_in", (L,), 0, 1024, OFF_GATE + b * 1024 + ng * 512, 512) for b in range(3)]
                for tq in range(4):
                    for n4 in range(4):
                        n = ng * 4 + n4
                        for b in range(3):
                            gw, gk = gws[b]
                            pg, kpg = next_ps()
                            for kc in range(8):
                                mm(pg[:], gw[:, kc, n4 * 128:(n4 + 1) * 128], xT[:, kc, tq * 512:(tq + 1) * 512],
                                   kc == 0, kc == 7, [gk] + K("xT", range(tq * 4, tq * 4 + 4)), [kpg])
                            act(sig[b], pg[:], AF.Sigmoid, [kpg, "vecT"], [("sig", b)], bias=vecT[:, b * 8 + n:b * 8 + n + 1])
                            pp, kpp = psb[2 + ppc % 2], ("ps", 2 + ppc % 2)
                            ppc += 1
                            ysb, ykf = ysrc[b]
                            for kc in range(4):
                                mm(pp[:], pwt[b][:, kc, n * 128:(n + 1) * 128], ysb[:, kc, tq * 512:(tq + 1) * 512],
                                   kc == 0, kc == 3, [("pwt", b)] + ykf(tq), [kpp])
                            if b == 0:
                                tt(acc, pp[:], sig[0], ALU.mult, [("sig", 0), kpp], ["acc"])
                            elif b == 1:
                                tt(tmpm, pp[:], sig[1], ALU.mult, [("sig", 1), kpp], ["tmpm"])
                                tt(acc, acc, tmpm, ALU.add, ["acc", "tmpm"], ["acc"])
                            else:
                                tt(tmpm, pp[:], sig[2], ALU.mult, [("sig", 2), kpp], ["tmpm"])
                                tt(mergedT[:, n, tq * 512:(tq + 1) * 512], acc, tmpm, ALU.add, ["acc", "tmpm"],
                                   [("mergedT", n, tq)])
            if stage("merge", L):
                dump(mergedT[:, 0:4, :].rearrange("p n t -> p (n t)"), [("mergedT", n, tq) for n in range(4) for tq in range(4)], 8192)
                dump(mergedT[:, 4:8, :].rearrange("p n t -> p (n t)"), [("mergedT", n, tq) for n in range(4, 8) for tq in range(4)], 8192)
                raise Stop()

            P.barrier()
            def ln_bufs(tt_):
                o = 16 * (tt_ % 2)
                return small[:, o:o + 12], small[:, o + 12:o + 14], small[:, o + 14:o + 15], tt_ % 2

            def ln_A(tt_, kx):
                stats, mv, rstd, par = ln_bufs(tt_)
                xr = xres[:, tt_, :]
                P.op("dve", lambda e: e.bn_stats(out=stats[:, 0:6], in_=xr[:, 0:512]), reads=[kx], writes=[("stats", par)])
                P.op("dve", lambda e: e.bn_stats(out=stats[:, 6:12], in_=xr[:, 512:1024]), reads=[kx, ("stats", par)],
                     writes=[("stats", par)])
                P.op("dve", lambda e: e.bn_aggr(out=mv, in_=stats), reads=[("stats", par)], writes=[("mv", par)])
                ts(rstd, mv[:, 1:2], LN_EPS, None, ALU.add, None, [("mv", par)], [("rstd", par)])
                act(rstd, rstd, AF.Sqrt, [("rstd", par)], [("rstd", par)])

            def ln_B(tt_, kx):
                stats, mv, rstd, par = ln_bufs(tt_)
                xr = xres[:, tt_, :]
                recip(rstd, rstd, [("rstd", par)], [("rstd", par)])
                stt(xr, xr, mv[:, 0:1], lnG[:], ALU.subtract, ALU.mult, [kx, ("mv", par), "lnG"], [kx])
                stt(xr, xr, rstd, lnB[:], ALU.mult, ALU.add, [kx, ("rstd", par), "lnB"], [kx])

            moe = (L % 2 == 1)
            router_hook = None
            if moe:
                rw = small[:, 64:128].rearrange("p (k e) -> p k e", k=8)
                logits = small[:, 128:256].rearrange("p (t e) -> p t e", t=16)
                wexp = small[:, 256:384].rearrange("p (t e) -> p t e", t=16)
                t1 = small[:, 384:512].rearrange("p (t e) -> p t e", t=16)
                t2 = small[:, 512:640].rearrange("p (t e) -> p t e", t=16)
                t3 = small[:, 640:768].rearrange("p (t e) -> p t e", t=16)
                m1, m2 = small[:, 768:784], small[:, 784:800]
                rbias = small[:, 800:808]
                xTf = Cf32(22528, 512)
                P.dma("sp", rw, din["moe_router"][0].rearrange("(k p) e -> p k e", p=128), writes=["rw"], semkey="rt")
                P.dma("sp", rbias, din["moe_router_bias"][0].partition_broadcast(128), writes=["rbias"], semkey="rt")
                plog, kplog = psb[5], ("ps", 5)

                def router_hook(tt_, h2, pm, kpm):
                    cp(xTf, pm[:], [kpm], ["xTf"])
                    for c4 in range(4):
                        mm(plog[:, 0:8], xTf[:, c4 * 128:(c4 + 1) * 128], rw[:, h2 * 4 + c4, :], h2 == 0 and c4 == 0,
                           h2 == 1 and c4 == 3, ["xTf", "rw"], [kplog])
                    if h2 == 1:
                        tt(logits[:, tt_, :], plog[:, 0:8], rbias, ALU.add, [kplog, "rbias"], [("logits", tt_)])

            P.dma("sp", lnG[:], din["ln1_g"][L].partition_broadcast(128), writes=["lnG"], semkey="lnp")
            P.dma("sp", lnB[:], din["ln1_b"][L].partition_broadcast(128), writes=["lnB"], semkey="lnp")
            wo = [ws.acquire("w_out", (L,), 0, 1024, hh * 512, 512) for hh in range(2)]
            mkeys = lambda: [("mergedT", n, tq_) for n in range(8) for tq_ in range(4)]
            def ln1_pre(tt_):
                xb = xin[tt_ % 2]
                kx = ("xin", tt_ % 2)
                if L == 0:
                    P.dma("sp", xb[:], din["x"][tt_ * 128:(tt_ + 1) * 128, :], writes=[kx])
                else:
                    P.dma("sp", xb[:], xs_ap[tt_ * 128:(tt_ + 1) * 128, :], reads=[("xs", tt_)], writes=[kx])
                kxr = ("xres", tt_)
                for hh in range(2):
                    pacc, kp = next_ps()
                    for kc in range(8):
                        mm(pacc[:], mergedT[:, kc, tt_ * 128:(tt_ + 1) * 128], wo[hh][0][:, kc, :], kc == 0, kc == 7,
                           [wo[hh][1]] + [("mergedT", kc, tt_ // 4)], [kp])
                    stt(xres[:, tt_, hh * 512:(hh + 1) * 512], xb[:, hh * 512:(hh + 1) * 512], DN_ALPHA, pacc[:],
                        ALU.mult, ALU.add, [kx, kp], [kxr])
                ln_A(tt_, kxr)

            ln1_pre(0)
            for tt_ in range(16):
                if tt_ + 1 < 16:
                    ln1_pre(tt_ + 1)
                ln_B(tt_, ("xres", tt_))
                tiles_to_xT(tt_, xres[:, tt_, :], [("xres", tt_)], router=router_hook)
            if stage("ln1", L):
                for tt_ in range(16):
                    P.dma("sp", dbg_ap[:, tt_ * 1024:(tt_ + 1) * 1024], xres[:, tt_, :], reads=[("xres", tt_)],
                          writes=[("dbgo", tt_)], semkey="dbg")
                raise Stop()

            P.barrier()
            hT = C[:, 0:8192].rearrange("p (f t) -> p f t", f=4)
            sg = Cf32(8192, 512)
            puc = [0]

            stg = [Cf32(9216 + 4096 * i, 2048) for i in range(3)]
            fblocks = []

            class FS:
                def __init__(self):
                    self.n = 0
                    self.nd = 0
                    self.ncast = 0

                def _half(self, j):
                    b, hf = j // 2, j % 2
                    name, idx, r0, nr, c0, ncw = fblocks[b]
                    src = din[name]
                    for i in idx:
                        src = src[i]
                    s_ = b % NS
                    if nr == 1024:
                        srcv = src[r0 + hf * 512:r0 + (hf + 1) * 512, c0:c0 + ncw].rearrange("(kc p) n -> p kc n", p=128)
                        sv = stg[j % 3][:, 0:4 * ncw].rearrange("p (kc n) -> p kc n", kc=4)
                        dv = ring[s_][:, 0:8 * ncw].rearrange("p (kc n) -> p kc n", kc=8)[:, hf * 4:(hf + 1) * 4, :]
                    else:
                        kc = nr // 128
                        srcv = src[r0:r0 + nr, c0 + hf * 512:c0 + (hf + 1) * 512].rearrange("(kc p) n -> p kc n", p=128)
                        sv = stg[j % 3][:, 0:kc * 512].rearrange("p (kc n) -> p kc n", kc=kc)
                        dv = ring[s_][:, 0:kc * 1024].rearrange("p (kc n) -> p kc n", kc=kc)[:, :, hf * 512:(hf + 1) * 512]
                    return srcv, sv, dv, s_

                def ensure_dma(self, upto):
                    while self.nd <= min(upto, 2 * len(fblocks) - 1):
                        j = self.nd
                        srcv, sv, dv, s_ = self._half(j)
                        P.dma("sp", sv, srcv, writes=[("stg", j % 3)])
                        self.nd += 1

                def ensure_cast(self, upto):
                    while self.ncast <= min(upto, len(fblocks) - 1):
                        b = self.ncast
                        for hf in range(2):
                            j = 2 * b + hf
                            self.ensure_dma(j)
                            srcv, sv, dv, s_ = self._half(j)
                            cp(dv, sv, [("stg", j % 3)], [("ring", s_)], eng="act")
                        self.ncast += 1

                def next(self):
                    b = self.n
                    self.n += 1
                    self.ensure_cast(b)
                    self.ensure_dma(2 * b + 3)
                    self.ensure_cast(b + 1)
                    self.ensure_dma(2 * b + 5)
                    name, idx, r0, nr, c0, ncw = fblocks[b]
                    kc = nr // 128
                    view = ring[b % NS][:, 0:kc * ncw].rearrange("p (kc n) -> p kc n", kc=kc)
                    return view, ("ring", b % NS)

            fs = FS()

            def ffn_group(gname, uname, dname, idx, f0, fw, first_scale, wcol):
                fc = fw // 128
                wg, kg = fs.next()
                wu, ku = fs.next()
                wd, kd = fs.next()
                for fcl in range(fc):
                    for tq in range(4):
                        pg, kpg = next_ps()
                        for kc in range(8):
                            mm(pg[:], wg[:, kc, fcl * 128:(fcl + 1) * 128], xT[:, kc, tq * 512:(tq + 1) * 512],
                               kc == 0, kc == 7, [kg] + K("xT", range(tq * 4, tq * 4 + 4)), [kpg])
                        pu, kpu = psb[2 + puc[0] % 2], ("ps", 2 + puc[0] % 2)
                        puc[0] += 1
                        for kc in range(8):
                            mm(pu[:], wu[:, kc, fcl * 128:(fcl + 1) * 128], xT[:, kc, tq * 512:(tq + 1) * 512],
                               kc == 0, kc == 7, [ku] + K("xT", range(tq * 4, tq * 4 + 4)), [kpu])
                        act(sg, pg[:], AF.Silu, [kpg], ["sg"])
                        tt(hT[:, fcl, tq * 512:(tq + 1) * 512], pu[:], sg, ALU.mult, ["sg", kpu], [("hT", fcl, tq)])
                for tt_ in range(16):
                    kxr = ("xres", tt_)
                    for hh in range(2):
                        pd, kpd = next_ps()
                        for fcl in range(fc):
                            mm(pd[:], hT[:, fcl, tt_ * 128:(tt_ + 1) * 128], wd[:, fcl, hh * 512:(hh + 1) * 512],
                               fcl == 0, fcl == fc - 1, [kd, ("hT", fcl, tt_ // 4)], [kpd])
                        xr = xres[:, tt_, hh * 512:(hh + 1) * 512]
                        if wcol is not None:
                            stt(xr, pd[:], wcol(tt_), xr, ALU.mult, ALU.add, [kpd, kxr, "wexp"], [kxr])
                        elif first_scale:
                            stt(xr, xr, DN_ALPHA, pd[:], ALU.mult, ALU.add, [kpd, kxr], [kxr])
                        else:
                            tt(xr, pd[:], xr, ALU.add, [kpd, kxr], [kxr])

            def add_blocks(gname, uname, dname, idx, f0, fw):
                fblocks.extend([(gname, idx, 0, 1024, f0, fw), (uname, idx, 0, 1024, f0, fw), (dname, idx, f0, fw, 0, 1024)])

            if not moe:
                f0 = 0
                while f0 < F_DENSE:
                    fw = min(512, F_DENSE - f0)
                    add_blocks("ffn_w_gate", "ffn_w_up", "ffn_w_down", (L // 2,), f0, fw)
                    f0 += fw
            else:
                for e_ in range(NEXP):
                    for gi in range(F_EXP // 512):
                        add_blocks("moe_w_gate", "moe_w_up", "moe_w_down", (L // 2, 0 if MOE_FAKE else e_),
                                   0 if MOE_FAKE else gi * 512, 512)
            if not moe:
                i_ = L // 2
                f0 = 0
                while f0 < F_DENSE:
                    fw = min(512, F_DENSE - f0)
                    ffn_group("ffn_w_gate", "ffn_w_up", "ffn_w_down", (i_,), f0, fw, f0 == 0, None)
                    f0 += fw
            else:
                i_ = L // 2
                lk = K("logits", range(16))
                red(m1, logits, lk, ["m1"], op=ALU.max)
                tt(t1, logits, m1.unsqueeze(2).broadcast_to([128, 16, 8]), ALU.is_equal, lk + ["m1"], ["t1"])
                stt(t2, t1, NEG, logits, ALU.mult, ALU.add, ["t1"] + lk, ["t2"])
                red(m2, t2, ["t2"], ["m2"], op=ALU.max)
                tt(t3, t2, m2.unsqueeze(2).broadcast_to([128, 16, 8]), ALU.is_equal, ["t2", "m2"], ["t3"])
                tt(m1, m1, m2, ALU.subtract, ["m1", "m2"], ["m1"])
                act(m1, m1, AF.Sigmoid, ["m1"], ["m1"])
                ts(m2, m1, -1.0, 1.0, ALU.mult, ALU.add, ["m1"], ["m2"])
                tt(t1, t1, m1.unsqueeze(2).broadcast_to([128, 16, 8]), ALU.mult, ["t1", "m1"], ["t1"])
                tt(t3, t3, m2.unsqueeze(2).broadcast_to([128, 16, 8]), ALU.mult, ["t3", "m2"], ["t3"])
                tt(wexp, t1, t3, ALU.add, ["t1", "t3"], ["wexp"])
                for tt_ in range(16):
                    ts(xres[:, tt_, :], xres[:, tt_, :], DN_ALPHA, None, ALU.mult, None, [("xres", tt_)], [("xres", tt_)])
                for e_ in range(NEXP):
                    for gi in range(F_EXP // 512):
                        ffn_group("moe_w_gate", "moe_w_up", "moe_w_down", (i_, 0 if MOE_FAKE else e_),
                                  0 if MOE_FAKE else gi * 512, 512, False,
                                  (lambda e__: (lambda tt_: wexp[:, tt_, e__:e__ + 1]))(e_))
            if stage("ffn", L):
                for tt_ in range(16):
                    P.dma("sp", dbg_ap[:, tt_ * 1024:(tt_ + 1) * 1024], xres[:, tt_, :], reads=[("xres", tt_)],
                          writes=[("dbgo", tt_)], semkey="dbg")
                raise Stop()

            P.dma("sp", lnG[:], din["ln2_g"][L].partition_broadcast(128), writes=["lnG"], semkey="lnp")
            P.dma("sp", lnB[:], din["ln2_b"][L].partition_broadcast(128), writes=["lnB"], semkey="lnp")
            ln_A(0, ("xres", 0))
            for tt_ in range(16):
                kxr = ("xres", tt_)
                if tt_ + 1 < 16:
                    ln_A(tt_ + 1, ("xres", tt_ + 1))
                ln_B(tt_, kxr)
                if L == nlayers - 1:
                    P.dma("sp", out_ap[tt_ * 128:(tt_ + 1) * 128, :], xres[:, tt_, :], reads=[kxr], writes=[("out", tt_)],
                          semkey="out")
                else:
                    P.dma("sp", xs_ap[tt_ * 128:(tt_ + 1) * 128, :], xres[:, tt_, :], reads=[kxr], writes=[("xs", tt_)],
                          semkey="xs")
                    tiles_to_xT(tt_, xres[:, tt_, :], [kxr])
    except Stop:
        pass
    P.emit()
    return nc, P


_CACHE = {}


def kernel(**inputs):
    n = 8
    if "prog" not in _CACHE:
        _CACHE["prog"] = build_program()[0]
    nc = _CACHE["prog"]
    consts = host_consts()
    x = np.ascontiguousarray(inputs["x"], dtype=np.float32)
    shared = {k: np.ascontiguousarray(inputs[k], dtype=np.float32) for k in W_SHAPES}
    shared.update(consts)
    in_maps = []
    for c in range(n):
        m = dict(shared)
        m["x"] = np.ascontiguousarray(x[c])
        in_maps.append(m)
    res = run_bass_kernel_spmd(nc, in_maps, core_ids=list(range(n)))
    return np.stack([np.asarray(r["out"], dtype=np.float32) for r in res.results], axis=0)
```

```python
import contextlib
import numpy as np
import concourse.bass as bass
import concourse.mybir as mybir
from concourse.bass_utils import run_bass_kernel_spmd

F32 = mybir.dt.float32
BF16 = mybir.dt.bfloat16
I32 = mybir.dt.int32
ALU = mybir.AluOpType
AF = mybir.ActivationFunctionType
AX = mybir.AxisListType

ENGS = ("pe", "act", "dve", "pool", "sp")

T = 2048
D = 1024
NL = 2
DN_ALPHA = (2.0 * NL) ** 0.25
LN_EPS = 1e-5
NEG = -30000.0
OFF_SGU = 1536
OFF_SSM = 2560
OFF_GATE = 3072
F_DENSE = 2816
F_EXP = 3584
NEXP = 8
MOE_FAKE = False
RING_NS = 4


class Op:
    __slots__ = ("eng", "fn", "deps", "dma", "semkey", "sig", "cnt")

    def __init__(self, eng, fn, deps, dma, semkey):
        self.eng = eng
        self.fn = fn
        self.deps = deps
        self.dma = dma
        self.semkey = semkey
        self.sig = False
        self.cnt = 0


class Prog:
    def __init__(self, nc):
        self.nc = nc
        self.ops = []
        self.state = {}
        self.dma_counts = {}
        self.stack = contextlib.ExitStack()
        self._nm = 0
        self.epoch = 0
        self.barrier_ops = []
        self.last_eng = {}
        self.last_dma = {}

    def sb(self, shape, dtype, name=None):
        self._nm += 1
        return self.stack.enter_context(self.nc.sbuf_tensor(name or f"sb{self._nm}", list(shape), dtype))

    def ps(self, shape, dtype=F32, name=None):
        self._nm += 1
        return self.stack.enter_context(self.nc.psum_tensor(name or f"ps{self._nm}", list(shape), dtype))

    def barrier(self):
        self.epoch += 1
        self.barrier_ops = list(self.last_eng.values()) + list(self.last_dma.values())

    def _deps(self, reads, writes):
        deps = []
        seen = set()

        def add(o):
            if o is not None and id(o) not in seen:
                seen.add(id(o))
                deps.append(o)
        for r in reads:
            st = self.state.get(r)
            if st is not None:
                add(st[0])
        for w in writes:
            st = self.state.get(w)
            if st is not None:
                add(st[0])
                lastr = {}
                for o in st[1]:
                    lastr[(o.eng, o.semkey if o.dma else None)] = o
                for o in lastr.values():
                    add(o)
            exempt = isinstance(w, tuple) and w[0] == "ring"
            if not exempt and (st is None or st[2] < self.epoch):
                for o in self.barrier_ops:
                    add(o)
        return deps

    def _update(self, o, reads, writes):
        for r in reads:
            st = self.state.setdefault(r, [None, [], self.epoch])
            st[1].append(o)
        for w in writes:
            self.state[w] = [o, [], self.epoch]

    def op(self, eng, fn, reads=(), writes=(), dma=False, semkey=None, slot=None):
        o = Op(eng, fn, self._deps(reads, writes), dma, semkey)
        if dma:
            self.dma_counts[semkey] = self.dma_counts.get(semkey, 0) + 1
            if slot is None:
                self.last_dma[semkey] = o
        else:
            self.last_eng[eng] = o
        self._update(o, reads, writes)
        if slot is None:
            self.ops.append(o)
        else:
            slot.append(o)
        return o

    def placeholder(self):
        ph = []
        self.ops.append(ph)
        return ph

    def alias(self, new_keys, old_keys):
        olds = []
        for k in old_keys:
            st = self.state.get(k)
            if st is not None:
                if st[0] is not None:
                    olds.append(st[0])
                olds.extend(st[1])
        for k in new_keys:
            st = self.state.setdefault(k, [None, [], self.epoch])
            st[1].extend(olds)

    def dma(self, eng, out, in_, reads=(), writes=(), semkey=None, slot=None, **kw):
        sk = semkey if semkey is not None else writes[0]
        return self.op(eng, lambda e: e.dma_start(out=out, in_=in_, **kw), reads=reads, writes=writes,
                       dma=True, semkey=sk, slot=slot)

    def mm(self, out, lhsT, rhs, start, stop, reads=(), writes=()):
        return self.op("pe", lambda e: e.matmul(out, lhsT, rhs, start=start, stop=stop), reads=reads, writes=writes)

    def emit(self):
        nc = self.nc
        ops = []
        for o in self.ops:
            if isinstance(o, list):
                ops.extend(o)
            else:
                ops.append(o)
        for o in ops:
            for d in o.deps:
                if not d.dma and not (d.eng == "pe" and o.eng == "pe"):
                    d.sig = True
        cnt = {e: 0 for e in ENGS}
        for o in ops:
            if not o.dma and o.sig:
                cnt[o.eng] += 1
                o.cnt = cnt[o.eng]
        run = {}
        for o in ops:
            if o.dma:
                run[o.semkey] = run.get(o.semkey, 0) + 1
                o.cnt = run[o.semkey]
        run = {}
        waits = []
        for o in ops:
            w = {}
            for d in o.deps:
                if d.dma:
                    key = ("dma", d.semkey)
                    if isinstance(d.semkey, tuple) and d.semkey[0] == "ring":
                        val = 16 * d.cnt
                    else:
                        val = 16 * max(run.get(d.semkey, 0), d.cnt)
                else:
                    if d.eng == "pe" and o.eng == "pe":
                        continue
                    key = ("eng", d.eng)
                    val = d.cnt
                if w.get(key, 0) < val:
                    w[key] = val
            waits.append(w)
            if o.dma:
                run[o.semkey] = run.get(o.semkey, 0) + 1
        sems = {}
        for e in ENGS:
            sems[("eng", e)] = self.stack.enter_context(nc.semaphore(f"s_{e}"))
        for i, k in enumerate(self.dma_counts.keys()):
            sems[("dma", k)] = self.stack.enter_context(nc.semaphore(f"d_{i}"))
        self.n_sems = len(sems)
        per_eng = {e: [] for e in ENGS}
        for o, w in zip(ops, waits):
            per_eng[o.eng].append((o, w))
        self.stats = {e: len(per_eng[e]) for e in ENGS}
        self.sigcnt = cnt

        semv = {k: 0 for k in sems}
        pos = {e: 0 for e in ENGS}
        progress = True
        while progress:
            progress = False
            for e in ENGS:
                q = per_eng[e]
                while pos[e] < len(q):
                    o, w = q[pos[e]]
                    if all(semv[k] >= v for k, v in w.items()):
                        if o.dma:
                            semv[("dma", o.semkey)] += 16
                        elif o.sig:
                            semv[("eng", e)] += 1
                        pos[e] += 1
                        progress = True
                    else:
                        break
        stuck = {e: (pos[e], len(per_eng[e])) for e in ENGS if pos[e] < len(per_eng[e])}
        if stuck:
            msg = []
            for e in stuck:
                o, w = per_eng[e][pos[e]]
                msg.append((e, pos[e], {k: (v, semv[k]) for k, v in w.items() if semv[k] < v}))
            raise RuntimeError(f"sync deadlock: {msg}")

        def run_engine(ename, eobj):
            waited = {}
            for o, w in per_eng[ename]:
                for key, val in w.items():
                    if waited.get(key, 0) >= val:
                        continue
                    waited[key] = val
                    eobj.wait_ge(sems[key], val)
                ins = o.fn(eobj)
                if o.dma:
                    ins.then_inc(sems[("dma", o.semkey)], 16)
                elif o.sig:
                    ins.then_inc(sems[("eng", ename)], 1)
            last = {}
            for o, w in per_eng[ename]:
                if o.dma:
                    last[o.semkey] = True
            for k in last:
                eobj.wait_ge(sems[("dma", k)], 16 * self.dma_counts[k])

        with nc.Block() as block:
            @block.sync
            def _(e):
                run_engine("sp", e)

            @block.scalar
            def _(e):
                run_engine("act", e)

            @block.vector
            def _(e):
                run_engine("dve", e)

            @block.gpsimd
            def _(e):
                run_engine("pool", e)

            @block.tensor
            def _(e):
                run_engine("pe", e)
        self.stack.close()


W_SHAPES = {
    "w_in": [2, 1024, 6144], "gate_bias": [2, 3, 1024], "sgu_ws": [2, 8, 128, 128], "sgu_bias": [2, 8, 128],
    "sgu_ln_g": [2, 8, 64], "sgu_ln_b": [2, 8, 64], "ssm_lam_re": [2, 32, 64], "ssm_lam_im": [2, 32, 64],
    "ssm_log_dt": [2, 32], "ssm_b_re": [2, 32, 64, 16], "ssm_b_im": [2, 32, 64, 16], "ssm_c_re": [2, 32, 16, 64],
    "ssm_c_im": [2, 32, 16, 64], "ssm_d": [2, 512], "ssm_w_glu": [2, 512, 512], "ssm_b_glu": [2, 512],
    "p_attn": [2, 512, 1024], "p_sgu": [2, 512, 1024], "p_ssm": [2, 512, 1024], "w_out": [2, 1024, 1024],
    "ln1_g": [2, 1024], "ln1_b": [2, 1024], "ffn_w_gate": [1, 1024, 2816], "ffn_w_up": [1, 1024, 2816],
    "ffn_w_down": [1, 2816, 1024], "moe_router": [1, 1024, 8], "moe_router_bias": [1, 8],
    "moe_w_gate": [1, 8, 1024, 3584], "moe_w_up": [1, 8, 1024, 3584], "moe_w_down": [1, 8, 3584, 1024],
    "ln2_g": [2, 1024], "ln2_b": [2, 1024],
}


def host_consts():
    c = {}
    c["c_ident"] = np.eye(128, dtype=np.float32)
    k = np.arange(128)[:, None, None]
    j = np.arange(4)[None, :, None]
    q = np.arange(512)[None, None, :]
    c["c_caus"] = np.where(j * 128 + k > q, NEG, 0.0).astype(np.float32)
    sel = np.zeros((96, 8, 128), np.float32)
    for hh in range(3):
        for n in range(8):
            sel[32 * hh + n, n, :] = 1.0
    c["c_sel96"] = sel
    s = np.arange(128)
    c["c_m01sgu"] = (s[:, None] <= s[None, :]).astype(np.float32)
    c["c_m01ssm"] = ((s[None, :] // 16) >= (s[:, None] // 16)).astype(np.float32)
    kv = np.concatenate([-np.arange(8), np.arange(8), np.arange(1, 9)]).astype(np.float32)
    c["c_kvec"] = np.tile(kv[None, :], (128, 1))
    return c


CONST_SHAPES = {"c_ident": [128, 128], "c_caus": [128, 4, 512], "c_sel96": [96, 8, 128], "c_m01sgu": [128, 128],
                "c_m01ssm": [128, 128], "c_kvec": [128, 24]}

PHASES = ["qkv", "attn", "sgu", "ssmw", "ssm", "merge", "ln1", "ffn", "ln2"]


def build_program(debug=None, nlayers=NL, dbg_layer=0):
    nc = bass.Bass("TRN2", target_bir_lowering=False)
    din = {}
    din["x"] = nc.dram_tensor("x", [T, D], F32, kind="ExternalInput").ap()
    for n, shp in W_SHAPES.items():
        din[n] = nc.dram_tensor(n, shp, F32, kind="ExternalInput").ap()
    for n, shp in CONST_SHAPES.items():
        din[n] = nc.dram_tensor(n, shp, F32, kind="ExternalInput").ap()
    out_ap = nc.dram_tensor("out", [T, D], F32, kind="ExternalOutput").ap()
    dbg_ap = None
    if debug is not None:
        dbg_ap = nc.dram_tensor("dbg", [128, 16384], F32, kind="ExternalOutput").ap()
    xs_ap = nc.dram_tensor("xs_scr", [T, D], F32).ap()
    scrU = nc.dram_tensor("scrU", [128, 4, 8, 256], BF16).ap()
    scrY = nc.dram_tensor("scrY", [128, 32, 256], BF16).ap()

    P = Prog(nc)
    xT = P.sb([128, 8, T], BF16, "xT")
    AB = P.sb([128, 32768], BF16, "AB")
    C = P.sb([128, 24576], BF16, "C")
    NS = RING_NS
    ring = [P.sb([128, 4096], BF16, f"ring{i}") for i in range(NS)]
    ident = P.sb([128, 128], F32, "ident")
    identb = P.sb([128, 128], BF16, "identb")
    caus = P.sb([128, 4, 512], BF16, "caus")
    sel96 = P.sb([96, 8, 128], BF16, "sel96")
    m01sgu = P.sb([128, 128], F32, "m01sgu")
    m01ssm = P.sb([128, 128], F32, "m01ssm")
    kvec = P.sb([128, 24], F32, "kvec")
    lnG = P.sb([128, 1024], F32, "lnG")
    lnB = P.sb([128, 1024], F32, "lnB")
    vecS = P.sb([32, 128], F32, "vecS")
    vecT = P.sb([128, 32], F32, "vecT")
    small = P.sb([128, 1024], F32, "small")
    xin = [P.sb([128, 1024], F32, f"xin{i}") for i in range(2)]
    psb = [P.ps([128, 512], F32, f"pb{i}") for i in range(8)]

    yaT = AB[:, 0:8192].rearrange("p (c t) -> p c t", c=4)
    ybT = AB[:, 8192:16384].rearrange("p (c t) -> p c t", c=4)
    ycT = AB[:, 16384:24576].rearrange("p (c t) -> p c t", c=4)
    xres = AB[:].bitcast(F32).rearrange("p (t d) -> p t d", d=1024)

    def Cf32(off, n):
        return C[:, off:off + 2 * n].bitcast(F32)

    def ABf32(off, n):
        return AB[:, off:off + 2 * n].bitcast(F32)

    def K(name, rng):
        return [(name, i) for i in rng]

    def act(out, in_, func, R, W, bias=0.0, scale=1.0):
        P.op("act", lambda e: e.activation(out=out, in_=in_, func=func, bias=bias, scale=scale), reads=R, writes=W)

    def tt(out, in0, in1, op, R, W, eng="dve"):
        P.op(eng, lambda e: e.tensor_tensor(out=out, in0=in0, in1=in1, op=op), reads=R, writes=W)

    def ts(out, in0, s1, s2, op0, op1, R, W, eng="dve"):
        if s2 is None:
            P.op(eng, lambda e: e.tensor_scalar(out=out, in0=in0, scalar1=s1, scalar2=None, op0=op0), reads=R, writes=W)
        else:
            P.op(eng, lambda e: e.tensor_scalar(out=out, in0=in0, scalar1=s1, scalar2=s2, op0=op0, op1=op1),
                 reads=R, writes=W)

    def stt(out, in0, scalar, in1, op0, op1, R, W, eng="dve"):
        P.op(eng, lambda e: e.scalar_tensor_tensor(out=out, in0=in0, scalar=scalar, in1=in1, op0=op0, op1=op1),
             reads=R, writes=W)

    def cp(out, in_, R, W, eng="dve"):
        if eng == "act":
            P.op("act", lambda e: e.copy(out=out, in_=in_), reads=R, writes=W)
        else:
            P.op(eng, lambda e: e.tensor_copy(out=out, in_=in_), reads=R, writes=W)

    def red(out, in_, R, W, op=ALU.add):
        P.op("dve", lambda e: e.tensor_reduce(out=out, in_=in_, axis=AX.X, op=op), reads=R, writes=W)

    def memset(ap, val, W, eng="dve", R=()):
        P.op(eng, lambda e: e.memset(ap, val), reads=R, writes=W)

    def recip(out, in_, R, W):
        P.op("dve", lambda e: e.reciprocal(out=out, in_=in_), reads=R, writes=W)

    def transpose(out, in_, idn, R, W):
        P.op("pe", lambda e: e.transpose(out, in_, idn), reads=list(R) + ["ident"], writes=W)

    def mm(out, lhsT, rhs, start, stop, R, W, sgc=False):
        if sgc:
            P.op("pe", lambda e: e.matmul(out, lhsT, rhs, start=start, stop=stop, skip_group_check=True),
                 reads=R, writes=W)
        else:
            P.op("pe", lambda e: e.matmul(out, lhsT, rhs, start=start, stop=stop), reads=R, writes=W)

    class WS:
        def __init__(self):
            self.n = 0
            self.ph = {}

        def acquire(self, name, idx, r0, nr, c0, ncw):
            n = self.n
            self.n += 1
            kc = nr // 128
            assert kc * ncw <= 4096 and nr % 128 == 0
            src = din[name]
            for i in idx:
                src = src[i]
            src = src[r0:r0 + nr, c0:c0 + ncw].rearrange("(kc p) n -> p kc n", p=128)
            s = n % NS
            view = ring[s][:, 0:kc * ncw].rearrange("p (kc n) -> p kc n", n=ncw)
            key = ("ring", s)
            slot = None if n < NS else self.ph[n - NS + 1]
            P.dma("pool", view, src, writes=[key], slot=slot)
            self.ph[n] = P.placeholder()
            return view, key

    ws = WS()

    P.dma("sp", ident[:], din["c_ident"], writes=["ident"], semkey="setup")
    P.dma("sp", m01sgu[:], din["c_m01sgu"], writes=["m01sgu"], semkey="setup")
    P.dma("sp", m01ssm[:], din["c_m01ssm"], writes=["m01ssm"], semkey="setup")
    P.dma("sp", kvec[:], din["c_kvec"], writes=["kvec"], semkey="setup")
    P.dma("pool", identb[:], din["c_ident"], writes=["identb"], semkey="setupc")
    P.dma("pool", caus[:], din["c_caus"], writes=["caus"], semkey="setupc")
    P.dma("pool", sel96[:], din["c_sel96"], writes=["sel96"], semkey="setupc")

    pcnt = [0]

    def next_ps():
        i = pcnt[0] % 2
        pcnt[0] += 1
        return psb[i], ("ps", i)

    mcnt = [0]

    def misc_ps():
        i = 6 + mcnt[0] % 2
        mcnt[0] += 1
        return psb[i], ("ps", i)

    dbg_off = [0]

    def dump(ap2d, R, ncols):
        done = 0
        i = 0
        while done < ncols:
            n = min(1024, ncols - done)
            st = xin[i % 2]
            kx = ("xin", i % 2)
            cp(st[:, 0:n], ap2d[:, done:done + n], list(R), [kx])
            P.dma("sp", dbg_ap[:, dbg_off[0]:dbg_off[0] + n], st[:, 0:n], reads=[kx], writes=[("dbgout", dbg_off[0])],
                  semkey="dbg")
            dbg_off[0] += n
            done += n
            i += 1

    class Stop(Exception):
        pass

    def stage(name, L):
        return debug == name and L == dbg_layer

    def tiles_to_xT(tt_, src_tile, ksrc, router=None):
        for h2 in range(2):
            pm, kpm = misc_ps()
            for c4 in range(4):
                kc = h2 * 4 + c4
                transpose(pm[:, c4 * 128:(c4 + 1) * 128], src_tile[:, kc * 128:(kc + 1) * 128], ident[:], ksrc, [kpm])
            cp(xT[:, h2 * 4:(h2 + 1) * 4, tt_ * 128:(tt_ + 1) * 128], pm[:].rearrange("p (c t) -> p c t", c=4),
               [kpm], [("xT", tt_)], eng="act" if (h2 and router is None) else "dve")
            if router is not None:
                router(tt_, h2, pm, kpm)

    for tt_ in range(16):
        xb = xin[tt_ % 2]
        kx = ("xin", tt_ % 2)
        P.dma("sp", xb[:], din["x"][tt_ * 128:(tt_ + 1) * 128, :], writes=[kx])
        tiles_to_xT(tt_, xb, [kx])

    try:
        for L in range(nlayers):
            P.dma("sp", vecS[0:24, :], din["gate_bias"][L].rearrange("b (n p) -> (b n) p", p=128), writes=["vecS"],
                  semkey="vec")
            P.dma("sp", vecS[24:28, :], din["ssm_b_glu"][L].rearrange("(n p) -> n p", p=128), writes=["vecS2"],
                  semkey="vec")
            pm, kpm = misc_ps()
            transpose(pm[:, 0:28], vecS[0:28, :], ident[0:28, 0:28], ["vecS", "vecS2"], [kpm])
            cp(vecT[:, 0:28], pm[:, 0:28], [kpm], ["vecT"])

            P.barrier()
            qT = C[:, 0:8192].rearrange("p (c t) -> p c t", c=4)
            kT = C[:, 8192:16384].rearrange("p (c t) -> p c t", c=4)
            vA = AB[:, 8192:16512].rearrange("p (t h e) -> p t h e", t=16, h=8)
            mbT3 = AB[:, 16512:22656].rearrange("p (s t) -> p s t", s=3)
            yatok = ABf32(22656, 2048).rearrange("p (q f) -> p q f", q=4)
            PT = [AB[:, 26752 + i * 512:26752 + (i + 1) * 512] for i in range(2)]
            kmBD = AB[:, 27776:28288].rearrange("p (a c n) -> p a c n", a=2, c=4)

            for blk, (dstT, nm) in enumerate([(qT, "qT"), (kT, "kT")]):
                wv, wk = ws.acquire("w_in", (L,), 0, 1024, blk * 512, 512)
                for m in range(4):
                    for tq in range(4):
                        pacc, kp = next_ps()
                        for kc in range(8):
                            mm(pacc[:], wv[:, kc, m * 128:(m + 1) * 128], xT[:, kc, tq * 512:(tq + 1) * 512],
                               kc == 0, kc == 7, [wk] + K("xT", range(tq * 4, tq * 4 + 4)), [kp])
                        cp(dstT[:, m, tq * 512:(tq + 1) * 512], pacc[:], [kp], [(nm, m, tq)],
                           eng="act" if (m + tq) % 2 == 0 else "dve")
            wv, wk = ws.acquire("w_in", (L,), 0, 1024, 1024, 512)
            memset(vA[:, :, :, 64:65], 1.0, [("vA1",)])
            for tt_ in range(16):
                pacc, kp = next_ps()
                for kc in range(8):
                    mm(pacc[:], xT[:, kc, tt_ * 128:(tt_ + 1) * 128], wv[:, kc, :], kc == 0, kc == 7,
                       [wk, ("xT", tt_)], [kp])
                cp(vA[:, tt_, :, 0:64], pacc[:].rearrange("p (h e) -> p h e", h=8), [kp, ("vA1",)], [("vA", tt_)],
                   eng="act" if tt_ % 2 else "dve")
            qkeys = [("qT", m, tq) for m in range(4) for tq in range(4)]
            kkeys = [("kT", m, tq) for m in range(4) for tq in range(4)]
            if stage("qkv", L):
                dump(qT.rearrange("p c t -> p (c t)"), qkeys, 8192)
                dump(kT.rearrange("p c t -> p (c t)"), kkeys, 8192)
                raise Stop()

            km = small[:, 0:32].rearrange("p (c n) -> p c n", c=4)
            kmr = small[:, 32:64].rearrange("p (c n) -> p c n", c=4)
            red(km, kT.rearrange("p c (n l) -> p c n l", n=8), kkeys, ["km"])
            ts(km, km, 1.0 / 256.0, None, ALU.mult, None, ["km"], ["km"])
            memset(kmBD, 0.0, ["kmBD"])
            for c4 in range(4):
                for hh in range(2):
                    h = 2 * c4 + hh
                    pr = slice(hh * 64, hh * 64 + 64)
                    cp(kmBD[pr, 0, c4, h * 8:(h + 1) * 8], km[pr, c4, :], ["km", "kmBD"], ["kmBD"])
                    cp(kmr[pr, c4, :], kmBD[pr, 0, c4, h * 8:(h + 1) * 8], ["kmBD"], ["kmr"])
                    tt(kmr[pr, c4, :], km[pr, c4, :], kmr[pr, c4, :], ALU.subtract, ["km", "kmr"], ["kmr"])
                    cp(kmBD[pr, 1, c4, h * 8:(h + 1) * 8], kmr[pr, c4, :], ["kmr", "kmBD"], ["kmBD"])
            mbpad = small[:, 64:64 + 288].rearrange("p (h n) -> p h n", h=9)
            mb = mbpad[:, 0:8, 0:8]
            gate_sb = small[:, 352:416].rearrange("p (h n) -> p h n", h=8)
            cmpb = small[:, 416:928].rearrange("p (h n m) -> p h n m", h=8, n=8)
            rank = small[:, 928:992].rearrange("p (h n) -> p h n", h=8)
            memset(mbpad, 0.0, ["mb"])

            def mask_dve(tt_):
                b = tt_ // 2
                memset(mb, NEG, ["mb"], R=["mb"])
                if b >= 4:
                    pm, kpm = misc_ps()
                    for c4 in range(4):
                        for a_ in range(2):
                            mm(pm[:, 0:64], qT[:, c4, tt_ * 128:(tt_ + 1) * 128], kmBD[:, a_, c4, :],
                               c4 == 0 and a_ == 0, c4 == 3 and a_ == 1, [("qT", c4, tt_ // 4), "kmBD"], [kpm])
                    cp(gate_sb, pm[:, 0:64].rearrange("p (h n) -> p h n", h=8), [kpm], ["gate_sb"])
                    g = gate_sb[:, :, 0:b]
                    in0 = g.unsqueeze(2).broadcast_to([128, 8, b, b])
                    in1 = g.unsqueeze(3).broadcast_to([128, 8, b, b])
                    tt(cmpb[:, :, 0:b, 0:b], in0, in1, ALU.is_gt, ["gate_sb"], ["cmpb"])
                    red(rank[:, :, 0:b], cmpb[:, :, 0:b, 0:b], ["cmpb"], ["rank"])
                    ts(mb[:, :, 0:b], rank[:, :, 0:b], 2.5, NEG, ALU.is_gt, ALU.mult, ["rank", "mb"], ["mb"])
                elif b > 0:
                    memset(mb[:, :, 0:b], 0.0, ["mb"], R=["mb"])
                memset(mb[:, :, b:b + 1], 0.0, ["mb"], R=["mb"])

            def mask_pe(tt_):
                pm, kpm = misc_ps()
                for s3 in range(3):
                    transpose(pm[0:96, s3 * 128:(s3 + 1) * 128],
                              mbpad[:, 3 * s3:3 * s3 + 3, :].rearrange("p h n -> p (h n)"), ident[:], ["mb"], [kpm])
                cp(mbT3[0:96, :, tt_ * 128:(tt_ + 1) * 128], pm[0:96, 0:384].rearrange("p (s t) -> p s t", s=3),
                   [kpm], [("mbT", tt_)])

            if stage("mask", L):
                for tt_ in range(16):
                    mask_dve(tt_)
                    mask_pe(tt_)
            if stage("mask", L):
                dump(mbT3.rearrange("p s t -> p (s t)"), K("mbT", range(16)), 6144)
                raise Stop()

            steps = [(tq, h, kt) for tq in range(4) for h in range(8) for kt in range(4 * tq + 4)]
            sbuf_of = {}
            scnt = [0]

            def emit_S(tq, h, kt):
                c4, po = h // 2, (h % 2) * 64
                s3, hb = h // 3, 32 * (h % 3)
                i_ = scnt[0] % 2
                scnt[0] += 1
                pS, kS = psb[2 + i_], ("ps", 2 + i_)
                ptb, kpt = PT[i_], ("PT", i_)
                sbuf_of[(tq, h, kt)] = (ptb, kpt)
                diag = kt >= 4 * tq
                need_sel = tq >= 2
                mm(pS[:], kT[po:po + 64, c4, kt * 128:(kt + 1) * 128], qT[po:po + 64, c4, tq * 512:(tq + 1) * 512],
                   True, not (need_sel or diag), [("kT", c4, kt // 4), ("qT", c4, tq)], [kS])
                if need_sel:
                    mm(pS[:], sel96[hb:hb + 8, kt // 2, :], mbT3[hb:hb + 8, s3, tq * 512:(tq + 1) * 512],
                       False, not diag, ["sel96"] + K("mbT", range(tq * 4, tq * 4 + 4)), [kS])
                if diag:
                    mm(pS[:], identb[:], caus[:, kt - 4 * tq, :], False, True, ["identb", "caus"], [kS])
                act(ptb, pS[:], AF.Exp, [kS], [kpt], scale=0.125)

            def emit_PV(tq, h, kt):
                ptb, kpt = sbuf_of.pop((tq, h, kt))
                pv, kpv = psb[4 + h % 2], ("ps", 4 + h % 2)
                pvv = pv[:, 0:260].rearrange("p (q e) -> p q e", q=4)
                for qs in range(4):
                    if kt > 4 * tq + qs:
                        continue
                    first = (kt == 0 and qs == 0)
                    mm(pvv[:, qs, :], ptb[:, qs * 128:(qs + 1) * 128], vA[:, kt, h, :], first, kt == 4 * tq + qs,
                       [kpt, ("vA", kt), ("vA1",)], [kpv], sgc=True)
                if kt == 4 * tq + 3:
                    rc = small[:, 992:996]
                    recip(rc, pvv[:, :, 64], [kpv], ["rc"])
                    tt(yatok[:, :, h * 64:(h + 1) * 64], pvv[:, :, 0:64], rc.unsqueeze(2).broadcast_to([128, 4, 64]),
                       ALU.mult, [kpv, "rc"], [("yatok", h)])
                    if h == 7:
                        for qs in range(4):
                            pm, kpm = misc_ps()
                            for c4 in range(4):
                                transpose(pm[:, c4 * 128:(c4 + 1) * 128], yatok[:, qs, c4 * 128:(c4 + 1) * 128], ident[:],
                                          K("yatok", range(8)), [kpm])
                            tti = tq * 4 + qs
                            cp(yaT[:, :, tti * 128:(tti + 1) * 128], pm[:].rearrange("p (c t) -> p c t", c=4), [kpm],
                               [("yaT", tti)], eng="act" if qs % 2 else "dve")

            emit_S(*steps[0])
            for i_s, st_ in enumerate(steps):
                if i_s + 1 < len(steps):
                    emit_S(*steps[i_s + 1])
                emit_PV(*st_)
                tq_, h_, kt_ = st_
                if tq_ < 2 and kt_ == 4 * tq_ + 3:
                    j_ = tq_ * 8 + h_
                    if j_ >= 1:
                        mask_pe(j_ - 1)
                    mask_dve(j_)
                    if j_ == 15:
                        mask_pe(15)
            if stage("attn", L):
                dump(yaT.rearrange("p c t -> p (c t)"), K("yaT", range(16)), 8192)
                raise Stop()
            P.barrier()
            vln = C[:, 0:8192].rearrange("p (t f) -> p t f", t=16)
            vg = Cf32(8192, 512)
            sq = Cf32(9216, 512)
            sgG = Cf32(10240, 512)
            sgB = Cf32(11264, 512)
            WsT = C[:, 12288:13312].rearrange("p (g t) -> p g t", g=8)
            wsn = Cf32(13312, 1024).rearrange("p (g s) -> p g s", g=8)
            biasT = Cf32(15360, 512).rearrange("p (i t) -> p i t", i=4)
            stmp = Cf32(16384, 512)
            P.dma("sp", sgG, din["sgu_ln_g"][L].rearrange("g d -> (g d)").partition_broadcast(128), writes=["sgG"],
                  semkey="sgu")
            P.dma("sp", sgB, din["sgu_ln_b"][L].rearrange("g d -> (g d)").partition_broadcast(128), writes=["sgB"],
                  semkey="sgu")
            P.dma("sp", wsn, din["sgu_ws"][L].rearrange("g t s -> t g s"), writes=["wsn"], semkey="sgu")
            for g in range(8):
                P.dma("sp", biasT[(g % 2) * 64:(g % 2) * 64 + 64, g // 2, :],
                      din["sgu_bias"][L][g].partition_broadcast(64), writes=[("biasT", g)], semkey="sgu")
            for g in range(8):
                pm, kpm = misc_ps()
                transpose(pm[:, 0:128], wsn[:, g, :], ident[:], ["wsn"], [kpm])
                tt(WsT[:, g, :], pm[:, 0:128], m01sgu[:], ALU.mult, [kpm, "m01sgu"], [("WsT", g)])
            wv, wk = ws.acquire("w_in", (L,), 0, 1024, OFF_SGU, 512)
            for m in range(4):
                for tq in range(4):
                    pacc, kp = next_ps()
                    for kc in range(8):
                        mm(pacc[:], wv[:, kc, m * 128:(m + 1) * 128], xT[:, kc, tq * 512:(tq + 1) * 512],
                           kc == 0, kc == 7, [wk] + K("xT", range(tq * 4, tq * 4 + 4)), [kp])
                    act(ybT[:, m, tq * 512:(tq + 1) * 512], pacc[:], AF.Gelu_apprx_tanh, [kp], [("ybT", m, tq)])
            wv, wk = ws.acquire("w_in", (L,), 0, 1024, OFF_SGU + 512, 512)
            vgs = [vg, Cf32(17408, 512)]
            sqs = [sq, Cf32(18432, 512)]

            def sgu_bufs(tt_):
                par = tt_ % 2
                o = 24 * par
                return vgs[par], sqs[par], small[:, o:o + 8], small[:, o + 8:o + 16], small[:, o + 16:o + 24], par

            def sgu_A(tt_):
                vg_, sq_, st1, st2, st3, par = sgu_bufs(tt_)
                kv, ks = ("vg", par), ("sq", par)
                k1, k2, k3 = ("st1", par), ("st2", par), ("st3", par)
                vg3 = vg_.rearrange("p (g d) -> p g d", g=8)
                sq3 = sq_.rearrange("p (g d) -> p g d", g=8)
                pacc, kp = next_ps()
                for kc in range(8):
                    mm(pacc[:], xT[:, kc, tt_ * 128:(tt_ + 1) * 128], wv[:, kc, :], kc == 0, kc == 7,
                       [wk, ("xT", tt_)], [kp])
                act(vg_, pacc[:], AF.Gelu_apprx_tanh, [kp], [kv])
                red(st1, vg3, [kv], [k1])
                act(sq_, vg_, AF.Square, [kv], [ks])
                red(st2, sq3, [ks], [k2])
                ts(st1, st1, 1.0 / 64.0, None, ALU.mult, None, [k1], [k1])
                tt(st3, st1, st1, ALU.mult, [k1], [k3])
                stt(st2, st2, 1.0 / 64.0, st3, ALU.mult, ALU.subtract, [k2, k3], [k2])
                ts(st2, st2, LN_EPS, None, ALU.add, None, [k2], [k2])
                act(st2, st2, AF.Sqrt, [k2], [k2])

            def sgu_B(tt_):
                vg_, sq_, st1, st2, st3, par = sgu_bufs(tt_)
                kv = ("vg", par)
                k1, k2 = ("st1", par), ("st2", par)
                vg3 = vg_.rearrange("p (g d) -> p g d", g=8)
                recip(st2, st2, [k2], [k2])
                tt(vg3, vg3, st1.unsqueeze(2).broadcast_to([128, 8, 64]), ALU.subtract, [kv, k1], [kv])
                tt(vg3, vg3, st2.unsqueeze(2).broadcast_to([128, 8, 64]), ALU.mult, [kv, k2], [kv])
                tt(vg_, vg_, sgG, ALU.mult, [kv, "sgG"], [kv])
                tt(vln[:, tt_, :], vg_, sgB, ALU.add, [kv, "sgB"], [("vln", tt_)])

            sgu_A(0)
            for tt_ in range(16):
                if tt_ + 1 < 16:
                    sgu_A(tt_ + 1)
                sgu_B(tt_)
            for tq in range(4):
                for i in range(4):
                    pacc, kp = next_ps()
                    for q4 in range(4):
                        tti = tq * 4 + q4
                        for pi in range(2):
                            g = 2 * i + pi
                            mm(pacc[pi * 64:(pi + 1) * 64, q4 * 128:(q4 + 1) * 128], vln[:, tti, g * 64:(g + 1) * 64],
                               WsT[:, g, :], True, True, [("vln", tti), ("WsT", g)], [kp])
                    tt(stmp.rearrange("p (q t) -> p q t", q=4), pacc[:].rearrange("p (q t) -> p q t", q=4),
                       biasT[:, i, :].unsqueeze(1).broadcast_to([128, 4, 128]), ALU.add,
                       [kp, ("biasT", 2 * i), ("biasT", 2 * i + 1)], ["stmp"])
                    tt(ybT[:, i, tq * 512:(tq + 1) * 512], stmp, ybT[:, i, tq * 512:(tq + 1) * 512], ALU.mult,
                       ["stmp", ("ybT", i, tq)], [("ybT", i, tq)])
            ybkeys = [("ybT", m, tq) for m in range(4) for tq in range(4)]
            if stage("sgu", L):
                dump(ybT.rearrange("p c t -> p (c t)"), ybkeys, 8192)
                raise Stop()
            P.barrier()
            ussmP = AB[:, 16384:24576].rearrange("p (m s c) -> p m s c", m=4, s=8)
            Gm = C[:, 0:2048].rearrange("p (g n) -> p g n", g=16)
            Ere = C[:, 2048:3072].rearrange("p (g n) -> p g n", g=16)
            Eim = C[:, 3072:4096].rearrange("p (g n) -> p g n", g=16)
            Ire = C[:, 4096:5120].rearrange("p (g n) -> p g n", g=8)
            Iimn = C[:, 5120:6144].rearrange("p (g n) -> p g n", g=8)
            uW = C[:, 6144:10240].rearrange("p (g c) -> p g c", g=16)
            ycP = C[:, 10240:18432].rearrange("p (m s c) -> p m s c", m=4, s=8)
            Sb = [[Cf32(18432 + 1024 * (2 * b + ri), 512).rearrange("p (i c) -> p i c", i=2) for ri in range(2)]
                  for b in range(2)]
            Sprev = [C[:, 22528 + 512 * ri:22528 + 512 * (ri + 1)].rearrange("p (i c) -> p i c", i=2) for ri in range(2)]
            tA = Cf32(10240, 384).rearrange("p (i k) -> p i k", i=16)
            tB = Cf32(11008, 384).rearrange("p (i k) -> p i k", i=16)
            tC = Cf32(11776, 384).rearrange("p (i k) -> p i k", i=16)
            tD = Cf32(12544, 384).rearrange("p (i k) -> p i k", i=16)
            tI = C[:, 13312:14080].bitcast(I32).rearrange("p (i k) -> p i k", i=16)
            A0dup = Cf32(14080, 128)
            T5 = Cf32(14336, 1024)
            Xre = ABf32(24576, 1024)
            Ximn = ABf32(24576 + 2048, 1024)
            Yre = ABf32(24576 + 4096, 1024)
            Yim = ABf32(24576 + 6144, 1024)
            pwr = lnG[:, 0:384].rearrange("p (i k) -> p i k", i=16)
            pwi = lnG[:, 384:768].rearrange("p (i k) -> p i k", i=16)
            cfre, cfim = lnG[:, 768:784], lnG[:, 784:800]
            dcol = lnG[:, 800:832]
            lamre, lamim, dtv = lnG[:, 832:848], lnG[:, 848:864], lnG[:, 864:880]
            zr, zi, den = lnG[:, 880:896], lnG[:, 896:912], lnG[:, 912:928]
            t16a, t16b, nre = lnG[:, 928:944], lnG[:, 944:960], lnG[:, 960:976]
            bbre = lnB[:, 0:256].rearrange("p (i j) -> p i j", i=16)
            bbim = lnB[:, 256:512].rearrange("p (i j) -> p i j", i=16)
            cTre = lnB[:, 512:768].rearrange("p (i j) -> p i j", i=16)
            cTim = lnB[:, 768:1024].rearrange("p (i j) -> p i j", i=16)
            Alv = [small[:, 128 * q:128 * (q + 1)].rearrange("p (l i) -> p l i", l=8) for q in range(3)]
            braw = [small[:, 384 + 256 * q:384 + 256 * (q + 1)].rearrange("p (i j) -> p i j", i=16) for q in range(2)]

            wv, wk = ws.acquire("w_in", (L,), 0, 1024, OFF_SSM, 512)
            for m in range(4):
                for tq in range(4):
                    pacc, kp = next_ps()
                    for kc in range(8):
                        mm(pacc[:], wv[:, kc, m * 128:(m + 1) * 128], xT[:, kc, tq * 512:(tq + 1) * 512],
                           kc == 0, kc == 7, [wk] + K("xT", range(tq * 4, tq * 4 + 4)), [kp])
                    cp(ussmP[:, m, :, tq * 64:(tq + 1) * 64], pacc[:].rearrange("p (c s) -> p s c", s=8), [kp],
                       [("ussmP", m, tq)], eng="act" if (m + tq) % 2 else "dve")
            ukeys = [("ussmP", m, tq) for m in range(4) for tq in range(4)]
            P.dma("sp", scrU.rearrange("p m s c -> p (m s c)"), ussmP.rearrange("p m s c -> p (m s c)"), reads=ukeys,
                  writes=["scrU"], semkey="scrU")

            NCK = dict(allow_slow_non_contiguous=True)
            for pi in range(2):
                pr = slice(pi * 64, pi * 64 + 64)
                P.dma("sp", lamre[pr, :], din["ssm_lam_re"][L].rearrange("(i two) p -> two p i", two=2)[pi],
                      writes=[("lamre", pi)], semkey="ssmw", **NCK)
                P.dma("sp", lamim[pr, :], din["ssm_lam_im"][L].rearrange("(i two) p -> two p i", two=2)[pi],
                      writes=[("lamim", pi)], semkey="ssmw", **NCK)
                P.dma("sp", dtv[pr, :], din["ssm_log_dt"][L].rearrange("(i two) -> two i", two=2)[pi].partition_broadcast(64),
                      writes=[("dtv", pi)], semkey="ssmw", **NCK)
                P.dma("sp", braw[0][pr, :, :], din["ssm_b_re"][L].rearrange("(i two) p j -> two p i j", two=2)[pi],
                      writes=[("braw0", pi)], semkey="ssmw")
                P.dma("sp", braw[1][pr, :, :], din["ssm_b_im"][L].rearrange("(i two) p j -> two p i j", two=2)[pi],
                      writes=[("braw1", pi)], semkey="ssmw")
            for s in range(8):
                P.dma("sp", dcol[s * 16:(s + 1) * 16, :], din["ssm_d"][L].rearrange("(g j) -> j g", j=16),
                      writes=[("dcol", s)], semkey="ssmw", **NCK)
            both = lambda nm: [(nm, 0), (nm, 1)]
            for ai, (cname, cT) in enumerate([("ssm_c_re", cTre), ("ssm_c_im", cTim)]):
                for m in range(4):
                    srcc = din[cname][L][8 * m:8 * m + 8].rearrange("g i p -> (g i) p")
                    P.dma("sp", A0dup[:, 0:64], srcc, writes=["A0a"], semkey="ssmc")
                    P.dma("sp", A0dup[:, 64:128], srcc, writes=["A0b"], semkey="ssmc")
                    pm, kpm = misc_ps()
                    transpose(pm[:, 0:128], A0dup, ident[:], ["A0a", "A0b"], [kpm])
                    for pi in range(2):
                        pr = slice(pi * 64, pi * 64 + 64)
                        cp(cT[pr, 4 * m:4 * m + 4, :],
                           pm[pr, 0:128].rearrange("p (pl two i) -> p pl two i", two=2, i=16)[:, :, pi, :], [kpm],
                           [("cT", ai, m, pi)])
            cTkeys = [("cT", ai, m, pi) for ai in range(2) for m in range(4) for pi in range(2)]
            if stage("ssmw0", L):
                dump(lnB[:, 512:1024], cTkeys, 512)
                dump(lnG[:, 832:880], both("lamre") + both("lamim") + both("dtv"), 48)
                dump(small[:, 384:896], both("braw0") + both("braw1"), 512)
                dump(lnG[:, 800:832], K("dcol", range(8)), 32)
                raise Stop()
            act(dtv, dtv, AF.Exp, both("dtv"), ["dtvx"])
            tt(zr, lamre, dtv, ALU.mult, both("lamre") + ["dtvx"], ["zr"])
            tt(zi, lamim, dtv, ALU.mult, both("lamim") + ["dtvx"], ["zi"])
            kv_bc = kvec[:].unsqueeze(1).broadcast_to([128, 16, 24])
            tt(tA, zr.unsqueeze(2).broadcast_to([128, 16, 24]), kv_bc, ALU.mult, ["zr", "kvec"], ["tA"])
            act(tA, tA, AF.Exp, ["tA"], ["tA"])
            ts(t16a, zi, float(1.0 / (2 * np.pi)), None, ALU.mult, None, ["zi"], ["t16a"])
            tt(tB, t16a.unsqueeze(2).broadcast_to([128, 16, 24]), kv_bc, ALU.mult, ["t16a", "kvec"], ["tB"])
            cp(tI, tB, ["tB"], ["tI"])
            cp(tC, tI, ["tI"], ["tC"])
            tt(tB, tB, tC, ALU.subtract, ["tB", "tC"], ["tB"])
            for thr, sgn, op in ((0.5, -1.0, ALU.is_gt), (-0.5, 1.0, ALU.is_lt)):
                ts(tC, tB, thr, sgn, op, ALU.mult, ["tB"], ["tC"])
                tt(tB, tB, tC, ALU.add, ["tB", "tC"], ["tB"])
            ts(tD, tB, 0.25, None, ALU.add, None, ["tB"], ["tD"])
            ts(tC, tD, 0.5, -1.0, ALU.is_gt, ALU.mult, ["tD"], ["tC"])
            tt(tD, tD, tC, ALU.add, ["tD", "tC"], ["tD"])
            act(tC, tB, AF.Sin, ["tB"], ["tC"], scale=float(2 * np.pi))
            act(tD, tD, AF.Sin, ["tD"], ["tD"], scale=float(2 * np.pi))
            tt(pwr, tA, tD, ALU.mult, ["tA", "tD"], ["pwr"])
            tt(pwi, tA, tC, ALU.mult, ["tA", "tC"], ["pwi"])
            pw = ["pwr", "pwi"]
            ar, aim = pwr[:, :, 16], pwi[:, :, 16]
            ts(nre, ar, -1.0, None, ALU.add, None, pw, ["nre"])
            tt(den, lamre, lamre, ALU.mult, both("lamre"), ["den"])
            tt(t16a, lamim, lamim, ALU.mult, both("lamim"), ["t16a"])
            tt(den, den, t16a, ALU.add, ["den", "t16a"], ["den"])
            recip(den, den, ["den"], ["den"])
            tt(cfre, nre, lamre, ALU.mult, ["nre"] + both("lamre"), ["cfre"])
            tt(t16a, aim, lamim, ALU.mult, pw + both("lamim"), ["t16a"])
            tt(cfre, cfre, t16a, ALU.add, ["cfre", "t16a"], ["cfre"])
            tt(cfre, cfre, den, ALU.mult, ["cfre", "den"], ["cfre"])
            tt(cfim, aim, lamre, ALU.mult, pw + both("lamre"), ["cfim"])
            tt(t16a, nre, lamim, ALU.mult, ["nre"] + both("lamim"), ["t16a"])
            tt(cfim, cfim, t16a, ALU.subtract, ["cfim", "t16a"], ["cfim"])
            tt(cfim, cfim, den, ALU.mult, ["cfim", "den"], ["cfim"])
            bc16 = lambda v: v.unsqueeze(2).broadcast_to([128, 16, 16])
            t256 = tA[:, :, 0:16]
            tt(bbre, braw[0], bc16(cfre), ALU.mult, both("braw0") + ["cfre"], ["bbre"])
            tt(t256, braw[1], bc16(cfim), ALU.mult, both("braw1") + ["cfim", "tA"], ["tA"])
            tt(bbre, bbre, t256, ALU.subtract, ["bbre", "tA"], ["bbre"])
            tt(bbim, braw[1], bc16(cfre), ALU.mult, both("braw1") + ["cfre"], ["bbim"])
            tt(t256, braw[0], bc16(cfim), ALU.mult, both("braw0") + ["cfim", "tA"], ["tA"])
            tt(bbim, bbim, t256, ALU.add, ["bbim", "tA"], ["bbim"])
            if stage("ssmw1", L):
                dump(lnG[:, 0:768], pw, 768)
                dump(lnB[:, 0:512], ["bbre", "bbim"], 512)
                raise Stop()
            cp(Alv[0][:, 0, :], pwr[:, :, 23], pw, ["Alv"])
            cp(Alv[1][:, 0, :], pwi[:, :, 23], pw + ["Alv"], ["Alv"])
            for l in range(7):
                tt(t16a, Alv[0][:, l, :], Alv[0][:, l, :], ALU.mult, ["Alv"], ["t16a"])
                tt(t16b, Alv[1][:, l, :], Alv[1][:, l, :], ALU.mult, ["Alv"], ["t16b"])
                tt(Alv[0][:, l + 1, :], t16a, t16b, ALU.subtract, ["t16a", "t16b", "Alv"], ["Alv"])
                stt(Alv[1][:, l + 1, :], Alv[0][:, l, :], 2.0, Alv[1][:, l, :], ALU.mult, ALU.mult, ["Alv"], ["Alv"])
            ts(Alv[2], Alv[1], -1.0, None, ALU.mult, None, ["Alv"], ["Alv"])

            def v4(buf):
                return buf.rearrange("p (i a b) -> p i a b", i=8, a=8)

            def v3(buf):
                return buf.rearrange("p (i n) -> p i n", i=8)

            for hf in range(2):
                prs = slice(8 * hf, 8 * hf + 8)

                def bcj(v):
                    return v[:, prs, :].unsqueeze(2).broadcast_to([128, 8, 8, 16])

                def bck(v, k0):
                    return v[:, prs, k0:k0 + 8].unsqueeze(3).broadcast_to([128, 8, 8, 16])

                XY = ["Xre", "Ximn", "Yre", "Yim"]
                tt(v4(Xre), bcj(bbre), bck(pwr, 0), ALU.mult, ["bbre"] + pw, ["Xre"])
                tt(v4(T5), bcj(bbim), bck(pwi, 0), ALU.mult, ["bbim"] + pw, ["T5"])
                tt(Xre, Xre, T5, ALU.subtract, ["Xre", "T5"], ["Xre"])
                tt(v4(Ximn), bcj(bbre), bck(pwi, 0), ALU.mult, ["bbre"] + pw, ["Ximn"])
                tt(v4(T5), bcj(bbim), bck(pwr, 0), ALU.mult, ["bbim"] + pw, ["T5"])
                stt(Ximn, Ximn, -1.0, T5, ALU.mult, ALU.subtract, ["Ximn", "T5"], ["Ximn"])
                tt(v4(Yre), bcj(cTre), bck(pwr, 8), ALU.mult, cTkeys + pw, ["Yre"])
                tt(v4(T5), bcj(cTim), bck(pwi, 8), ALU.mult, cTkeys + pw, ["T5"])
                tt(Yre, Yre, T5, ALU.subtract, ["Yre", "T5"], ["Yre"])
                tt(v4(Yim), bcj(cTre), bck(pwi, 8), ALU.mult, cTkeys + pw, ["Yim"])
                tt(v4(T5), bcj(cTim), bck(pwr, 8), ALU.mult, cTkeys + pw, ["T5"])
                tt(Yim, Yim, T5, ALU.add, ["Yim", "T5"], ["Yim"])
                if stage("ssmw2", L):
                    dump(Xre, ["Xre"], 1024)
                    dump(Ximn, ["Ximn"], 1024)
                    dump(Yre, ["Yre"], 1024)
                    dump(Yim, ["Yim"], 1024)
                    raise Stop()
                for i in range(8):
                    for pi in range(2):
                        pr = slice(pi * 64, pi * 64 + 64)
                        gl = 2 * i + pi
                        g = 16 * hf + gl
                        pm, kpm = misc_ps()
                        mm(pm[:, 0:128], v3(Xre)[pr, i, :], v3(Yre)[pr, i, :], True, False, ["Xre", "Yre"], [kpm])
                        mm(pm[:, 0:128], v3(Ximn)[pr, i, :], v3(Yim)[pr, i, :], False, True, ["Ximn", "Yim"], [kpm])
                        tt(T5[:, 0:128], pm[:, 0:128], m01ssm[:], ALU.mult, [kpm, "m01ssm"], ["T5"])
                        stt(Gm[:, gl, :], ident[:], dcol[:, g:g + 1], T5[:, 0:128], ALU.mult, ALU.add,
                            ["ident", "T5"] + K("dcol", range(8)), [("Gm", gl)])
                if stage("ssmw3", L):
                    dump(Gm.rearrange("p g n -> p (g n)"), K("Gm", range(16)), 2048)
                    raise Stop()
                a7r = pwr[:, prs, 15:16].broadcast_to([128, 8, 128])
                a7i = pwi[:, prs, 15:16].broadcast_to([128, 8, 128])
                tt(v3(Yre), v3(Xre), a7r, ALU.mult, ["Xre"] + pw, ["Yre"])
                tt(v3(T5), v3(Ximn), a7i, ALU.mult, ["Ximn"] + pw, ["T5"])
                tt(Yre, Yre, T5, ALU.add, ["Yre", "T5"], ["Yre"])
                tt(v3(Yim), v3(Xre), a7i, ALU.mult, ["Xre"] + pw, ["Yim"])
                tt(v3(T5), v3(Ximn), a7r, ALU.mult, ["Ximn"] + pw, ["T5"])
                tt(Yim, Yim, T5, ALU.subtract, ["Yim", "T5"], ["Yim"])
                for i in range(8):
                    for pi in range(2):
                        pr = slice(pi * 64, pi * 64 + 64)
                        gl = 2 * i + pi
                        pm, kpm = misc_ps()
                        transpose(pm[:, 0:64], v3(Yre)[pr, i, :], ident[pr, pr], ["Yre"], [kpm])
                        transpose(pm[:, 64:128], v3(Yim)[pr, i, :], ident[pr, pr], ["Yim"], [kpm])
                        cp(Ere[:, gl, :], pm[:, 0:64], [kpm], [("Ere", gl)])
                        cp(Eim[:, gl, :], pm[:, 64:128], [kpm], [("Eim", gl)])
                if stage("ssmw4", L):
                    dump(Gm.rearrange("p g n -> p (g n)"), K("Gm", range(16)), 2048)
                    dump(Ere.rearrange("p g n -> p (g n)"), K("Ere", range(16)), 1024)
                    dump(Eim.rearrange("p g n -> p (g n)"), K("Eim", range(16)), 1024)
                    raise Stop()
                tt(v4(Xre), bcj(cTre), bck(pwr, 16), ALU.mult, cTkeys + pw, ["Xre"])
                tt(v4(Ximn), bcj(cTim), bck(pwi, 16), ALU.mult, cTkeys + pw, ["Ximn"])
                tt(Ire.rearrange("p g n -> p (g n)"), Xre, Ximn, ALU.subtract, ["Xre", "Ximn"], ["Ire"])
                tt(v4(Xre), bcj(cTre), bck(pwi, 16), ALU.mult, cTkeys + pw, ["Xre"])
                tt(v4(Ximn), bcj(cTim), bck(pwr, 16), ALU.mult, cTkeys + pw, ["Ximn"])
                stt(Iimn.rearrange("p g n -> p (g n)"), Xre, -1.0, Ximn, ALU.mult, ALU.subtract, ["Xre", "Ximn"],
                    ["Iimn"])
                if stage("ssmw", L) and hf == 0:
                    dump(Gm.rearrange("p g n -> p (g n)"), K("Gm", range(16)), 2048)
                    dump(Ere.rearrange("p g n -> p (g n)"), K("Ere", range(16)), 1024)
                    dump(Eim.rearrange("p g n -> p (g n)"), K("Eim", range(16)), 1024)
                    dump(Ire.rearrange("p g n -> p (g n)"), ["Ire"], 1024)
                    dump(Iimn.rearrange("p g n -> p (g n)"), ["Iimn"], 1024)
                    raise Stop()
                for s in range(8):
                    for m2 in range(2):
                        srcu = scrU.rearrange("(gg j) m s c -> j s m gg c", j=16)[:, s, 2 * hf + m2, :, :]
                        P.dma("sp", uW[s * 16:(s + 1) * 16, m2 * 8:(m2 + 1) * 8, :], srcu,
                              reads=["scrU"], writes=[("uW", s, m2)], semkey=("uW", hf))
                uWk = [("uW", s, m2) for s in range(8) for m2 in range(2)]

                def do_E(bt):
                    pe_ = [psb[2 + 2 * (bt % 2)], psb[3 + 2 * (bt % 2)]]
                    ke_ = [("ps", 2 + 2 * (bt % 2)), ("ps", 3 + 2 * (bt % 2))]
                    for pl in range(2):
                        for pi in range(2):
                            gl = 4 * bt + 2 * pl + pi
                            for ri, Em in enumerate((Ere, Eim)):
                                mm(pe_[ri][pi * 64:(pi + 1) * 64, pl * 256:(pl + 1) * 256], Em[:, gl, :], uW[:, gl, :],
                                   True, True, [("Ere" if ri == 0 else "Eim", gl), ("yW", gl)] + uWk, [ke_[ri]])
                    return pe_, ke_

                def do_scan(bt, pe_, ke_):
                    cp(Sb[0][0].rearrange("p i c -> p (i c)"), pe_[0][:], [ke_[0]], ["S00"], eng="act")
                    cp(Sb[0][1].rearrange("p i c -> p (i c)"), pe_[1][:], [ke_[1]], ["S01"])
                    for l in range(8):
                        d = 1 << l
                        cur, nxt = Sb[l % 2], Sb[(l + 1) % 2]
                        kc_ = [f"S{l % 2}0", f"S{l % 2}1"]
                        kn_ = [f"S{(l + 1) % 2}0", f"S{(l + 1) % 2}1"]
                        cp(nxt[0][:, :, 0:d], cur[0][:, :, 0:d], [kc_[0]], [kn_[0]])
                        cp(nxt[1][:, :, 0:d], cur[1][:, :, 0:d], [kc_[1]], [kn_[1]])
                        for pl in range(2):
                            pg = 8 * hf + 2 * bt + pl
                            sar = Alv[0][:, l, pg:pg + 1]
                            sai = Alv[1][:, l, pg:pg + 1]
                            sni = Alv[2][:, l, pg:pg + 1]
                            n = 256 - d
                            stt(nxt[0][:, pl, d:], cur[0][:, pl, 0:n], sar, cur[0][:, pl, d:], ALU.mult, ALU.add,
                                [kc_[0], "Alv"], [kn_[0]])
                            stt(nxt[0][:, pl, d:], cur[1][:, pl, 0:n], sni, nxt[0][:, pl, d:], ALU.mult, ALU.add,
                                [kc_[1], kn_[0], "Alv"], [kn_[0]])
                            stt(nxt[1][:, pl, d:], cur[1][:, pl, 0:n], sar, cur[1][:, pl, d:], ALU.mult, ALU.add,
                                [kc_[1], "Alv"], [kn_[1]])
                            stt(nxt[1][:, pl, d:], cur[0][:, pl, 0:n], sai, nxt[1][:, pl, d:], ALU.mult, ALU.add,
                                [kc_[0], kn_[1], "Alv"], [kn_[1]])
                    for ri in range(2):
                        memset(Sprev[ri][:, :, 0:1], 0.0, [("Sprev", ri)])
                        cp(Sprev[ri][:, :, 1:256], Sb[0][ri][:, :, 0:255], [f"S0{ri}", ("Sprev", ri)], [("Sprev", ri)],
                           eng="act" if ri else "dve")

                def do_GI(bt):
                    for pl in range(2):
                        for pi in range(2):
                            pr = slice(pi * 64, pi * 64 + 64)
                            gl = 4 * bt + 2 * pl + pi
                            il = 2 * bt + pl
                            py, kpy = next_ps()
                            mm(py[:, 0:256], Gm[:, gl, :], uW[:, gl, :], True, False, [("Gm", gl), ("yW", gl)] + uWk, [kpy])
                            mm(py[:, 0:256], Ire[pr, il, :], Sprev[0][pr, pl, :], False, False, ["Ire", ("Sprev", 0)], [kpy])
                            mm(py[:, 0:256], Iimn[pr, il, :], Sprev[1][pr, pl, :], False, True, ["Iimn", ("Sprev", 1)], [kpy])
                            cp(uW[:, gl, :], py[:, 0:256], [kpy] + uWk, [("yW", gl)], eng="act" if gl % 2 else "dve")

                e_cur = do_E(0)
                for bt in range(4):
                    do_scan(bt, *e_cur)
                    if bt < 3:
                        e_cur = do_E(bt + 1)
                    do_GI(bt)
                P.dma("sp", scrY[:, 16 * hf:16 * hf + 16, :], uW[:, :, :], reads=K("yW", range(16)) + uWk,
                      writes=[("scrY", hf)], semkey="scrY")
            P.alias([("ycP", gg, m) for gg in range(8) for m in range(4)], ["tA", "tB", "tC", "tD", "tI", "A0a", "A0b", "T5"])
            for gg in range(8):
                for m in range(4):
                    srcy = scrY.rearrange("(t i) (m gg) c -> i gg m t c", i=16, gg=8)[:, gg, m]
                    P.dma("sp", ycP[gg * 16:(gg + 1) * 16, m, :, :], srcy, reads=[("scrY", 0), ("scrY", 1)],
                          writes=[("ycP", gg, m)], semkey="ycP")
            ycPk = [("ycP", gg, m) for gg in range(8) for m in range(4)]
            if stage("ssm", L):
                dump(ycP.rearrange("p m s c -> p (m s c)"), ycPk, 8192)
                raise Stop()
            ycU = C[:, 0:8192].rearrange("p (m t) -> p m t", m=4)
            P.alias([("ycU", m) for m in range(4)],
                    K("Gm", range(16)) + K("Ere", range(16)) + K("Eim", range(16)) + ["Ire", "Iimn"]
                    + K("yW", range(16)) + [("uW", s_, m2) for s_ in range(8) for m2 in range(2)])
            for m in range(4):
                act(ycU[:, m, :].rearrange("p (c s) -> p c s", s=8), ycP[:, m].rearrange("p s c -> p c s"),
                    AF.Gelu_apprx_tanh, ycPk, [("ycU", m)])
            ycUk = K("ycU", range(4))
            if stage("glu0", L):
                dump(ycU.rearrange("p m t -> p (m t)"), ycUk, 8192)
                raise Stop()
            sigb = ABf32(24576, 512)
            wv, wk = ws.acquire("ssm_w_glu", (L,), 0, 512, 0, 512)
            P.alias([("ycT", n4, tq) for n4 in range(4) for tq in range(4)], ukeys)
            for n4 in range(4):
                for tq in range(4):
                    pacc, kp = next_ps()
                    for kc in range(4):
                        mm(pacc[:], wv[:, kc, n4 * 128:(n4 + 1) * 128], ycU[:, kc, tq * 512:(tq + 1) * 512],
                           kc == 0, kc == 3, [wk] + ycUk, [kp])
                    act(sigb, pacc[:], AF.Sigmoid, [kp, "vecT"], ["sigb"], bias=vecT[:, 24 + n4:25 + n4])
                    tt(ycT[:, n4, tq * 512:(tq + 1) * 512], ycU[:, n4, tq * 512:(tq + 1) * 512], sigb, ALU.mult,
                       ycUk + ["sigb"], [("ycT", n4, tq)])
            yckeys = [("ycT", n4, tq) for n4 in range(4) for tq in range(4)]
            if stage("glu", L):
                dump(ycT.rearrange("p c t -> p (c t)"), yckeys, 8192)
                raise Stop()
            P.barrier()
            mergedT = C[:, 0:16384].rearrange("p (n t) -> p n t", n=8)
            pwt = [AB[:, 24576:28672].rearrange("p (k n) -> p k n", k=4),
                   AB[:, 28672:32768].rearrange("p (k n) -> p k n", k=4),
                   C[:, 16384:20480].rearrange("p (k n) -> p k n", k=4)]
            acc = Cf32(20480, 512)
            tmpm = Cf32(21504, 512)
            sig = [C[:, 22528 + 512 * b:22528 + 512 * (b + 1)] for b in range(3)]
            for b, nm in enumerate(["p_attn", "p_sgu", "p_ssm"]):
                P.dma("pool", pwt[b], din[nm][L].rearrange("(k p) n -> p k n", p=128), writes=[("pwt", b)],
                      semkey=("pwt", b))
            ysrc = [(yaT, lambda tq: K("yaT", range(tq * 4, tq * 4 + 4))),
                    (ybT, lambda tq: [("ybT", m, tq) for m in range(4)]),
                    (ycT, lambda tq: [("ycT", m, tq) for m in range(4)])]
            ppc = 0
            for ng in range(2):
                gws = [ws.acquire("w_in", (L,), 0, 1024, OFF_GATE + b * 1024 + ng * 512, 512) for b in range(3)]
                for tq in range(4):
                    for n4 in range(4):
                        n = ng * 4 + n4
                        for b in range(3):
                            gw, gk = gws[b]
                            pg, kpg = next_ps()
                            for kc in range(8):
                                mm(pg[:], gw[:, kc, n4 * 128:(n4 + 1) * 128], xT[:, kc, tq * 512:(tq + 1) * 512],
                                   kc == 0, kc == 7, [gk] + K("xT", range(tq * 4, tq * 4 + 4)), [kpg])
                            act(sig[b], pg[:], AF.Sigmoid, [kpg, "vecT"], [("sig", b)], bias=vecT[:, b * 8 + n:b * 8 + n + 1])
                            pp, kpp = psb[2 + ppc % 2], ("ps", 2 + ppc % 2)
                            ppc += 1
                            ysb, ykf = ysrc[b]
                            for kc in range(4):
                                mm(pp[:], pwt[b][:, kc, n * 128:(n + 1) * 128], ysb[:, kc, tq * 512:(tq + 1) * 512],
                                   kc == 0, kc == 3, [("pwt", b)] + ykf(tq), [kpp])
                            if b == 0:
                                tt(acc, pp[:], sig[0], ALU.mult, [("sig", 0), kpp], ["acc"])
                            elif b == 1:
                                tt(tmpm, pp[:], sig[1], ALU.mult, [("sig", 1), kpp], ["tmpm"])
                                tt(acc, acc, tmpm, ALU.add, ["acc", "tmpm"], ["acc"])
                            else:
                                tt(tmpm, pp[:], sig[2], ALU.mult, [("sig", 2), kpp], ["tmpm"])
                                tt(mergedT[:, n, tq * 512:(tq + 1) * 512], acc, tmpm, ALU.add, ["acc", "tmpm"],
                                   [("mergedT", n, tq)])
            if stage("merge", L):
                dump(mergedT[:, 0:4, :].rearrange("p n t -> p (n t)"), [("mergedT", n, tq) for n in range(4) for tq in range(4)], 8192)
                dump(mergedT[:, 4:8, :].rearrange("p n t -> p (n t)"), [("mergedT", n, tq) for n in range(4, 8) for tq in range(4)], 8192)
                raise Stop()

            P.barrier()
            def ln_bufs(tt_):
                o = 16 * (tt_ % 2)
                return small[:, o:o + 12], small[:, o + 12:o + 14], small[:, o + 14:o + 15], tt_ % 2

            def ln_A(tt_, kx):
                stats, mv, rstd, par = ln_bufs(tt_)
                xr = xres[:, tt_, :]
                P.op("dve", lambda e: e.bn_stats(out=stats[:, 0:6], in_=xr[:, 0:512]), reads=[kx], writes=[("stats", par)])
                P.op("dve", lambda e: e.bn_stats(out=stats[:, 6:12], in_=xr[:, 512:1024]), reads=[kx, ("stats", par)],
                     writes=[("stats", par)])
                P.op("dve", lambda e: e.bn_aggr(out=mv, in_=stats), reads=[("stats", par)], writes=[("mv", par)])
                ts(rstd, mv[:, 1:2], LN_EPS, None, ALU.add, None, [("mv", par)], [("rstd", par)])
                act(rstd, rstd, AF.Sqrt, [("rstd", par)], [("rstd", par)])

            def ln_B(tt_, kx):
                stats, mv, rstd, par = ln_bufs(tt_)
                xr = xres[:, tt_, :]
                recip(rstd, rstd, [("rstd", par)], [("rstd", par)])
                stt(xr, xr, mv[:, 0:1], lnG[:], ALU.subtract, ALU.mult, [kx, ("mv", par), "lnG"], [kx])
                stt(xr, xr, rstd, lnB[:], ALU.mult, ALU.add, [kx, ("rstd", par), "lnB"], [kx])

            moe = (L % 2 == 1)
            router_hook = None
            if moe:
                rw = small[:, 64:128].rearrange("p (k e) -> p k e", k=8)
                logits = small[:, 128:256].rearrange("p (t e) -> p t e", t=16)
                wexp = small[:, 256:384].rearrange("p (t e) -> p t e", t=16)
                t1 = small[:, 384:512].rearrange("p (t e) -> p t e", t=16)
                t2 = small[:, 512:640].rearrange("p (t e) -> p t e", t=16)
                t3 = small[:, 640:768].rearrange("p (t e) -> p t e", t=16)
                m1, m2 = small[:, 768:784], small[:, 784:800]
                rbias = small[:, 800:808]
                xTf = Cf32(22528, 512)
                P.dma("sp", rw, din["moe_router"][0].rearrange("(k p) e -> p k e", p=128), writes=["rw"], semkey="rt")
                P.dma("sp", rbias, din["moe_router_bias"][0].partition_broadcast(128), writes=["rbias"], semkey="rt")
                plog, kplog = psb[5], ("ps", 5)

                def router_hook(tt_, h2, pm, kpm):
                    cp(xTf, pm[:], [kpm], ["xTf"])
                    for c4 in range(4):
                        mm(plog[:, 0:8], xTf[:, c4 * 128:(c4 + 1) * 128], rw[:, h2 * 4 + c4, :], h2 == 0 and c4 == 0,
                           h2 == 1 and c4 == 3, ["xTf", "rw"], [kplog])
                    if h2 == 1:
                        tt(logits[:, tt_, :], plog[:, 0:8], rbias, ALU.add, [kplog, "rbias"], [("logits", tt_)])

            P.dma("sp", lnG[:], din["ln1_g"][L].partition_broadcast(128), writes=["lnG"], semkey="lnp")
            P.dma("sp", lnB[:], din["ln1_b"][L].partition_broadcast(128), writes=["lnB"], semkey="lnp")
            wo = [ws.acquire("w_out", (L,), 0, 1024, hh * 512, 512) for hh in range(2)]
            mkeys = lambda: [("mergedT", n, tq_) for n in range(8) for tq_ in range(4)]
            def ln1_pre(tt_):
                xb = xin[tt_ % 2]
                kx = ("xin", tt_ % 2)
                if L == 0:
                    P.dma("sp", xb[:], din["x"][tt_ * 128:(tt_ + 1) * 128, :], writes=[kx])
                else:
                    P.dma("sp", xb[:], xs_ap[tt_ * 128:(tt_ + 1) * 128, :], reads=[("xs", tt_)], writes=[kx])
                kxr = ("xres", tt_)
                for hh in range(2):
                    pacc, kp = next_ps()
                    for kc in range(8):
                        mm(pacc[:], mergedT[:, kc, tt_ * 128:(tt_ + 1) * 128], wo[hh][0][:, kc, :], kc == 0, kc == 7,
                           [wo[hh][1]] + [("mergedT", kc, tt_ // 4)], [kp])
                    stt(xres[:, tt_, hh * 512:(hh + 1) * 512], xb[:, hh * 512:(hh + 1) * 512], DN_ALPHA, pacc[:],
                        ALU.mult, ALU.add, [kx, kp], [kxr])
                ln_A(tt_, kxr)

            ln1_pre(0)
            for tt_ in range(16):
                if tt_ + 1 < 16:
                    ln1_pre(tt_ + 1)
                ln_B(tt_, ("xres", tt_))
                tiles_to_xT(tt_, xres[:, tt_, :], [("xres", tt_)], router=router_hook)
            if stage("ln1", L):
                for tt_ in range(16):
                    P.dma("sp", dbg_ap[:, tt_ * 1024:(tt_ + 1) * 1024], xres[:, tt_, :], reads=[("xres", tt_)],
                          writes=[("dbgo", tt_)], semkey="dbg")
                raise Stop()

            P.barrier()
            hT = C[:, 0:8192].rearrange("p (f t) -> p f t", f=4)
            sg = Cf32(8192, 512)
            puc = [0]

            stg = [Cf32(9216 + 4096 * i, 2048) for i in range(3)]
            fblocks = []

            class FS:
                def __init__(self):
                    self.n = 0
                    self.nd = 0
                    self.ncast = 0

                def _half(self, j):
                    b, hf = j // 2, j % 2
                    name, idx, r0, nr, c0, ncw = fblocks[b]
                    src = din[name]
                    for i in idx:
                        src = src[i]
                    s_ = b % NS
                    if nr == 1024:
                        srcv = src[r0 + hf * 512:r0 + (hf + 1) * 512, c0:c0 + ncw].rearrange("(kc p) n -> p kc n", p=128)
                        sv = stg[j % 3][:, 0:4 * ncw].rearrange("p (kc n) -> p kc n", kc=4)
                        dv = ring[s_][:, 0:8 * ncw].rearrange("p (kc n) -> p kc n", kc=8)[:, hf * 4:(hf + 1) * 4, :]
                    else:
                        kc = nr // 128
                        srcv = src[r0:r0 + nr, c0 + hf * 512:c0 + (hf + 1) * 512].rearrange("(kc p) n -> p kc n", p=128)
                        sv = stg[j % 3][:, 0:kc * 512].rearrange("p (kc n) -> p kc n", kc=kc)
                        dv = ring[s_][:, 0:kc * 1024].rearrange("p (kc n) -> p kc n", kc=kc)[:, :, hf * 512:(hf + 1) * 512]
                    return srcv, sv, dv, s_

                def ensure_dma(self, upto):
                    while self.nd <= min(upto, 2 * len(fblocks) - 1):
                        j = self.nd
                        srcv, sv, dv, s_ = self._half(j)
                        P.dma("sp", sv, srcv, writes=[("stg", j % 3)])
                        self.nd += 1

                def ensure_cast(self, upto):
                    while self.ncast <= min(upto, len(fblocks) - 1):
                        b = self.ncast
                        for hf in range(2):
                            j = 2 * b + hf
                            self.ensure_dma(j)
                            srcv, sv, dv, s_ = self._half(j)
                            cp(dv, sv, [("stg", j % 3)], [("ring", s_)], eng="act")
                        self.ncast += 1

                def next(self):
                    b = self.n
                    self.n += 1
                    self.ensure_cast(b)
                    self.ensure_dma(2 * b + 3)
                    self.ensure_cast(b + 1)
                    self.ensure_dma(2 * b + 5)
                    name, idx, r0, nr, c0, ncw = fblocks[b]
                    kc = nr // 128
                    view = ring[b % NS][:, 0:kc * ncw].rearrange("p (kc n) -> p kc n", kc=kc)
                    return view, ("ring", b % NS)

            fs = FS()

            def ffn_group(gname, uname, dname, idx, f0, fw, first_scale, wcol):
                fc = fw // 128
                wg, kg = fs.next()
                wu, ku = fs.next()
                wd, kd = fs.next()
                for fcl in range(fc):
                    for tq in range(4):
                        pg, kpg = next_ps()
                        for kc in range(8):
                            mm(pg[:], wg[:, kc, fcl * 128:(fcl + 1) * 128], xT[:, kc, tq * 512:(tq + 1) * 512],
                               kc == 0, kc == 7, [kg] + K("xT", range(tq * 4, tq * 4 + 4)), [kpg])
                        pu, kpu = psb[2 + puc[0] % 2], ("ps", 2 + puc[0] % 2)
                        puc[0] += 1
                        for kc in range(8):
                            mm(pu[:], wu[:, kc, fcl * 128:(fcl + 1) * 128], xT[:, kc, tq * 512:(tq + 1) * 512],
                               kc == 0, kc == 7, [ku] + K("xT", range(tq * 4, tq * 4 + 4)), [kpu])
                        act(sg, pg[:], AF.Silu, [kpg], ["sg"])
                        tt(hT[:, fcl, tq * 512:(tq + 1) * 512], pu[:], sg, ALU.mult, ["sg", kpu], [("hT", fcl, tq)])
                for tt_ in range(16):
                    kxr = ("xres", tt_)
                    for hh in range(2):
                        pd, kpd = next_ps()
                        for fcl in range(fc):
                            mm(pd[:], hT[:, fcl, tt_ * 128:(tt_ + 1) * 128], wd[:, fcl, hh * 512:(hh + 1) * 512],
                               fcl == 0, fcl == fc - 1, [kd, ("hT", fcl, tt_ // 4)], [kpd])
                        xr = xres[:, tt_, hh * 512:(hh + 1) * 512]
                        if wcol is not None:
                            stt(xr, pd[:], wcol(tt_), xr, ALU.mult, ALU.add, [kpd, kxr, "wexp"], [kxr])
                        elif first_scale:
                            stt(xr, xr, DN_ALPHA, pd[:], ALU.mult, ALU.add, [kpd, kxr], [kxr])
                        else:
                            tt(xr, pd[:], xr, ALU.add, [kpd, kxr], [kxr])

            def add_blocks(gname, uname, dname, idx, f0, fw):
                fblocks.extend([(gname, idx, 0, 1024, f0, fw), (uname, idx, 0, 1024, f0, fw), (dname, idx, f0, fw, 0, 1024)])

            if not moe:
                f0 = 0
                while f0 < F_DENSE:
                    fw = min(512, F_DENSE - f0)
                    add_blocks("ffn_w_gate", "ffn_w_up", "ffn_w_down", (L // 2,), f0, fw)
                    f0 += fw
            else:
                for e_ in range(NEXP):
                    for gi in range(F_EXP // 512):
                        add_blocks("moe_w_gate", "moe_w_up", "moe_w_down", (L // 2, 0 if MOE_FAKE else e_),
                                   0 if MOE_FAKE else gi * 512, 512)
            if not moe:
                i_ = L // 2
                f0 = 0
                while f0 < F_DENSE:
                    fw = min(512, F_DENSE - f0)
                    ffn_group("ffn_w_gate", "ffn_w_up", "ffn_w_down", (i_,), f0, fw, f0 == 0, None)
                    f0 += fw
            else:
                i_ = L // 2
                lk = K("logits", range(16))
                red(m1, logits, lk, ["m1"], op=ALU.max)
                tt(t1, logits, m1.unsqueeze(2).broadcast_to([128, 16, 8]), ALU.is_equal, lk + ["m1"], ["t1"])
                stt(t2, t1, NEG, logits, ALU.mult, ALU.add, ["t1"] + lk, ["t2"])
                red(m2, t2, ["t2"], ["m2"], op=ALU.max)
                tt(t3, t2, m2.unsqueeze(2).broadcast_to([128, 16, 8]), ALU.is_equal, ["t2", "m2"], ["t3"])
                tt(m1, m1, m2, ALU.subtract, ["m1", "m2"], ["m1"])
                act(m1, m1, AF.Sigmoid, ["m1"], ["m1"])
                ts(m2, m1, -1.0, 1.0, ALU.mult, ALU.add, ["m1"], ["m2"])
                tt(t1, t1, m1.unsqueeze(2).broadcast_to([128, 16, 8]), ALU.mult, ["t1", "m1"], ["t1"])
                tt(t3, t3, m2.unsqueeze(2).broadcast_to([128, 16, 8]), ALU.mult, ["t3", "m2"], ["t3"])
                tt(wexp, t1, t3, ALU.add, ["t1", "t3"], ["wexp"])
                for tt_ in range(16):
                    ts(xres[:, tt_, :], xres[:, tt_, :], DN_ALPHA, None, ALU.mult, None, [("xres", tt_)], [("xres", tt_)])
                for e_ in range(NEXP):
                    for gi in range(F_EXP // 512):
                        ffn_group("moe_w_gate", "moe_w_up", "moe_w_down", (i_, 0 if MOE_FAKE else e_),
                                  0 if MOE_FAKE else gi * 512, 512, False,
                                  (lambda e__: (lambda tt_: wexp[:, tt_, e__:e__ + 1]))(e_))
            if stage("ffn", L):
                for tt_ in range(16):
                    P.dma("sp", dbg_ap[:, tt_ * 1024:(tt_ + 1) * 1024], xres[:, tt_, :], reads=[("xres", tt_)],
                          writes=[("dbgo", tt_)], semkey="dbg")
                raise Stop()

            P.dma("sp", lnG[:], din["ln2_g"][L].partition_broadcast(128), writes=["lnG"], semkey="lnp")
            P.dma("sp", lnB[:], din["ln2_b"][L].partition_broadcast(128), writes=["lnB"], semkey="lnp")
            ln_A(0, ("xres", 0))
            for tt_ in range(16):
                kxr = ("xres", tt_)
                if tt_ + 1 < 16:
                    ln_A(tt_ + 1, ("xres", tt_ + 1))
                ln_B(tt_, kxr)
                if L == nlayers - 1:
                    P.dma("sp", out_ap[tt_ * 128:(tt_ + 1) * 128, :], xres[:, tt_, :], reads=[kxr], writes=[("out", tt_)],
                          semkey="out")
                else:
                    P.dma("sp", xs_ap[tt_ * 128:(tt_ + 1) * 128, :], xres[:, tt_, :], reads=[kxr], writes=[("xs", tt_)],
                          semkey="xs")
                    tiles_to_xT(tt_, xres[:, tt_, :], [kxr])
    except Stop:
        pass
    P.emit()
    return nc, P


_CACHE = {}


def kernel(**inputs):
    n = 8
    if "prog" not in _CACHE:
        _CACHE["prog"] = build_program()[0]
    nc = _CACHE["prog"]
    consts = host_consts()
    x = np.ascontiguousarray(inputs["x"], dtype=np.float32)
    shared = {k: np.ascontiguousarray(inputs[k], dtype=np.float32) for k in W_SHAPES}
    shared.update(consts)
    in_maps = []
    for c in range(n):
        m = dict(shared)
        m["x"] = np.ascontiguousarray(x[c])
        in_maps.append(m)
    res = run_bass_kernel_spmd(nc, in_maps, core_ids=list(range(n)))
    return np.stack([np.asarray(r["out"], dtype=np.float32) for r in res.results], axis=0)
```

```python
import contextlib
import numpy as np
import concourse.bass as bass
import concourse.mybir as mybir
from concourse.bass_utils import run_bass_kernel_spmd

F32 = mybir.dt.float32
BF16 = mybir.dt.bfloat16
I32 = mybir.dt.int32
ALU = mybir.AluOpType
AF = mybir.ActivationFunctionType
AX = mybir.AxisListType

ENGS = ("pe", "act", "dve", "pool", "sp")

T = 2048
D = 1024
NL = 2
DN_ALPHA = (2.0 * NL) ** 0.25
LN_EPS = 1e-5
NEG = -30000.0
OFF_SGU = 1536
OFF_SSM = 2560
OFF_GATE = 3072
F_DENSE = 2816
F_EXP = 3584
NEXP = 8
MOE_FAKE = False
RING_NS = 4


class Op:
    __slots__ = ("eng", "fn", "deps", "dma", "semkey", "sig", "cnt")

    def __init__(self, eng, fn, deps, dma, semkey):
        self.eng = eng
        self.fn = fn
        self.deps = deps
        self.dma = dma
        self.semkey = semkey
        self.sig = False
        self.cnt = 0


class Prog:
    def __init__(self, nc):
        self.nc = nc
        self.ops = []
        self.state = {}
        self.dma_counts = {}
        self.stack = contextlib.ExitStack()
        self._nm = 0
        self.epoch = 0
        self.barrier_ops = []
        self.last_eng = {}
        self.last_dma = {}

    def sb(self, shape, dtype, name=None):
        self._nm += 1
        return self.stack.enter_context(self.nc.sbuf_tensor(name or f"sb{self._nm}", list(shape), dtype))

    def ps(self, shape, dtype=F32, name=None):
        self._nm += 1
        return self.stack.enter_context(self.nc.psum_tensor(name or f"ps{self._nm}", list(shape), dtype))

    def barrier(self):
        self.epoch += 1
        self.barrier_ops = list(self.last_eng.values()) + list(self.last_dma.values())

    def _deps(self, reads, writes):
        deps = []
        seen = set()

        def add(o):
            if o is not None and id(o) not in seen:
                seen.add(id(o))
                deps.append(o)
        for r in reads:
            st = self.state.get(r)
            if st is not None:
                add(st[0])
        for w in writes:
            st = self.state.get(w)
            if st is not None:
                add(st[0])
                lastr = {}
                for o in st[1]:
                    lastr[(o.eng, o.semkey if o.dma else None)] = o
                for o in lastr.values():
                    add(o)
            exempt = isinstance(w, tuple) and w[0] == "ring"
            if not exempt and (st is None or st[2] < self.epoch):
                for o in self.barrier_ops:
                    add(o)
        return deps

    def _update(self, o, reads, writes):
        for r in reads:
            st = self.state.setdefault(r, [None, [], self.epoch])
            st[1].append(o)
        for w in writes:
            self.state[w] = [o, [], self.epoch]

    def op(self, eng, fn, reads=(), writes=(), dma=False, semkey=None, slot=None):
        o = Op(eng, fn, self._deps(reads, writes), dma, semkey)
        if dma:
            self.dma_counts[semkey] = self.dma_counts.get(semkey, 0) + 1
            if slot is None:
                self.last_dma[semkey] = o
        else:
            self.last_eng[eng] = o
        self._update(o, reads, writes)
        if slot is None:
            self.ops.append(o)
        else:
            slot.append(o)
        return o

    def placeholder(self):
        ph = []
        self.ops.append(ph)
        return ph

    def alias(self, new_keys, old_keys):
        olds = []
        for k in old_keys:
            st = self.state.get(k)
            if st is not None:
                if st[0] is not None:
                    olds.append(st[0])
                olds.extend(st[1])
        for k in new_keys:
            st = self.state.setdefault(k, [None, [], self.epoch])
            st[1].extend(olds)

    def dma(self, eng, out, in_, reads=(), writes=(), semkey=None, slot=None, **kw):
        sk = semkey if semkey is not None else writes[0]
        return self.op(eng, lambda e: e.dma_start(out=out, in_=in_, **kw), reads=reads, writes=writes,
                       dma=True, semkey=sk, slot=slot)

    def mm(self, out, lhsT, rhs, start, stop, reads=(), writes=()):
        return self.op("pe", lambda e: e.matmul(out, lhsT, rhs, start=start, stop=stop), reads=reads, writes=writes)

    def emit(self):
        nc = self.nc
        ops = []
        for o in self.ops:
            if isinstance(o, list):
                ops.extend(o)
            else:
                ops.append(o)
        for o in ops:
            for d in o.deps:
                if not d.dma and not (d.eng == "pe" and o.eng == "pe"):
                    d.sig = True
        cnt = {e: 0 for e in ENGS}
        for o in ops:
            if not o.dma and o.sig:
                cnt[o.eng] += 1
                o.cnt = cnt[o.eng]
        run = {}
        for o in ops:
            if o.dma:
                run[o.semkey] = run.get(o.semkey, 0) + 1
                o.cnt = run[o.semkey]
        run = {}
        waits = []
        for o in ops:
            w = {}
            for d in o.deps:
                if d.dma:
                    key = ("dma", d.semkey)
                    if isinstance(d.semkey, tuple) and d.semkey[0] == "ring":
                        val = 16 * d.cnt
                    else:
                        val = 16 * max(run.get(d.semkey, 0), d.cnt)
                else:
                    if d.eng == "pe" and o.eng == "pe":
                        continue
                    key = ("eng", d.eng)
                    val = d.cnt
                if w.get(key, 0) < val:
                    w[key] = val
            waits.append(w)
            if o.dma:
                run[o.semkey] = run.get(o.semkey, 0) + 1
        sems = {}
        for e in ENGS:
            sems[("eng", e)] = self.stack.enter_context(nc.semaphore(f"s_{e}"))
        for i, k in enumerate(self.dma_counts.keys()):
            sems[("dma", k)] = self.stack.enter_context(nc.semaphore(f"d_{i}"))
        self.n_sems = len(sems)
        per_eng = {e: [] for e in ENGS}
        for o, w in zip(ops, waits):
            per_eng[o.eng].append((o, w))
        self.stats = {e: len(per_eng[e]) for e in ENGS}
        self.sigcnt = cnt

        semv = {k: 0 for k in sems}
        pos = {e: 0 for e in ENGS}
        progress = True
        while progress:
            progress = False
            for e in ENGS:
                q = per_eng[e]
                while pos[e] < len(q):
                    o, w = q[pos[e]]
                    if all(semv[k] >= v for k, v in w.items()):
                        if o.dma:
                            semv[("dma", o.semkey)] += 16
                        elif o.sig:
                            semv[("eng", e)] += 1
                        pos[e] += 1
                        progress = True
                    else:
                        break
        stuck = {e: (pos[e], len(per_eng[e])) for e in ENGS if pos[e] < len(per_eng[e])}
        if stuck:
            msg = []
            for e in stuck:
                o, w = per_eng[e][pos[e]]
                msg.append((e, pos[e], {k: (v, semv[k]) for k, v in w.items() if semv[k] < v}))
            raise RuntimeError(f"sync deadlock: {msg}")

        def run_engine(ename, eobj):
            waited = {}
            for o, w in per_eng[ename]:
                for key, val in w.items():
                    if waited.get(key, 0) >= val:
                        continue
                    waited[key] = val
                    eobj.wait_ge(sems[key], val)
                ins = o.fn(eobj)
                if o.dma:
                    ins.then_inc(sems[("dma", o.semkey)], 16)
                elif o.sig:
                    ins.then_inc(sems[("eng", ename)], 1)
            last = {}
            for o, w in per_eng[ename]:
                if o.dma:
                    last[o.semkey] = True
            for k in last:
                eobj.wait_ge(sems[("dma", k)], 16 * self.dma_counts[k])

        with nc.Block() as block:
            @block.sync
            def _(e):
                run_engine("sp", e)

            @block.scalar
            def _(e):
                run_engine("act", e)

            @block.vector
            def _(e):
                run_engine("dve", e)

            @block.gpsimd
            def _(e):
                run_engine("pool", e)

            @block.tensor
            def _(e):
                run_engine("pe", e)
        self.stack.close()


W_SHAPES = {
    "w_in": [2, 1024, 6144], "gate_bias": [2, 3, 1024], "sgu_ws": [2, 8, 128, 128], "sgu_bias": [2, 8, 128],
    "sgu_ln_g": [2, 8, 64], "sgu_ln_b": [2, 8, 64], "ssm_lam_re": [2, 32, 64], "ssm_lam_im": [2, 32, 64],
    "ssm_log_dt": [2, 32], "ssm_b_re": [2, 32, 64, 16], "ssm_b_im": [2, 32, 64, 16], "ssm_c_re": [2, 32, 16, 64],
    "ssm_c_im": [2, 32, 16, 64], "ssm_d": [2, 512], "ssm_w_glu": [2, 512, 512], "ssm_b_glu": [2, 512],
    "p_attn": [2, 512, 1024], "p_sgu": [2, 512, 1024], "p_ssm": [2, 512, 1024], "w_out": [2, 1024, 1024],
    "ln1_g": [2, 1024], "ln1_b": [2, 1024], "ffn_w_gate": [1, 1024, 2816], "ffn_w_up": [1, 1024, 2816],
    "ffn_w_down": [1, 2816, 1024], "moe_router": [1, 1024, 8], "moe_router_bias": [1, 8],
    "moe_w_gate": [1, 8, 1024, 3584], "moe_w_up": [1, 8, 1024, 3584], "moe_w_down": [1, 8, 3584, 1024],
    "ln2_g": [2, 1024], "ln2_b": [2, 1024],
}


def host_consts():
    c = {}
    c["c_ident"] = np.eye(128, dtype=np.float32)
    k = np.arange(128)[:, None, None]
    j = np.arange(4)[None, :, None]
    q = np.arange(512)[None, None, :]
    c["c_caus"] = np.where(j * 128 + k > q, NEG, 0.0).astype(np.float32)
    sel = np.zeros((96, 8, 128), np.float32)
    for hh in range(3):
        for n in range(8):
            sel[32 * hh + n, n, :] = 1.0
    c["c_sel96"] = sel
    s = np.arange(128)
    c["c_m01sgu"] = (s[:, None] <= s[None, :]).astype(np.float32)
    c["c_m01ssm"] = ((s[None, :] // 16) >= (s[:, None] // 16)).astype(np.float32)
    kv = np.concatenate([-np.arange(8), np.arange(8), np.arange(1, 9)]).astype(np.float32)
    c["c_kvec"] = np.tile(kv[None, :], (128, 1))
    return c


CONST_SHAPES = {"c_ident": [128, 128], "c_caus": [128, 4, 512], "c_sel96": [96, 8, 128], "c_m01sgu": [128, 128],
                "c_m01ssm": [128, 128], "c_kvec": [128, 24]}

PHASES = ["qkv", "attn", "sgu", "ssmw", "ssm", "merge", "ln1", "ffn", "ln2"]


def build_program(debug=None, nlayers=NL, dbg_layer=0):
    nc = bass.Bass("TRN2", target_bir_lowering=False)
    din = {}
    din["x"] = nc.dram_tensor("x", [T, D], F32, kind="ExternalInput").ap()
    for n, shp in W_SHAPES.items():
        din[n] = nc.dram_tensor(n, shp, F32, kind="ExternalInput").ap()
    for n, shp in CONST_SHAPES.items():
        din[n] = nc.dram_tensor(n, shp, F32, kind="ExternalInput").ap()
    out_ap = nc.dram_tensor("out", [T, D], F32, kind="ExternalOutput").ap()
    dbg_ap = None
    if debug is not None:
        dbg_ap = nc.dram_tensor("dbg", [128, 16384], F32, kind="ExternalOutput").ap()
    xs_ap = nc.dram_tensor("xs_scr", [T, D], F32).ap()
    scrU = nc.dram_tensor("scrU", [128, 4, 8, 256], BF16).ap()
    scrY = nc.dram_tensor("scrY", [128, 32, 256], BF16).ap()

    P = Prog(nc)
    xT = P.sb([128, 8, T], BF16, "xT")
    AB = P.sb([128, 32768], BF16, "AB")
    C = P.sb([128, 24576], BF16, "C")
    NS = RING_NS
    ring = [P.sb([128, 4096], BF16, f"ring{i}") for i in range(NS)]
    ident = P.sb([128, 128], F32, "ident")
    identb = P.sb([128, 128], BF16, "identb")
    caus = P.sb([128, 4, 512], BF16, "caus")
    sel96 = P.sb([96, 8, 128], BF16, "sel96")
    m01sgu = P.sb([128, 128], F32, "m01sgu")
    m01ssm = P.sb([128, 128], F32, "m01ssm")
    kvec = P.sb([128, 24], F32, "kvec")
    lnG = P.sb([128, 1024], F32, "lnG")
    lnB = P.sb([128, 1024], F32, "lnB")
    vecS = P.sb([32, 128], F32, "vecS")
    vecT = P.sb([128, 32], F32, "vecT")
    small = P.sb([128, 1024], F32, "small")
    xin = [P.sb([128, 1024], F32, f"xin{i}") for i in range(2)]
    psb = [P.ps([128, 512], F32, f"pb{i}") for i in range(8)]

    yaT = AB[:, 0:8192].rearrange("p (c t) -> p c t", c=4)
    ybT = AB[:, 8192:16384].rearrange("p (c t) -> p c t", c=4)
    ycT = AB[:, 16384:24576].rearrange("p (c t) -> p c t", c=4)
    xres = AB[:].bitcast(F32).rearrange("p (t d) -> p t d", d=1024)

    def Cf32(off, n):
        return C[:, off:off + 2 * n].bitcast(F32)

    def ABf32(off, n):
        return AB[:, off:off + 2 * n].bitcast(F32)

    def K(name, rng):
        return [(name, i) for i in rng]

    def act(out, in_, func, R, W, bias=0.0, scale=1.0):
        P.op("act", lambda e: e.activation(out=out, in_=in_, func=func, bias=bias, scale=scale), reads=R, writes=W)

    def tt(out, in0, in1, op, R, W, eng="dve"):
        P.op(eng, lambda e: e.tensor_tensor(out=out, in0=in0, in1=in1, op=op), reads=R, writes=W)

    def ts(out, in0, s1, s2, op0, op1, R, W, eng="dve"):
        if s2 is None:
            P.op(eng, lambda e: e.tensor_scalar(out=out, in0=in0, scalar1=s1, scalar2=None, op0=op0), reads=R, writes=W)
        else:
            P.op(eng, lambda e: e.tensor_scalar(out=out, in0=in0, scalar1=s1, scalar2=s2, op0=op0, op1=op1),
                 reads=R, writes=W)

    def stt(out, in0, scalar, in1, op0, op1, R, W, eng="dve"):
        P.op(eng, lambda e: e.scalar_tensor_tensor(out=out, in0=in0, scalar=scalar, in1=in1, op0=op0, op1=op1),
             reads=R, writes=W)

    def cp(out, in_, R, W, eng="dve"):
        if eng == "act":
            P.op("act", lambda e: e.copy(out=out, in_=in_), reads=R, writes=W)
        else:
            P.op(eng, lambda e: e.tensor_copy(out=out, in_=in_), reads=R, writes=W)

    def red(out, in_, R, W, op=ALU.add):
        P.op("dve", lambda e: e.tensor_reduce(out=out, in_=in_, axis=AX.X, op=op), reads=R, writes=W)

    def memset(ap, val, W, eng="dve", R=()):
        P.op(eng, lambda e: e.memset(ap, val), reads=R, writes=W)

    def recip(out, in_, R, W):
        P.op("dve", lambda e: e.reciprocal(out=out, in_=in_), reads=R, writes=W)

    def transpose(out, in_, idn, R, W):
        P.op("pe", lambda e: e.transpose(out, in_, idn), reads=list(R) + ["ident"], writes=W)

    def mm(out, lhsT, rhs, start, stop, R, W, sgc=False):
        if sgc:
            P.op("pe", lambda e: e.matmul(out, lhsT, rhs, start=start, stop=stop, skip_group_check=True),
                 reads=R, writes=W)
        else:
            P.op("pe", lambda e: e.matmul(out, lhsT, rhs, start=start, stop=stop), reads=R, writes=W)

    class WS:
        def __init__(self):
            self.n = 0
            self.ph = {}

        def acquire(self, name, idx, r0, nr, c0, ncw):
            n = self.n
            self.n += 1
            kc = nr // 128
            assert kc * ncw <= 4096 and nr % 128 == 0
            src = din[name]
            for i in idx:
                src = src[i]
            src = src[r0:r0 + nr, c0:c0 + ncw].rearrange("(kc p) n -> p kc n", p=128)
            s = n % NS
            view = ring[s][:, 0:kc * ncw].rearrange("p (kc n) -> p kc n", n=ncw)
            key = ("ring", s)
            slot = None if n < NS else self.ph[n - NS + 1]
            P.dma("pool", view, src, writes=[key], slot=slot)
            self.ph[n] = P.placeholder()
            return view, key

    ws = WS()

    P.dma("sp", ident[:], din["c_ident"], writes=["ident"], semkey="setup")
    P.dma("sp", m01sgu[:], din["c_m01sgu"], writes=["m01sgu"], semkey="setup")
    P.dma("sp", m01ssm[:], din["c_m01ssm"], writes=["m01ssm"], semkey="setup")
    P.dma("sp", kvec[:], din["c_kvec"], writes=["kvec"], semkey="setup")
    P.dma("pool", identb[:], din["c_ident"], writes=["identb"], semkey="setupc")
    P.dma("pool", caus[:], din["c_caus"], writes=["caus"], semkey="setupc")
    P.dma("pool", sel96[:], din["c_sel96"], writes=["sel96"], semkey="setupc")

    pcnt = [0]

    def next_ps():
        i = pcnt[0] % 2
        pcnt[0] += 1
        return psb[i], ("ps", i)

    mcnt = [0]

    def misc_ps():
        i = 6 + mcnt[0] % 2
        mcnt[0] += 1
        return psb[i], ("ps", i)

    dbg_off = [0]

    def dump(ap2d, R, ncols):
        done = 0
        i = 0
        while done < ncols:
            n = min(1024, ncols - done)
            st = xin[i % 2]
            kx = ("xin", i % 2)
            cp(st[:, 0:n], ap2d[:, done:done + n], list(R), [kx])
            P.dma("sp", dbg_ap[:, dbg_off[0]:dbg_off[0] + n], st[:, 0:n], reads=[kx], writes=[("dbgout", dbg_off[0])],
                  semkey="dbg")
            dbg_off[0] += n
            done += n
            i += 1

    class Stop(Exception):
        pass

    def stage(name, L):
        return debug == name and L == dbg_layer

    def tiles_to_xT(tt_, src_tile, ksrc, router=None):
        for h2 in range(2):
            pm, kpm = misc_ps()
            for c4 in range(4):
                kc = h2 * 4 + c4
                transpose(pm[:, c4 * 128:(c4 + 1) * 128], src_tile[:, kc * 128:(kc + 1) * 128], ident[:], ksrc, [kpm])
            cp(xT[:, h2 * 4:(h2 + 1) * 4, tt_ * 128:(tt_ + 1) * 128], pm[:].rearrange("p (c t) -> p c t", c=4),
               [kpm], [("xT", tt_)], eng="act" if (h2 and router is None) else "dve")
            if router is not None:
                router(tt_, h2, pm, kpm)

    for tt_ in range(16):
        xb = xin[tt_ % 2]
        kx = ("xin", tt_ % 2)
        P.dma("sp", xb[:], din["x"][tt_ * 128:(tt_ + 1) * 128, :], writes=[kx])
        tiles_to_xT(tt_, xb, [kx])

    try:
        for L in range(nlayers):
            P.dma("sp", vecS[0:24, :], din["gate_bias"][L].rearrange("b (n p) -> (b n) p", p=128), writes=["vecS"],
                  semkey="vec")
            P.dma("sp", vecS[24:28, :], din["ssm_b_glu"][L].rearrange("(n p) -> n p", p=128), writes=["vecS2"],
                  semkey="vec")
            pm, kpm = misc_ps()
            transpose(pm[:, 0:28], vecS[0:28, :], ident[0:28, 0:28], ["vecS", "vecS2"], [kpm])
            cp(vecT[:, 0:28], pm[:, 0:28], [kpm], ["vecT"])

            P.barrier()
            qT = C[:, 0:8192].rearrange("p (c t) -> p c t", c=4)
            kT = C[:, 8192:16384].rearrange("p (c t) -> p c t", c=4)
            vA = AB[:, 8192:16512].rearrange("p (t h e) -> p t h e", t=16, h=8)
            mbT3 = AB[:, 16512:22656].rearrange("p (s t) -> p s t", s=3)
            yatok = ABf32(22656, 2048).rearrange("p (q f) -> p q f", q=4)
            PT = [AB[:, 26752 + i * 512:26752 + (i + 1) * 512] for i in range(2)]
            kmBD = AB[:, 27776:28288].rearrange("p (a c n) -> p a c n", a=2, c=4)

            for blk, (dstT, nm) in enumerate([(qT, "qT"), (kT, "kT")]):
                wv, wk = ws.acquire("w_in", (L,), 0, 1024, blk * 512, 512)
                for m in range(4):
                    for tq in range(4):
                        pacc, kp = next_ps()
                        for kc in range(8):
                            mm(pacc[:], wv[:, kc, m * 128:(m + 1) * 128], xT[:, kc, tq * 512:(tq + 1) * 512],
                               kc == 0, kc == 7, [wk] + K("xT", range(tq * 4, tq * 4 + 4)), [kp])
                        cp(dstT[:, m, tq * 512:(tq + 1) * 512], pacc[:], [kp], [(nm, m, tq)],
                           eng="act" if (m + tq) % 2 == 0 else "dve")
            wv, wk = ws.acquire("w_in", (L,), 0, 1024, 1024, 512)
            memset(vA[:, :, :, 64:65], 1.0, [("vA1",)])
            for tt_ in range(16):
                pacc, kp = next_ps()
                for kc in range(8):
                    mm(pacc[:], xT[:, kc, tt_ * 128:(tt_ + 1) * 128], wv[:, kc, :], kc == 0, kc == 7,
                       [wk, ("xT", tt_)], [kp])
                cp(vA[:, tt_, :, 0:64], pacc[:].rearrange("p (h e) -> p h e", h=8), [kp, ("vA1",)], [("vA", tt_)],
                   eng="act" if tt_ % 2 else "dve")
            qkeys = [("qT", m, tq) for m in range(4) for tq in range(4)]
            kkeys = [("kT", m, tq) for m in range(4) for tq in range(4)]
            if stage("qkv", L):
                dump(qT.rearrange("p c t -> p (c t)"), qkeys, 8192)
                dump(kT.rearrange("p c t -> p (c t)"), kkeys, 8192)
                raise Stop()

            km = small[:, 0:32].rearrange("p (c n) -> p c n", c=4)
            kmr = small[:, 32:64].rearrange("p (c n) -> p c n", c=4)
            red(km, kT.rearrange("p c (n l) -> p c n l", n=8), kkeys, ["km"])
            ts(km, km, 1.0 / 256.0, None, ALU.mult, None, ["km"], ["km"])
            memset(kmBD, 0.0, ["kmBD"])
            for c4 in range(4):
                for hh in range(2):
                    h = 2 * c4 + hh
                    pr = slice(hh * 64, hh * 64 + 64)
                    cp(kmBD[pr, 0, c4, h * 8:(h + 1) * 8], km[pr, c4, :], ["km", "kmBD"], ["kmBD"])
                    cp(kmr[pr, c4, :], kmBD[pr, 0, c4, h * 8:(h + 1) * 8], ["kmBD"], ["kmr"])
                    tt(kmr[pr, c4, :], km[pr, c4, :], kmr[pr, c4, :], ALU.subtract, ["km", "kmr"], ["kmr"])
                    cp(kmBD[pr, 1, c4, h * 8:(h + 1) * 8], kmr[pr, c4, :], ["kmr", "kmBD"], ["kmBD"])
            mbpad = small[:, 64:64 + 288].rearrange("p (h n) -> p h n", h=9)
            mb = mbpad[:, 0:8, 0:8]
            gate_sb = small[:, 352:416].rearrange("p (h n) -> p h n", h=8)
            cmpb = small[:, 416:928].rearrange("p (h n m) -> p h n m", h=8, n=8)
            rank = small[:, 928:992].rearrange("p (h n) -> p h n", h=8)
            memset(mbpad, 0.0, ["mb"])

            def mask_dve(tt_):
                b = tt_ // 2
                memset(mb, NEG, ["mb"], R=["mb"])
                if b >= 4:
                    pm, kpm = misc_ps()
                    for c4 in range(4):
                        for a_ in range(2):
                            mm(pm[:, 0:64], qT[:, c4, tt_ * 128:(tt_ + 1) * 128], kmBD[:, a_, c4, :],
                               c4 == 0 and a_ == 0, c4 == 3 and a_ == 1, [("qT", c4, tt_ // 4), "kmBD"], [kpm])
                    cp(gate_sb, pm[:, 0:64].rearrange("p (h n) -> p h n", h=8), [kpm], ["gate_sb"])
                    g = gate_sb[:, :, 0:b]
                    in0 = g.unsqueeze(2).broadcast_to([128, 8, b, b])
                    in1 = g.unsqueeze(3).broadcast_to([128, 8, b, b])
                    tt(cmpb[:, :, 0:b, 0:b], in0, in1, ALU.is_gt, ["gate_sb"], ["cmpb"])
                    red(rank[:, :, 0:b], cmpb[:, :, 0:b, 0:b], ["cmpb"], ["rank"])
                    ts(mb[:, :, 0:b], rank[:, :, 0:b], 2.5, NEG, ALU.is_gt, ALU.mult, ["rank", "mb"], ["mb"])
                elif b > 0:
                    memset(mb[:, :, 0:b], 0.0, ["mb"], R=["mb"])
                memset(mb[:, :, b:b + 1], 0.0, ["mb"], R=["mb"])

            def mask_pe(tt_):
                pm, kpm = misc_ps()
                for s3 in range(3):
                    transpose(pm[0:96, s3 * 128:(s3 + 1) * 128],
                              mbpad[:, 3 * s3:3 * s3 + 3, :].rearrange("p h n -> p (h n)"), ident[:], ["mb"], [kpm])
                cp(mbT3[0:96, :, tt_ * 128:(tt_ + 1) * 128], pm[0:96, 0:384].rearrange("p (s t) -> p s t", s=3),
                   [kpm], [("mbT", tt_)])

            if stage("mask", L):
                for tt_ in range(16):
                    mask_dve(tt_)
                    mask_pe(tt_)
            if stage("mask", L):
                dump(mbT3.rearrange("p s t -> p (s t)"), K("mbT", range(16)), 6144)
                raise Stop()

            steps = [(tq, h, kt) for tq in range(4) for h in range(8) for kt in range(4 * tq + 4)]
            sbuf_of = {}
            scnt = [0]

            def emit_S(tq, h, kt):
                c4, po = h // 2, (h % 2) * 64
                s3, hb = h // 3, 32 * (h % 3)
                i_ = scnt[0] % 2
                scnt[0] += 1
                pS, kS = psb[2 + i_], ("ps", 2 + i_)
                ptb, kpt = PT[i_], ("PT", i_)
                sbuf_of[(tq, h, kt)] = (ptb, kpt)
                diag = kt >= 4 * tq
                need_sel = tq >= 2
                mm(pS[:], kT[po:po + 64, c4, kt * 128:(kt + 1) * 128], qT[po:po + 64, c4, tq * 512:(tq + 1) * 512],
                   True, not (need_sel or diag), [("kT", c4, kt // 4), ("qT", c4, tq)], [kS])
                if need_sel:
                    mm(pS[:], sel96[hb:hb + 8, kt // 2, :], mbT3[hb:hb + 8, s3, tq * 512:(tq + 1) * 512],
                       False, not diag, ["sel96"] + K("mbT", range(tq * 4, tq * 4 + 4)), [kS])
                if diag:
                    mm(pS[:], identb[:], caus[:, kt - 4 * tq, :], False, True, ["identb", "caus"], [kS])
                act(ptb, pS[:], AF.Exp, [kS], [kpt], scale=0.125)

            def emit_PV(tq, h, kt):
                ptb, kpt = sbuf_of.pop((tq, h, kt))
                pv, kpv = psb[4 + h % 2], ("ps", 4 + h % 2)
                pvv = pv[:, 0:260].rearrange("p (q e) -> p q e", q=4)
                for qs in range(4):
                    if kt > 4 * tq + qs:
                        continue
                    first = (kt == 0 and qs == 0)
                    mm(pvv[:, qs, :], ptb[:, qs * 128:(qs + 1) * 128], vA[:, kt, h, :], first, kt == 4 * tq + qs,
                       [kpt, ("vA", kt), ("vA1",)], [kpv], sgc=True)
                if kt == 4 * tq + 3:
                    rc = small[:, 992:996]
                    recip(rc, pvv[:, :, 64], [kpv], ["rc"])
                    tt(yatok[:, :, h * 64:(h + 1) * 64], pvv[:, :, 0:64], rc.unsqueeze(2).broadcast_to([128, 4, 64]),
                       ALU.mult, [kpv, "rc"], [("yatok", h)])
                    if h == 7:
                        for qs in range(4):
                            pm, kpm = misc_ps()
                            for c4 in range(4):
                                transpose(pm[:, c4 * 128:(c4 + 1) * 128], yatok[:, qs, c4 * 128:(c4 + 1) * 128], ident[:],
                                          K("yatok", range(8)), [kpm])
                            tti = tq * 4 + qs
                            cp(yaT[:, :, tti * 128:(tti + 1) * 128], pm[:].rearrange("p (c t) -> p c t", c=4), [kpm],
                               [("yaT", tti)], eng="act" if qs % 2 else "dve")

            emit_S(*steps[0])
            for i_s, st_ in enumerate(steps):
                if i_s + 1 < len(steps):
                    emit_S(*steps[i_s + 1])
                emit_PV(*st_)
                tq_, h_, kt_ = st_
                if tq_ < 2 and kt_ == 4 * tq_ + 3:
                    j_ = tq_ * 8 + h_
                    if j_ >= 1:
                        mask_pe(j_ - 1)
                    mask_dve(j_)
                    if j_ == 15:
                        mask_pe(15)
            if stage("attn", L):
                dump(yaT.rearrange("p c t -> p (c t)"), K("yaT", range(16)), 8192)
                raise Stop()
            P.barrier()
            vln = C[:, 0:8192].rearrange("p (t f) -> p t f", t=16)
            vg = Cf32(8192, 512)
            sq = Cf32(9216, 512)
            sgG = Cf32(10240, 512)
            sgB = Cf32(11264, 512)
            WsT = C[:, 12288:13312].rearrange("p (g t) -> p g t", g=8)
            wsn = Cf32(13312, 1024).rearrange("p (g s) -> p g s", g=8)
            biasT = Cf32(15360, 512).rearrange("p (i t) -> p i t", i=4)
            stmp = Cf32(16384, 512)
            P.dma("sp", sgG, din["sgu_ln_g"][L].rearrange("g d -> (g d)").partition_broadcast(128), writes=["sgG"],
                  semkey="sgu")
            P.dma("sp", sgB, din["sgu_ln_b"][L].rearrange("g d -> (g d)").partition_broadcast(128), writes=["sgB"],
                  semkey="sgu")
            P.dma("sp", wsn, din["sgu_ws"][L].rearrange("g t s -> t g s"), writes=["wsn"], semkey="sgu")
            for g in range(8):
                P.dma("sp", biasT[(g % 2) * 64:(g % 2) * 64 + 64, g // 2, :],
                      din["sgu_bias"][L][g].partition_broadcast(64), writes=[("biasT", g)], semkey="sgu")
            wv, wk = ws.acquire("w_in", (L,), 0, 1024, OFF_SGU, 512)
            for m in range(4):
                for tq in range(4):
                    pacc, kp = next_ps()
                    for kc in range(8):
                        mm(pacc[:], wv[:, kc, m * 128:(m + 1) * 128], xT[:, kc, tq * 512:(tq + 1) * 512],
                           kc == 0, kc == 7, [wk] + K("xT", range(tq * 4, tq * 4 + 4)), [kp])
                    act(ybT[:, m, tq * 512:(tq + 1) * 512], pacc[:], AF.Gelu_apprx_tanh, [kp], [("ybT", m, tq)])
            for g in range(8):
                pm, kpm = misc_ps()
                transpose(pm[:, 0:128], wsn[:, g, :], ident[:], ["wsn"], [kpm])
                tt(WsT[:, g, :], pm[:, 0:128], m01sgu[:], ALU.mult, [kpm, "m01sgu"], [("WsT", g)])
            wv, wk = ws.acquire("w_in", (L,), 0, 1024, OFF_SGU + 512, 512)
            vgs = [vg, Cf32(17408, 512)]
            sqs = [sq, Cf32(18432, 512)]

            def sgu_bufs(tt_):
                par = tt_ % 2
                o = 24 * par
                return vgs[par], sqs[par], small[:, o:o + 8], small[:, o + 8:o + 16], small[:, o + 16:o + 24], par

            def sgu_A(tt_):
                vg_, sq_, st1, st2, st3, par = sgu_bufs(tt_)
                kv, ks = ("vg", par), ("sq", par)
                k1, k2, k3 = ("st1", par), ("st2", par), ("st3", par)
                vg3 = vg_.rearrange("p (g d) -> p g d", g=8)
                sq3 = sq_.rearrange("p (g d) -> p g d", g=8)
                pacc, kp = next_ps()
                for kc in range(8):
                    mm(pacc[:], xT[:, kc, tt_ * 128:(tt_ + 1) * 128], wv[:, kc, :], kc == 0, kc == 7,
                       [wk, ("xT", tt_)], [kp])
                act(vg_, pacc[:], AF.Gelu_apprx_tanh, [kp], [kv])
                red(st1, vg3, [kv], [k1])
                act(sq_, vg_, AF.Square, [kv], [ks])
                red(st2, sq3, [ks], [k2])
                ts(st1, st1, 1.0 / 64.0, None, ALU.mult, None, [k1], [k1])
                tt(st3, st1, st1, ALU.mult, [k1], [k3])
                stt(st2, st2, 1.0 / 64.0, st3, ALU.mult, ALU.subtract, [k2, k3], [k2])
                ts(st2, st2, LN_EPS, None, ALU.add, None, [k2], [k2])
                act(st2, st2, AF.Sqrt, [k2], [k2])

            def sgu_B(tt_):
                vg_, sq_, st1, st2, st3, par = sgu_bufs(tt_)
                kv = ("vg", par)
                k1, k2 = ("st1", par), ("st2", par)
                vg3 = vg_.rearrange("p (g d) -> p g d", g=8)
                recip(st2, st2, [k2], [k2])
                tt(vg3, vg3, st1.unsqueeze(2).broadcast_to([128, 8, 64]), ALU.subtract, [kv, k1], [kv])
                tt(vg3, vg3, st2.unsqueeze(2).broadcast_to([128, 8, 64]), ALU.mult, [kv, k2], [kv])
                tt(vg_, vg_, sgG, ALU.mult, [kv, "sgG"], [kv])
                tt(vln[:, tt_, :], vg_, sgB, ALU.add, [kv, "sgB"], [("vln", tt_)])

            sgu_A(0)
            for tt_ in range(16):
                if tt_ + 1 < 16:
                    sgu_A(tt_ + 1)
                sgu_B(tt_)
            for tq in range(4):
                for i in range(4):
                    pacc, kp = next_ps()
                    for q4 in range(4):
                        tti = tq * 4 + q4
                        for pi in range(2):
                            g = 2 * i + pi
                            mm(pacc[pi * 64:(pi + 1) * 64, q4 * 128:(q4 + 1) * 128], vln[:, tti, g * 64:(g + 1) * 64],
                               WsT[:, g, :], True, True, [("vln", tti), ("WsT", g)], [kp])
                    tt(stmp.rearrange("p (q t) -> p q t", q=4), pacc[:].rearrange("p (q t) -> p q t", q=4),
                       biasT[:, i, :].unsqueeze(1).broadcast_to([128, 4, 128]), ALU.add,
                       [kp, ("biasT", 2 * i), ("biasT", 2 * i + 1)], ["stmp"])
                    tt(ybT[:, i, tq * 512:(tq + 1) * 512], stmp, ybT[:, i, tq * 512:(tq + 1) * 512], ALU.mult,
                       ["stmp", ("ybT", i, tq)], [("ybT", i, tq)])
            ybkeys = [("ybT", m, tq) for m in range(4) for tq in range(4)]
            if stage("sgu", L):
                dump(ybT.rearrange("p c t -> p (c t)"), ybkeys, 8192)
                raise Stop()
            P.barrier()
            ussmP = AB[:, 16384:24576].rearrange("p (m s c) -> p m s c", m=4, s=8)
            Gm = C[:, 0:2048].rearrange("p (g n) -> p g n", g=16)
            Ere = C[:, 2048:3072].rearrange("p (g n) -> p g n", g=16)
            Eim = C[:, 3072:4096].rearrange("p (g n) -> p g n", g=16)
            Ire = C[:, 4096:5120].rearrange("p (g n) -> p g n", g=8)
            Iimn = C[:, 5120:6144].rearrange("p (g n) -> p g n", g=8)
            uW = C[:, 6144:10240].rearrange("p (g c) -> p g c", g=16)
            ycP = C[:, 10240:18432].rearrange("p (m s c) -> p m s c", m=4, s=8)
            Sb = [[Cf32(18432 + 1024 * (2 * b + ri), 512).rearrange("p (i c) -> p i c", i=2) for ri in range(2)]
                  for b in range(2)]
            Sprev = [C[:, 22528 + 512 * ri:22528 + 512 * (ri + 1)].rearrange("p (i c) -> p i c", i=2) for ri in range(2)]
            tA = Cf32(10240, 384).rearrange("p (i k) -> p i k", i=16)
            tB = Cf32(11008, 384).rearrange("p (i k) -> p i k", i=16)
            tC = Cf32(11776, 384).rearrange("p (i k) -> p i k", i=16)
            tD = Cf32(12544, 384).rearrange("p (i k) -> p i k", i=16)
            tI = C[:, 13312:14080].bitcast(I32).rearrange("p (i k) -> p i k", i=16)
            A0dup = Cf32(14080, 128)
            A0dups = [A0dup, Cf32(16384, 128)]
            T5 = Cf32(14336, 1024)
            Xre = ABf32(24576, 1024)
            Ximn = ABf32(24576 + 2048, 1024)
            Yre = ABf32(24576 + 4096, 1024)
            Yim = ABf32(24576 + 6144, 1024)
            pwr = lnG[:, 0:384].rearrange("p (i k) -> p i k", i=16)
            pwi = lnG[:, 384:768].rearrange("p (i k) -> p i k", i=16)
            cfre, cfim = lnG[:, 768:784], lnG[:, 784:800]
            dcol = lnG[:, 800:832]
            lamre, lamim, dtv = lnG[:, 832:848], lnG[:, 848:864], lnG[:, 864:880]
            zr, zi, den = lnG[:, 880:896], lnG[:, 896:912], lnG[:, 912:928]
            t16a, t16b, nre = lnG[:, 928:944], lnG[:, 944:960], lnG[:, 960:976]
            bbre = lnB[:, 0:256].rearrange("p (i j) -> p i j", i=16)
            bbim = lnB[:, 256:512].rearrange("p (i j) -> p i j", i=16)
            cTre = lnB[:, 512:768].rearrange("p (i j) -> p i j", i=16)
            cTim = lnB[:, 768:1024].rearrange("p (i j) -> p i j", i=16)
            Alv = [small[:, 128 * q:128 * (q + 1)].rearrange("p (l i) -> p l i", l=8) for q in range(3)]
            braw = [small[:, 384 + 256 * q:384 + 256 * (q + 1)].rearrange("p (i j) -> p i j", i=16) for q in range(2)]

            wv, wk = ws.acquire("w_in", (L,), 0, 1024, OFF_SSM, 512)
            for m in range(4):
                for tq in range(4):
                    pacc, kp = next_ps()
                    for kc in range(8):
                        mm(pacc[:], wv[:, kc, m * 128:(m + 1) * 128], xT[:, kc, tq * 512:(tq + 1) * 512],
                           kc == 0, kc == 7, [wk] + K("xT", range(tq * 4, tq * 4 + 4)), [kp])
                    cp(ussmP[:, m, :, tq * 64:(tq + 1) * 64], pacc[:].rearrange("p (c s) -> p s c", s=8), [kp],
                       [("ussmP", m, tq)], eng="act" if (m + tq) % 2 else "dve")
            ukeys = [("ussmP", m, tq) for m in range(4) for tq in range(4)]
            P.dma("sp", scrU.rearrange("p m s c -> p (m s c)"), ussmP.rearrange("p m s c -> p (m s c)"), reads=ukeys,
                  writes=["scrU"], semkey="scrU")

            NCK = dict(allow_slow_non_contiguous=True)
            for pi in range(2):
                pr = slice(pi * 64, pi * 64 + 64)
                P.dma("sp", lamre[pr, :], din["ssm_lam_re"][L].rearrange("(i two) p -> two p i", two=2)[pi],
                      writes=[("lamre", pi)], semkey="ssmw", **NCK)
                P.dma("sp", lamim[pr, :], din["ssm_lam_im"][L].rearrange("(i two) p -> two p i", two=2)[pi],
                      writes=[("lamim", pi)], semkey="ssmw", **NCK)
                P.dma("sp", dtv[pr, :], din["ssm_log_dt"][L].rearrange("(i two) -> two i", two=2)[pi].partition_broadcast(64),
                      writes=[("dtv", pi)], semkey="ssmw", **NCK)
                P.dma("sp", braw[0][pr, :, :], din["ssm_b_re"][L].rearrange("(i two) p j -> two p i j", two=2)[pi],
                      writes=[("braw0", pi)], semkey="ssmw")
                P.dma("sp", braw[1][pr, :, :], din["ssm_b_im"][L].rearrange("(i two) p j -> two p i j", two=2)[pi],
                      writes=[("braw1", pi)], semkey="ssmw")
            for s in range(8):
                P.dma("sp", dcol[s * 16:(s + 1) * 16, :], din["ssm_d"][L].rearrange("(g j) -> j g", j=16),
                      writes=[("dcol", s)], semkey="ssmw", **NCK)
            both = lambda nm: [(nm, 0), (nm, 1)]
            for ai, (cname, cT) in enumerate([("ssm_c_re", cTre), ("ssm_c_im", cTim)]):
                for m in range(4):
                    srcc = din[cname][L][8 * m:8 * m + 8].rearrange("g i p -> (g i) p")
                    ab = (ai * 4 + m) % 2
                    A0d = A0dups[ab]
                    P.dma("sp", A0d[:, 0:64], srcc, writes=[("A0a", ab)], semkey=("ssmc", ab))
                    P.dma("sp", A0d[:, 64:128], srcc, writes=[("A0b", ab)], semkey=("ssmc", ab))
                    pm, kpm = misc_ps()
                    transpose(pm[:, 0:128], A0d, ident[:], [("A0a", ab), ("A0b", ab)], [kpm])
                    for pi in range(2):
                        pr = slice(pi * 64, pi * 64 + 64)
                        cp(cT[pr, 4 * m:4 * m + 4, :],
                           pm[pr, 0:128].rearrange("p (pl two i) -> p pl two i", two=2, i=16)[:, :, pi, :], [kpm],
                           [("cT", ai, m, pi)])
            cTkeys = [("cT", ai, m, pi) for ai in range(2) for m in range(4) for pi in range(2)]
            if stage("ssmw0", L):
                dump(lnB[:, 512:1024], cTkeys, 512)
                dump(lnG[:, 832:880], both("lamre") + both("lamim") + both("dtv"), 48)
                dump(small[:, 384:896], both("braw0") + both("braw1"), 512)
                dump(lnG[:, 800:832], K("dcol", range(8)), 32)
                raise Stop()
            act(dtv, dtv, AF.Exp, both("dtv"), ["dtvx"])
            tt(zr, lamre, dtv, ALU.mult, both("lamre") + ["dtvx"], ["zr"])
            tt(zi, lamim, dtv, ALU.mult, both("lamim") + ["dtvx"], ["zi"])
            kv_bc = kvec[:].unsqueeze(1).broadcast_to([128, 16, 24])
            tt(tA, zr.unsqueeze(2).broadcast_to([128, 16, 24]), kv_bc, ALU.mult, ["zr", "kvec"], ["tA"])
            act(tA, tA, AF.Exp, ["tA"], ["tA"])
            ts(t16a, zi, float(1.0 / (2 * np.pi)), None, ALU.mult, None, ["zi"], ["t16a"])
            tt(tB, t16a.unsqueeze(2).broadcast_to([128, 16, 24]), kv_bc, ALU.mult, ["t16a", "kvec"], ["tB"])
            cp(tI, tB, ["tB"], ["tI"])
            cp(tC, tI, ["tI"], ["tC"])
            tt(tB, tB, tC, ALU.subtract, ["tB", "tC"], ["tB"])
            for thr, sgn, op in ((0.5, -1.0, ALU.is_gt), (-0.5, 1.0, ALU.is_lt)):
                ts(tC, tB, thr, sgn, op, ALU.mult, ["tB"], ["tC"])
                tt(tB, tB, tC, ALU.add, ["tB", "tC"], ["tB"])
            ts(tD, tB, 0.25, None, ALU.add, None, ["tB"], ["tD"])
            ts(tC, tD, 0.5, -1.0, ALU.is_gt, ALU.mult, ["tD"], ["tC"])
            tt(tD, tD, tC, ALU.add, ["tD", "tC"], ["tD"])
            act(tC, tB, AF.Sin, ["tB"], ["tC"], scale=float(2 * np.pi))
            act(tD, tD, AF.Sin, ["tD"], ["tD"], scale=float(2 * np.pi))
            tt(pwr, tA, tD, ALU.mult, ["tA", "tD"], ["pwr"])
            tt(pwi, tA, tC, ALU.mult, ["tA", "tC"], ["pwi"])
            pw = ["pwr", "pwi"]
            ar, aim = pwr[:, :, 16], pwi[:, :, 16]
            ts(nre, ar, -1.0, None, ALU.add, None, pw, ["nre"])
            tt(den, lamre, lamre, ALU.mult, both("lamre"), ["den"])
            tt(t16a, lamim, lamim, ALU.mult, both("lamim"), ["t16a"])
            tt(den, den, t16a, ALU.add, ["den", "t16a"], ["den"])
            recip(den, den, ["den"], ["den"])
            tt(cfre, nre, lamre, ALU.mult, ["nre"] + both("lamre"), ["cfre"])
            tt(t16a, aim, lamim, ALU.mult, pw + both("lamim"), ["t16a"])
            tt(cfre, cfre, t16a, ALU.add, ["cfre", "t16a"], ["cfre"])
            tt(cfre, cfre, den, ALU.mult, ["cfre", "den"], ["cfre"])
            tt(cfim, aim, lamre, ALU.mult, pw + both("lamre"), ["cfim"])
            tt(t16a, nre, lamim, ALU.mult, ["nre"] + both("lamim"), ["t16a"])
            tt(cfim, cfim, t16a, ALU.subtract, ["cfim", "t16a"], ["cfim"])
            tt(cfim, cfim, den, ALU.mult, ["cfim", "den"], ["cfim"])
            bc16 = lambda v: v.unsqueeze(2).broadcast_to([128, 16, 16])
            t256 = tA[:, :, 0:16]
            tt(bbre, braw[0], bc16(cfre), ALU.mult, both("braw0") + ["cfre"], ["bbre"])
            tt(t256, braw[1], bc16(cfim), ALU.mult, both("braw1") + ["cfim", "tA"], ["tA"])
            tt(bbre, bbre, t256, ALU.subtract, ["bbre", "tA"], ["bbre"])
            tt(bbim, braw[1], bc16(cfre), ALU.mult, both("braw1") + ["cfre"], ["bbim"])
            tt(t256, braw[0], bc16(cfim), ALU.mult, both("braw0") + ["cfim", "tA"], ["tA"])
            tt(bbim, bbim, t256, ALU.add, ["bbim", "tA"], ["bbim"])
            if stage("ssmw1", L):
                dump(lnG[:, 0:768], pw, 768)
                dump(lnB[:, 0:512], ["bbre", "bbim"], 512)
                raise Stop()
            cp(Alv[0][:, 0, :], pwr[:, :, 23], pw, ["Alv"])
            cp(Alv[1][:, 0, :], pwi[:, :, 23], pw + ["Alv"], ["Alv"])
            for l in range(7):
                tt(t16a, Alv[0][:, l, :], Alv[0][:, l, :], ALU.mult, ["Alv"], ["t16a"])
                tt(t16b, Alv[1][:, l, :], Alv[1][:, l, :], ALU.mult, ["Alv"], ["t16b"])
                tt(Alv[0][:, l + 1, :], t16a, t16b, ALU.subtract, ["t16a", "t16b", "Alv"], ["Alv"])
                stt(Alv[1][:, l + 1, :], Alv[0][:, l, :], 2.0, Alv[1][:, l, :], ALU.mult, ALU.mult, ["Alv"], ["Alv"])
            ts(Alv[2], Alv[1], -1.0, None, ALU.mult, None, ["Alv"], ["Alv"])

            def v4(buf):
                return buf.rearrange("p (i a b) -> p i a b", i=8, a=8)

            def v3(buf):
                return buf.rearrange("p (i n) -> p i n", i=8)

            for hf in range(2):
                prs = slice(8 * hf, 8 * hf + 8)

                def bcj(v):
                    return v[:, prs, :].unsqueeze(2).broadcast_to([128, 8, 8, 16])

                def bck(v, k0):
                    return v[:, prs, k0:k0 + 8].unsqueeze(3).broadcast_to([128, 8, 8, 16])

                XY = ["Xre", "Ximn", "Yre", "Yim"]
                tt(v4(Xre), bcj(bbre), bck(pwr, 0), ALU.mult, ["bbre"] + pw, ["Xre"])
                tt(v4(T5), bcj(bbim), bck(pwi, 0), ALU.mult, ["bbim"] + pw, ["T5"])
                tt(Xre, Xre, T5, ALU.subtract, ["Xre", "T5"], ["Xre"])
                tt(v4(Ximn), bcj(bbre), bck(pwi, 0), ALU.mult, ["bbre"] + pw, ["Ximn"])
                tt(v4(T5), bcj(bbim), bck(pwr, 0), ALU.mult, ["bbim"] + pw, ["T5"])
                stt(Ximn, Ximn, -1.0, T5, ALU.mult, ALU.subtract, ["Ximn", "T5"], ["Ximn"])
                tt(v4(Yre), bcj(cTre), bck(pwr, 8), ALU.mult, cTkeys + pw, ["Yre"])
                tt(v4(T5), bcj(cTim), bck(pwi, 8), ALU.mult, cTkeys + pw, ["T5"])
                tt(Yre, Yre, T5, ALU.subtract, ["Yre", "T5"], ["Yre"])
                tt(v4(Yim), bcj(cTre), bck(pwi, 8), ALU.mult, cTkeys + pw, ["Yim"])
                tt(v4(T5), bcj(cTim), bck(pwr, 8), ALU.mult, cTkeys + pw, ["T5"])
                tt(Yim, Yim, T5, ALU.add, ["Yim", "T5"], ["Yim"])
                if stage("ssmw2", L):
                    dump(Xre, ["Xre"], 1024)
                    dump(Ximn, ["Ximn"], 1024)
                    dump(Yre, ["Yre"], 1024)
                    dump(Yim, ["Yim"], 1024)
                    raise Stop()
                for i in range(8):
                    for pi in range(2):
                        pr = slice(pi * 64, pi * 64 + 64)
                        gl = 2 * i + pi
                        g = 16 * hf + gl
                        pm, kpm = misc_ps()
                        mm(pm[:, 0:128], v3(Xre)[pr, i, :], v3(Yre)[pr, i, :], True, False, ["Xre", "Yre"], [kpm])
                        mm(pm[:, 0:128], v3(Ximn)[pr, i, :], v3(Yim)[pr, i, :], False, True, ["Ximn", "Yim"], [kpm])
                        tt(T5[:, 0:128], pm[:, 0:128], m01ssm[:], ALU.mult, [kpm, "m01ssm"], ["T5"])
                        stt(Gm[:, gl, :], ident[:], dcol[:, g:g + 1], T5[:, 0:128], ALU.mult, ALU.add,
                            ["ident", "T5"] + K("dcol", range(8)), [("Gm", gl)])
                if stage("ssmw3", L):
                    dump(Gm.rearrange("p g n -> p (g n)"), K("Gm", range(16)), 2048)
                    raise Stop()
                a7r = pwr[:, prs, 15:16].broadcast_to([128, 8, 128])
                a7i = pwi[:, prs, 15:16].broadcast_to([128, 8, 128])
                tt(v3(Yre), v3(Xre), a7r, ALU.mult, ["Xre"] + pw, ["Yre"])
                tt(v3(T5), v3(Ximn), a7i, ALU.mult, ["Ximn"] + pw, ["T5"])
                tt(Yre, Yre, T5, ALU.add, ["Yre", "T5"], ["Yre"])
                tt(v3(Yim), v3(Xre), a7i, ALU.mult, ["Xre"] + pw, ["Yim"])
                tt(v3(T5), v3(Ximn), a7r, ALU.mult, ["Ximn"] + pw, ["T5"])
                tt(Yim, Yim, T5, ALU.subtract, ["Yim", "T5"], ["Yim"])
                for i in range(8):
                    for pi in range(2):
                        pr = slice(pi * 64, pi * 64 + 64)
                        gl = 2 * i + pi
                        pm, kpm = misc_ps()
                        transpose(pm[:, 0:64], v3(Yre)[pr, i, :], ident[pr, pr], ["Yre"], [kpm])
                        transpose(pm[:, 64:128], v3(Yim)[pr, i, :], ident[pr, pr], ["Yim"], [kpm])
                        cp(Ere[:, gl, :], pm[:, 0:64], [kpm], [("Ere", gl)])
                        cp(Eim[:, gl, :], pm[:, 64:128], [kpm], [("Eim", gl)])
                if stage("ssmw4", L):
                    dump(Gm.rearrange("p g n -> p (g n)"), K("Gm", range(16)), 2048)
                    dump(Ere.rearrange("p g n -> p (g n)"), K("Ere", range(16)), 1024)
                    dump(Eim.rearrange("p g n -> p (g n)"), K("Eim", range(16)), 1024)
                    raise Stop()
                tt(v4(Xre), bcj(cTre), bck(pwr, 16), ALU.mult, cTkeys + pw, ["Xre"])
                tt(v4(Ximn), bcj(cTim), bck(pwi, 16), ALU.mult, cTkeys + pw, ["Ximn"])
                tt(Ire.rearrange("p g n -> p (g n)"), Xre, Ximn, ALU.subtract, ["Xre", "Ximn"], ["Ire"])
                tt(v4(Xre), bcj(cTre), bck(pwi, 16), ALU.mult, cTkeys + pw, ["Xre"])
                tt(v4(Ximn), bcj(cTim), bck(pwr, 16), ALU.mult, cTkeys + pw, ["Ximn"])
                stt(Iimn.rearrange("p g n -> p (g n)"), Xre, -1.0, Ximn, ALU.mult, ALU.subtract, ["Xre", "Ximn"],
                    ["Iimn"])
                if stage("ssmw", L) and hf == 0:
                    dump(Gm.rearrange("p g n -> p (g n)"), K("Gm", range(16)), 2048)
                    dump(Ere.rearrange("p g n -> p (g n)"), K("Ere", range(16)), 1024)
                    dump(Eim.rearrange("p g n -> p (g n)"), K("Eim", range(16)), 1024)
                    dump(Ire.rearrange("p g n -> p (g n)"), ["Ire"], 1024)
                    dump(Iimn.rearrange("p g n -> p (g n)"), ["Iimn"], 1024)
                    raise Stop()
                for s in range(8):
                    for m2 in range(2):
                        srcu = scrU.rearrange("(gg j) m s c -> j s m gg c", j=16)[:, s, 2 * hf + m2, :, :]
                        P.dma("sp", uW[s * 16:(s + 1) * 16, m2 * 8:(m2 + 1) * 8, :], srcu,
                              reads=["scrU"], writes=[("uW", s, m2)], semkey=("uW", hf))
                uWk = [("uW", s, m2) for s in range(8) for m2 in range(2)]

                def do_E(bt):
                    pe_ = [psb[2 + 2 * (bt % 2)], psb[3 + 2 * (bt % 2)]]
                    ke_ = [("ps", 2 + 2 * (bt % 2)), ("ps", 3 + 2 * (bt % 2))]
                    for pl in range(2):
                        for pi in range(2):
                            gl = 4 * bt + 2 * pl + pi
                            for ri, Em in enumerate((Ere, Eim)):
                                mm(pe_[ri][pi * 64:(pi + 1) * 64, pl * 256:(pl + 1) * 256], Em[:, gl, :], uW[:, gl, :],
                                   True, True, [("Ere" if ri == 0 else "Eim", gl), ("yW", gl)] + uWk, [ke_[ri]])
                    return pe_, ke_

                def do_scan(bt, pe_, ke_):
                    cp(Sb[0][0].rearrange("p i c -> p (i c)"), pe_[0][:], [ke_[0]], ["S00"], eng="act")
                    cp(Sb[0][1].rearrange("p i c -> p (i c)"), pe_[1][:], [ke_[1]], ["S01"])
                    for l in range(8):
                        d = 1 << l
                        cur, nxt = Sb[l % 2], Sb[(l + 1) % 2]
                        kc_ = [f"S{l % 2}0", f"S{l % 2}1"]
                        kn_ = [f"S{(l + 1) % 2}0", f"S{(l + 1) % 2}1"]
                        cp(nxt[0][:, :, 0:d], cur[0][:, :, 0:d], [kc_[0]], [kn_[0]])
                        cp(nxt[1][:, :, 0:d], cur[1][:, :, 0:d], [kc_[1]], [kn_[1]])
                        for pl in range(2):
                            pg = 8 * hf + 2 * bt + pl
                            sar = Alv[0][:, l, pg:pg + 1]
                            sai = Alv[1][:, l, pg:pg + 1]
                            sni = Alv[2][:, l, pg:pg + 1]
                            n = 256 - d
                            stt(nxt[0][:, pl, d:], cur[0][:, pl, 0:n], sar, cur[0][:, pl, d:], ALU.mult, ALU.add,
                                [kc_[0], "Alv"], [kn_[0]])
                            stt(nxt[0][:, pl, d:], cur[1][:, pl, 0:n], sni, nxt[0][:, pl, d:], ALU.mult, ALU.add,
                                [kc_[1], kn_[0], "Alv"], [kn_[0]])
                            stt(nxt[1][:, pl, d:], cur[1][:, pl, 0:n], sar, cur[1][:, pl, d:], ALU.mult, ALU.add,
                                [kc_[1], "Alv"], [kn_[1]])
                            stt(nxt[1][:, pl, d:], cur[0][:, pl, 0:n], sai, nxt[1][:, pl, d:], ALU.mult, ALU.add,
                                [kc_[0], kn_[1], "Alv"], [kn_[1]])
                    for ri in range(2):
                        memset(Sprev[ri][:, :, 0:1], 0.0, [("Sprev", ri)])
                        cp(Sprev[ri][:, :, 1:256], Sb[0][ri][:, :, 0:255], [f"S0{ri}", ("Sprev", ri)], [("Sprev", ri)],
                           eng="act" if ri else "dve")

                def do_GI(bt):
                    for pl in range(2):
                        for pi in range(2):
                            pr = slice(pi * 64, pi * 64 + 64)
                            gl = 4 * bt + 2 * pl + pi
                            il = 2 * bt + pl
                            py, kpy = next_ps()
                            mm(py[:, 0:256], Gm[:, gl, :], uW[:, gl, :], True, False, [("Gm", gl), ("yW", gl)] + uWk, [kpy])
                            mm(py[:, 0:256], Ire[pr, il, :], Sprev[0][pr, pl, :], False, False, ["Ire", ("Sprev", 0)], [kpy])
                            mm(py[:, 0:256], Iimn[pr, il, :], Sprev[1][pr, pl, :], False, True, ["Iimn", ("Sprev", 1)], [kpy])
                            cp(uW[:, gl, :], py[:, 0:256], [kpy] + uWk, [("yW", gl)], eng="act" if gl % 2 else "dve")

                e_cur = do_E(0)
                for bt in range(4):
                    do_scan(bt, *e_cur)
                    if bt < 3:
                        e_cur = do_E(bt + 1)
                    do_GI(bt)
                P.dma("sp", scrY[:, 16 * hf:16 * hf + 16, :], uW[:, :, :], reads=K("yW", range(16)) + uWk,
                      writes=[("scrY", hf)], semkey="scrY")
            P.alias([("ycP", gg, m) for gg in range(8) for m in range(4)], ["tA", "tB", "tC", "tD", "tI", ("A0a", 0), ("A0b", 0), ("A0a", 1), ("A0b", 1), "T5"])
            for gg in range(8):
                for m in range(4):
                    srcy = scrY.rearrange("(t i) (m gg) c -> i gg m t c", i=16, gg=8)[:, gg, m]
                    P.dma("sp", ycP[gg * 16:(gg + 1) * 16, m, :, :], srcy, reads=[("scrY", 0), ("scrY", 1)],
                          writes=[("ycP", gg, m)], semkey="ycP")
            ycPk = [("ycP", gg, m) for gg in range(8) for m in range(4)]
            if stage("ssm", L):
                dump(ycP.rearrange("p m s c -> p (m s c)"), ycPk, 8192)
                raise Stop()
            ycU = C[:, 0:8192].rearrange("p (m t) -> p m t", m=4)
            P.alias([("ycU", m) for m in range(4)],
                    K("Gm", range(16)) + K("Ere", range(16)) + K("Eim", range(16)) + ["Ire", "Iimn"]
                    + K("yW", range(16)) + [("uW", s_, m2) for s_ in range(8) for m2 in range(2)])
            for m in range(4):
                act(ycU[:, m, :].rearrange("p (c s) -> p c s", s=8), ycP[:, m].rearrange("p s c -> p c s"),
                    AF.Gelu_apprx_tanh, ycPk, [("ycU", m)])
            ycUk = K("ycU", range(4))
            if stage("glu0", L):
                dump(ycU.rearrange("p m t -> p (m t)"), ycUk, 8192)
                raise Stop()
            sigb = ABf32(24576, 512)
            wv, wk = ws.acquire("ssm_w_glu", (L,), 0, 512, 0, 512)
            P.alias([("ycT", n4, tq) for n4 in range(4) for tq in range(4)], ukeys)
            for n4 in range(4):
                for tq in range(4):
                    pacc, kp = next_ps()
                    for kc in range(4):
                        mm(pacc[:], wv[:, kc, n4 * 128:(n4 + 1) * 128], ycU[:, kc, tq * 512:(tq + 1) * 512],
                           kc == 0, kc == 3, [wk] + ycUk, [kp])
                    act(sigb, pacc[:], AF.Sigmoid, [kp, "vecT"], ["sigb"], bias=vecT[:, 24 + n4:25 + n4])
                    tt(ycT[:, n4, tq * 512:(tq + 1) * 512], ycU[:, n4, tq * 512:(tq + 1) * 512], sigb, ALU.mult,
                       ycUk + ["sigb"], [("ycT", n4, tq)])
            yckeys = [("ycT", n4, tq) for n4 in range(4) for tq in range(4)]
            if stage("glu", L):
                dump(ycT.rearrange("p c t -> p (c t)"), yckeys, 8192)
                raise Stop()
            P.barrier()
            mergedT = C[:, 0:16384].rearrange("p (n t) -> p n t", n=8)
            pwt = [AB[:, 24576:28672].rearrange("p (k n) -> p k n", k=4),
                   AB[:, 28672:32768].rearrange("p (k n) -> p k n", k=4),
                   C[:, 16384:20480].rearrange("p (k n) -> p k n", k=4)]
            acc = Cf32(20480, 512)
            tmpm = Cf32(21504, 512)
            sig = [C[:, 22528 + 512 * b:22528 + 512 * (b + 1)] for b in range(3)]
            for b, nm in enumerate(["p_attn", "p_sgu", "p_ssm"]):
                P.dma("pool", pwt[b], din[nm][L].rearrange("(k p) n -> p k n", p=128), writes=[("pwt", b)],
                      semkey=("pwt", b))
            ysrc = [(yaT, lambda tq: K("yaT", range(tq * 4, tq * 4 + 4))),
                    (ybT, lambda tq: [("ybT", m, tq) for m in range(4)]),
                    (ycT, lambda tq: [("ycT", m, tq) for m in range(4)])]
            ppc = 0
            for ng in range(2):
                gws = [ws.acquire("w_in", (L,), 0, 1024, OFF_GATE + b * 1024 + ng * 512, 512) for b in range(3)]
                for tq in range(4):
                    for n4 in range(4):
                        n = ng * 4 + n4
                        for b in range(3):
                            gw, gk = gws[b]
                            pg, kpg = next_ps()
                            for kc in range(8):
                                mm(pg[:], gw[:, kc, n4 * 128:(n4 + 1) * 128], xT[:, kc, tq * 512:(tq + 1) * 512],
                                   kc == 0, kc == 7, [gk] + K("xT", range(tq * 4, tq * 4 + 4)), [kpg])
                            act(sig[b], pg[:], AF.Sigmoid, [kpg, "vecT"], [("sig", b)], bias=vecT[:, b * 8 + n:b * 8 + n + 1])
                            pp, kpp = psb[2 + ppc % 2], ("ps", 2 + ppc % 2)
                            ppc += 1
                            ysb, ykf = ysrc[b]
                            for kc in range(4):
                                mm(pp[:], pwt[b][:, kc, n * 128:(n + 1) * 128], ysb[:, kc, tq * 512:(tq + 1) * 512],
                                   kc == 0, kc == 3, [("pwt", b)] + ykf(tq), [kpp])
                            if b == 0:
                                tt(acc, pp[:], sig[0], ALU.mult, [("sig", 0), kpp], ["acc"])
                            elif b == 1:
                                tt(tmpm, pp[:], sig[1], ALU.mult, [("sig", 1), kpp], ["tmpm"])
                                tt(acc, acc, tmpm, ALU.add, ["acc", "tmpm"], ["acc"])
                            else:
                                tt(tmpm, pp[:], sig[2], ALU.mult, [("sig", 2), kpp], ["tmpm"])
                                tt(mergedT[:, n, tq * 512:(tq + 1) * 512], acc, tmpm, ALU.add, ["acc", "tmpm"],
                                   [("mergedT", n, tq)])
            if stage("merge", L):
                dump(mergedT[:, 0:4, :].rearrange("p n t -> p (n t)"), [("mergedT", n, tq) for n in range(4) for tq in range(4)], 8192)
                dump(mergedT[:, 4:8, :].rearrange("p n t -> p (n t)"), [("mergedT", n, tq) for n in range(4, 8) for tq in range(4)], 8192)
                raise Stop()

            P.barrier()
            def ln_bufs(tt_):
                o = 16 * (tt_ % 2)
                return small[:, o:o + 12], small[:, o + 12:o + 14], small[:, o + 14:o + 15], tt_ % 2

            def ln_A(tt_, kx):
                stats, mv, rstd, par = ln_bufs(tt_)
                xr = xres[:, tt_, :]
                P.op("dve", lambda e: e.bn_stats(out=stats[:, 0:6], in_=xr[:, 0:512]), reads=[kx], writes=[("stats", par)])
                P.op("dve", lambda e: e.bn_stats(out=stats[:, 6:12], in_=xr[:, 512:1024]), reads=[kx, ("stats", par)],
                     writes=[("stats", par)])
                P.op("dve", lambda e: e.bn_aggr(out=mv, in_=stats), reads=[("stats", par)], writes=[("mv", par)])
                ts(rstd, mv[:, 1:2], LN_EPS, None, ALU.add, None, [("mv", par)], [("rstd", par)])
                act(rstd, rstd, AF.Sqrt, [("rstd", par)], [("rstd", par)])

            def ln_B(tt_, kx):
                stats, mv, rstd, par = ln_bufs(tt_)
                xr = xres[:, tt_, :]
                recip(rstd, rstd, [("rstd", par)], [("rstd", par)])
                stt(xr, xr, mv[:, 0:1], lnG[:], ALU.subtract, ALU.mult, [kx, ("mv", par), "lnG"], [kx])
                stt(xr, xr, rstd, lnB[:], ALU.mult, ALU.add, [kx, ("rstd", par), "lnB"], [kx])

            moe = (L % 2 == 1)
            router_hook = None
            if moe:
                rw = small[:, 64:128].rearrange("p (k e) -> p k e", k=8)
                logits = small[:, 128:256].rearrange("p (t e) -> p t e", t=16)
                wexp = small[:, 256:384].rearrange("p (t e) -> p t e", t=16)
                t1 = small[:, 384:512].rearrange("p (t e) -> p t e", t=16)
                t2 = small[:, 512:640].rearrange("p (t e) -> p t e", t=16)
                t3 = small[:, 640:768].rearrange("p (t e) -> p t e", t=16)
                m1, m2 = small[:, 768:784], small[:, 784:800]
                rbias = small[:, 800:808]
                xTf = Cf32(22528, 512)
                P.dma("sp", rw, din["moe_router"][0].rearrange("(k p) e -> p k e", p=128), writes=["rw"], semkey="rt")
                P.dma("sp", rbias, din["moe_router_bias"][0].partition_broadcast(128), writes=["rbias"], semkey="rt")
                plog, kplog = psb[5], ("ps", 5)

                def router_hook(tt_, h2, pm, kpm):
                    cp(xTf, pm[:], [kpm], ["xTf"])
                    for c4 in range(4):
                        mm(plog[:, 0:8], xTf[:, c4 * 128:(c4 + 1) * 128], rw[:, h2 * 4 + c4, :], h2 == 0 and c4 == 0,
                           h2 == 1 and c4 == 3, ["xTf", "rw"], [kplog])
                    if h2 == 1:
                        tt(logits[:, tt_, :], plog[:, 0:8], rbias, ALU.add, [kplog, "rbias"], [("logits", tt_)])

            P.dma("sp", lnG[:], din["ln1_g"][L].partition_broadcast(128), writes=["lnG"], semkey="lnp")
            P.dma("sp", lnB[:], din["ln1_b"][L].partition_broadcast(128), writes=["lnB"], semkey="lnp")
            wo = [ws.acquire("w_out", (L,), 0, 1024, hh * 512, 512) for hh in range(2)]
            mkeys = lambda: [("mergedT", n, tq_) for n in range(8) for tq_ in range(4)]
            def ln1_pre(tt_):
                xb = xin[tt_ % 2]
                kx = ("xin", tt_ % 2)
                if L == 0:
                    P.dma("sp", xb[:], din["x"][tt_ * 128:(tt_ + 1) * 128, :], writes=[kx])
                else:
                    P.dma("sp", xb[:], xs_ap[tt_ * 128:(tt_ + 1) * 128, :], reads=[("xs", tt_)], writes=[kx])
                kxr = ("xres", tt_)
                for hh in range(2):
                    pacc, kp = next_ps()
                    for kc in range(8):
                        mm(pacc[:], mergedT[:, kc, tt_ * 128:(tt_ + 1) * 128], wo[hh][0][:, kc, :], kc == 0, kc == 7,
                           [wo[hh][1]] + [("mergedT", kc, tt_ // 4)], [kp])
                    stt(xres[:, tt_, hh * 512:(hh + 1) * 512], xb[:, hh * 512:(hh + 1) * 512], DN_ALPHA, pacc[:],
                        ALU.mult, ALU.add, [kx, kp], [kxr])
                ln_A(tt_, kxr)

            ln1_pre(0)
            for tt_ in range(16):
                if tt_ + 1 < 16:
                    ln1_pre(tt_ + 1)
                ln_B(tt_, ("xres", tt_))
                tiles_to_xT(tt_, xres[:, tt_, :], [("xres", tt_)], router=router_hook)
            if stage("ln1", L):
                for tt_ in range(16):
                    P.dma("sp", dbg_ap[:, tt_ * 1024:(tt_ + 1) * 1024], xres[:, tt_, :], reads=[("xres", tt_)],
                          writes=[("dbgo", tt_)], semkey="dbg")
                raise Stop()

            P.barrier()
            hT = C[:, 0:8192].rearrange("p (f t) -> p f t", f=4)
            sg = Cf32(8192, 512)
            puc = [0]

            stg = [Cf32(9216 + 4096 * i, 2048) for i in range(3)]
            fblocks = []

            class FS:
                def __init__(self):
                    self.n = 0
                    self.nd = 0
                    self.ncast = 0

                def _half(self, j):
                    b, hf = j // 2, j % 2
                    name, idx, r0, nr, c0, ncw = fblocks[b]
                    src = din[name]
                    for i in idx:
                        src = src[i]
                    s_ = b % NS
                    if nr == 1024:
                        srcv = src[r0 + hf * 512:r0 + (hf + 1) * 512, c0:c0 + ncw].rearrange("(kc p) n -> p kc n", p=128)
                        sv = stg[j % 3][:, 0:4 * ncw].rearrange("p (kc n) -> p kc n", kc=4)
                        dv = ring[s_][:, 0:8 * ncw].rearrange("p (kc n) -> p kc n", kc=8)[:, hf * 4:(hf + 1) * 4, :]
                    else:
                        kc = nr // 128
                        srcv = src[r0:r0 + nr, c0 + hf * 512:c0 + (hf + 1) * 512].rearrange("(kc p) n -> p kc n", p=128)
                        sv = stg[j % 3][:, 0:kc * 512].rearrange("p (kc n) -> p kc n", kc=kc)
                        dv = ring[s_][:, 0:kc * 1024].rearrange("p (kc n) -> p kc n", kc=kc)[:, :, hf * 512:(hf + 1) * 512]
                    return srcv, sv, dv, s_

                def ensure_dma(self, upto):
                    while self.nd <= min(upto, 2 * len(fblocks) - 1):
                        j = self.nd
                        srcv, sv, dv, s_ = self._half(j)
                        P.dma("sp", sv, srcv, writes=[("stg", j % 3)])
                        self.nd += 1

                def ensure_cast(self, upto):
                    while self.ncast <= min(upto, len(fblocks) - 1):
                        b = self.ncast
                        for hf in range(2):
                            j = 2 * b + hf
                            self.ensure_dma(j)
                            srcv, sv, dv, s_ = self._half(j)
                            cp(dv, sv, [("stg", j % 3)], [("ring", s_)], eng="act")
                        self.ncast += 1

                def next(self):
                    b = self.n
                    self.n += 1
                    self.ensure_cast(b)
                    self.ensure_dma(2 * b + 3)
                    self.ensure_cast(b + 1)
                    self.ensure_dma(2 * b + 5)
                    name, idx, r0, nr, c0, ncw = fblocks[b]
                    kc = nr // 128
                    view = ring[b % NS][:, 0:kc * ncw].rearrange("p (kc n) -> p kc n", kc=kc)
                    return view, ("ring", b % NS)

            fs = FS()

            def ffn_group(gname, uname, dname, idx, f0, fw, first_scale, wcol):
                fc = fw // 128
                wg, kg = fs.next()
                wu, ku = fs.next()
                wd, kd = fs.next()
                for fcl in range(fc):
                    for tq in range(4):
                        pg, kpg = next_ps()
                        for kc in range(8):
                            mm(pg[:], wg[:, kc, fcl * 128:(fcl + 1) * 128], xT[:, kc, tq * 512:(tq + 1) * 512],
                               kc == 0, kc == 7, [kg] + K("xT", range(tq * 4, tq * 4 + 4)), [kpg])
                        pu, kpu = psb[2 + puc[0] % 2], ("ps", 2 + puc[0] % 2)
                        puc[0] += 1
                        for kc in range(8):
                            mm(pu[:], wu[:, kc, fcl * 128:(fcl + 1) * 128], xT[:, kc, tq * 512:(tq + 1) * 512],
                               kc == 0, kc == 7, [ku] + K("xT", range(tq * 4, tq * 4 + 4)), [kpu])
                        act(sg, pg[:], AF.Silu, [kpg], ["sg"])
                        tt(hT[:, fcl, tq * 512:(tq + 1) * 512], pu[:], sg, ALU.mult, ["sg", kpu], [("hT", fcl, tq)])
                for tt_ in range(16):
                    kxr = ("xres", tt_)
                    for hh in range(2):
                        pd, kpd = next_ps()
                        for fcl in range(fc):
                            mm(pd[:], hT[:, fcl, tt_ * 128:(tt_ + 1) * 128], wd[:, fcl, hh * 512:(hh + 1) * 512],
                               fcl == 0, fcl == fc - 1, [kd, ("hT", fcl, tt_ // 4)], [kpd])
                        xr = xres[:, tt_, hh * 512:(hh + 1) * 512]
                        if wcol is not None:
                            stt(xr, pd[:], wcol(tt_), xr, ALU.mult, ALU.add, [kpd, kxr, "wexp"], [kxr])
                        elif first_scale:
                            stt(xr, xr, DN_ALPHA, pd[:], ALU.mult, ALU.add, [kpd, kxr], [kxr])
                        else:
                            tt(xr, pd[:], xr, ALU.add, [kpd, kxr], [kxr])

            def add_blocks(gname, uname, dname, idx, f0, fw):
                fblocks.extend([(gname, idx, 0, 1024, f0, fw), (uname, idx, 0, 1024, f0, fw), (dname, idx, f0, fw, 0, 1024)])

            if not moe:
                f0 = 0
                while f0 < F_DENSE:
                    fw = min(512, F_DENSE - f0)
                    add_blocks("ffn_w_gate", "ffn_w_up", "ffn_w_down", (L // 2,), f0, fw)
                    f0 += fw
            else:
                for e_ in range(NEXP):
                    for gi in range(F_EXP // 512):
                        add_blocks("moe_w_gate", "moe_w_up", "moe_w_down", (L // 2, 0 if MOE_FAKE else e_),
                                   0 if MOE_FAKE else gi * 512, 512)
            if not moe:
                i_ = L // 2
                f0 = 0
                while f0 < F_DENSE:
                    fw = min(512, F_DENSE - f0)
                    ffn_group("ffn_w_gate", "ffn_w_up", "ffn_w_down", (i_,), f0, fw, f0 == 0, None)
                    f0 += fw
            else:
                i_ = L // 2
                lk = K("logits", range(16))
                red(m1, logits, lk, ["m1"], op=ALU.max)
                tt(t1, logits, m1.unsqueeze(2).broadcast_to([128, 16, 8]), ALU.is_equal, lk + ["m1"], ["t1"])
                stt(t2, t1, NEG, logits, ALU.mult, ALU.add, ["t1"] + lk, ["t2"])
                red(m2, t2, ["t2"], ["m2"], op=ALU.max)
                tt(t3, t2, m2.unsqueeze(2).broadcast_to([128, 16, 8]), ALU.is_equal, ["t2", "m2"], ["t3"])
                tt(m1, m1, m2, ALU.subtract, ["m1", "m2"], ["m1"])
                act(m1, m1, AF.Sigmoid, ["m1"], ["m1"])
                ts(m2, m1, -1.0, 1.0, ALU.mult, ALU.add, ["m1"], ["m2"])
                tt(t1, t1, m1.unsqueeze(2).broadcast_to([128, 16, 8]), ALU.mult, ["t1", "m1"], ["t1"])
                tt(t3, t3, m2.unsqueeze(2).broadcast_to([128, 16, 8]), ALU.mult, ["t3", "m2"], ["t3"])
                tt(wexp, t1, t3, ALU.add, ["t1", "t3"], ["wexp"])
                for tt_ in range(16):
                    ts(xres[:, tt_, :], xres[:, tt_, :], DN_ALPHA, None, ALU.mult, None, [("xres", tt_)], [("xres", tt_)])
                for e_ in range(NEXP):
                    for gi in range(F_EXP // 512):
                        ffn_group("moe_w_gate", "moe_w_up", "moe_w_down", (i_, 0 if MOE_FAKE else e_),
                                  0 if MOE_FAKE else gi * 512, 512, False,
                                  (lambda e__: (lambda tt_: wexp[:, tt_, e__:e__ + 1]))(e_))
            if stage("ffn", L):
                for tt_ in range(16):
                    P.dma("sp", dbg_ap[:, tt_ * 1024:(tt_ + 1) * 1024], xres[:, tt_, :], reads=[("xres", tt_)],
                          writes=[("dbgo", tt_)], semkey="dbg")
                raise Stop()

            P.dma("sp", lnG[:], din["ln2_g"][L].partition_broadcast(128), writes=["lnG"], semkey="lnp")
            P.dma("sp", lnB[:], din["ln2_b"][L].partition_broadcast(128), writes=["lnB"], semkey="lnp")
            ln_A(0, ("xres", 0))
            for tt_ in range(16):
                kxr = ("xres", tt_)
                if tt_ + 1 < 16:
                    ln_A(tt_ + 1, ("xres", tt_ + 1))
                ln_B(tt_, kxr)
                if L == nlayers - 1:
                    P.dma("sp", out_ap[tt_ * 128:(tt_ + 1) * 128, :], xres[:, tt_, :], reads=[kxr], writes=[("out", tt_)],
                          semkey="out")
                else:
                    P.dma("sp", xs_ap[tt_ * 128:(tt_ + 1) * 128, :], xres[:, tt_, :], reads=[kxr], writes=[("xs", tt_)],
                          semkey="xs")
                    tiles_to_xT(tt_, xres[:, tt_, :], [kxr])
    except Stop:
        pass
    P.emit()
    return nc, P


_CACHE = {}


def kernel(**inputs):
    n = 8
    if "prog" not in _CACHE:
        _CACHE["prog"] = build_program()[0]
    nc = _CACHE["prog"]
    consts = host_consts()
    x = np.ascontiguousarray(inputs["x"], dtype=np.float32)
    shared = {k: np.ascontiguousarray(inputs[k], dtype=np.float32) for k in W_SHAPES}
    shared.update(consts)
    in_maps = []
    for c in range(n):
        m = dict(shared)
        m["x"] = np.ascontiguousarray(x[c])
        in_maps.append(m)
    res = run_bass_kernel_spmd(nc, in_maps, core_ids=list(range(n)))
    return np.stack([np.asarray(r["out"], dtype=np.float32) for r in res.results], axis=0)
```
